# Optimizing a Trainium2 kernel written in Bass

```python
import math
import jax, jax.numpy as jnp
from jax import lax
import numpy as np

D_MODEL = 1024
BATCH = 8
SEQ = 8192
DEPTH = 1

D_CONV = 512
CONV_WIDTH = 31
N_Q_HEADS = 8
N_KV_HEADS = 2
HEAD_DIM = 64
Q_PER_KV = N_Q_HEADS // N_KV_HEADS
WINDOW = 128
N_BUCKETS = 32
MAX_DISTANCE = 128
N_GROUPS = 4
EXPERTS_PER_GROUP = 8
N_EXPERTS = N_GROUPS * EXPERTS_PER_GROUP
TOP_K_IN_GROUP = 2
D_EXPERT = 256
MOE_BLOCK = 512
N_BRANCHES = 2
D_Q = N_Q_HEADS * HEAD_DIM
D_KV = N_KV_HEADS * HEAD_DIM
D_IN = 2 * D_CONV + D_Q + 2 * D_KV + N_BRANCHES * D_MODEL
EPS = 1e-6
NEG_INF = -1e30

kernel_name = "hybrid_conv_swa_hmoe_block"


def rms_norm(t, g):
    tf = t.astype(jnp.float32)
    y = tf * lax.rsqrt(jnp.mean(tf * tf, axis=-1, keepdims=True) + EPS)
    return (y * g.astype(jnp.float32)).astype(t.dtype)


def layer_norm(t, g, b):
    tf = t.astype(jnp.float32)
    mu = jnp.mean(tf, axis=-1, keepdims=True)
    var = jnp.mean(jnp.square(tf - mu), axis=-1, keepdims=True)
    y = (tf - mu) * lax.rsqrt(var + EPS) * g.astype(jnp.float32) + b.astype(jnp.float32)
    return y.astype(t.dtype)


def t5_causal_bucket(dist):
    max_exact = N_BUCKETS // 2
    d = jnp.maximum(dist, 1).astype(jnp.float32)
    large = max_exact + (jnp.log(d / max_exact) / math.log(MAX_DISTANCE / max_exact)
                         * (N_BUCKETS - max_exact)).astype(jnp.int32)
    large = jnp.minimum(large, N_BUCKETS - 1)
    return jnp.where(dist < max_exact, dist, large)


def conformer_conv(a, b, dw_kernel, dw_bias, ln_g, ln_b, w_pw_out):
    u = a * jax.nn.sigmoid(b)
    u = lax.conv_general_dilated(
        u, dw_kernel[:, None, :].astype(u.dtype), window_strides=(1,),
        padding=[(CONV_WIDTH - 1, 0)], dimension_numbers=('NWC', 'WIO', 'NWC'),
        feature_group_count=D_CONV) + dw_bias
    u = jax.nn.silu(layer_norm(u, ln_g, ln_b))
    return u @ w_pw_out


def sliding_window_attention(q, k, v, q_norm_g, k_norm_g, rel_bias_table, sinks):
    B, T = q.shape[0], q.shape[1]
    nb = T // WINDOW
    q = rms_norm(q, q_norm_g)
    k = rms_norm(k, k_norm_g)
    qb = q.reshape(B, nb, WINDOW, N_KV_HEADS, Q_PER_KV, HEAD_DIM)

    def band(t):
        tb = t.reshape(B, nb, WINDOW, N_KV_HEADS, HEAD_DIM)
        prev = jnp.pad(tb, ((0, 0), (1, 0), (0, 0), (0, 0), (0, 0)))[:, :-1]
        return jnp.concatenate([prev, tb], axis=2)

    kb, vb = band(k), band(v)
    s = jnp.einsum('bnqhgd,bnshd->bnhgqs', qb, kb,
                   preferred_element_type=jnp.float32) * (HEAD_DIM ** -0.5)
    qi = jnp.arange(WINDOW)[:, None]
    kj = jnp.arange(2 * WINDOW)[None, :]
    dist = qi + WINDOW - kj
    in_window = (dist >= 0) & (dist < WINDOW)
    bias = rel_bias_table[t5_causal_bucket(jnp.clip(dist, 0, MAX_DISTANCE))]
    bias = bias.astype(jnp.float32).transpose(2, 0, 1).reshape(
        N_KV_HEADS, Q_PER_KV, WINDOW, 2 * WINDOW)
    key_pos = jnp.arange(nb)[:, None] * WINDOW + kj - WINDOW
    mask = in_window[None] & (key_pos >= 0)[:, None, :]
    logits = jnp.where(mask[None, :, None, None], s + bias, NEG_INF)
    sink = sinks.astype(jnp.float32).reshape(N_KV_HEADS, Q_PER_KV)[:, :, None, None]
    m = jnp.maximum(jnp.max(logits, axis=-1, keepdims=True), sink)
    p = jnp.exp(logits - m)
    probs = p / (jnp.sum(p, axis=-1, keepdims=True) + jnp.exp(sink - m))
    o = jnp.einsum('bnhgqs,bnshd->bnqhgd', probs.astype(v.dtype), vb)
    return o.reshape(B, T, D_Q)


def hierarchical_moe(h, w_router_group, b_router_group, w_router_expert, b_router_expert,
                     w_exp_gate, w_exp_up, w_exp_down):
    B, T, D = h.shape
    N = B * T
    hf = h.reshape(N, D)
    gl = (hf @ w_router_group).astype(jnp.float32) + b_router_group.astype(jnp.float32)
    pg = jax.nn.softmax(gl, axis=-1)
    _, g_idx = lax.top_k(gl, 1)
    p_top = jnp.take_along_axis(pg, g_idx, axis=-1)
    el = (hf @ w_router_expert).astype(jnp.float32) + b_router_expert.astype(jnp.float32)
    el = el.reshape(N, N_GROUPS, EXPERTS_PER_GROUP)
    el_sel = jnp.take_along_axis(el, g_idx[:, :, None], axis=1)[:, 0]
    pe = jax.nn.softmax(el_sel, axis=-1)
    vals, e_in = lax.top_k(pe, TOP_K_IN_GROUP)
    w = vals / jnp.sum(vals, axis=-1, keepdims=True) * p_top
    expert_id = g_idx * EXPERTS_PER_GROUP + e_in

    M = N * TOP_K_IN_GROUP
    e_flat = expert_id.reshape(M)
    w_flat = w.reshape(M)
    tok = jnp.repeat(jnp.arange(N, dtype=jnp.int32), TOP_K_IN_GROUP)
    order = jnp.argsort(e_flat)
    e_sorted = e_flat[order]
    counts = jnp.zeros((N_EXPERTS,), jnp.int32).at[e_flat].add(1)
    start = jnp.cumsum(counts) - counts
    pcounts = (counts + MOE_BLOCK - 1) // MOE_BLOCK * MOE_BLOCK
    pend = jnp.cumsum(pcounts)
    pstart = pend - pcounts
    dest = pstart[e_sorted] + (jnp.arange(M, dtype=jnp.int32) - start[e_sorted])
    n_blocks = -(-M // MOE_BLOCK) + N_EXPERTS
    n_slots = n_blocks * MOE_BLOCK
    slot_tok = jnp.full((n_slots,), N, jnp.int32).at[dest].set(tok[order])
    slot_w = jnp.zeros((n_slots,), jnp.float32).at[dest].set(w_flat[order])
    block_e = jnp.minimum(
        jnp.searchsorted(pend, jnp.arange(n_blocks, dtype=jnp.int32) * MOE_BLOCK, side='right'),
        N_EXPERTS - 1)
    hpad = jnp.concatenate([hf, jnp.zeros((1, D), hf.dtype)], axis=0)

    def expert_block(args):
        e, toks, ws = args
        xb = hpad[toks]
        hid = jax.nn.silu(xb @ w_exp_gate[e]) * (xb @ w_exp_up[e])
        return (hid @ w_exp_down[e]) * ws[:, None].astype(xb.dtype)

    yb = lax.map(expert_block, (block_e, slot_tok.reshape(n_blocks, MOE_BLOCK),
                                slot_w.reshape(n_blocks, MOE_BLOCK)))
    out = jnp.zeros((N + 1, D), yb.dtype).at[slot_tok].add(yb.reshape(n_slots, D))[:N]
    return out.reshape(B, T, D)


def setup_inputs(seed: int = 0) -> dict:
    key = jax.random.key(seed)
    ks = jax.random.split(key, 32)
    f32 = jnp.float32
    nrm = lambda k, shape, s: jax.random.normal(k, shape, f32) * s
    L, D = DEPTH, D_MODEL
    return {
        "x": nrm(ks[0], (BATCH, SEQ, D), 1.0),
        "c": nrm(ks[1], (BATCH, D), 1.0),
        "w_ada": nrm(ks[2], (L, D, 6 * D), 0.5 * D ** -0.5),
        "b_ada": nrm(ks[3], (L, 6 * D), 0.02),
        "norm_mix_g": 1.0 + nrm(ks[4], (L, D), 0.02),
        "w_in": nrm(ks[5], (L, D, D_IN), D ** -0.5),
        "dw_kernel": nrm(ks[6], (L, CONV_WIDTH, D_CONV), CONV_WIDTH ** -0.5),
        "dw_bias": nrm(ks[7], (L, D_CONV), 0.02),
        "conv_ln_g": 1.0 + nrm(ks[8], (L, D_CONV), 0.02),
        "conv_ln_b": nrm(ks[9], (L, D_CONV), 0.02),
        "w_conv_out": nrm(ks[10], (L, D_CONV, D), D_CONV ** -0.5),
        "q_norm_g": 1.0 + nrm(ks[11], (L, HEAD_DIM), 0.02),
        "k_norm_g": 1.0 + nrm(ks[12], (L, HEAD_DIM), 0.02),
        "sinks": nrm(ks[13], (L, N_Q_HEADS), 0.5),
        "w_attn_out": nrm(ks[14], (L, D_Q, D), D_Q ** -0.5),
        "w_out": nrm(ks[15], (L, D, D), D ** -0.5),
        "rel_bias_table": nrm(ks[16], (N_BUCKETS, N_Q_HEADS), 0.5),
        "norm_ffn_g": 1.0 + nrm(ks[17], (L, D), 0.02),
        "w_router_group": nrm(ks[18], (L, D, N_GROUPS), D ** -0.5),
        "b_router_group": nrm(ks[19], (L, N_GROUPS), 0.01),
        "w_router_expert": nrm(ks[20], (L, D, N_EXPERTS), D ** -0.5),
        "b_router_expert": nrm(ks[21], (L, N_EXPERTS), 0.01),
        "w_exp_gate": nrm(ks[22], (L, N_EXPERTS, D, D_EXPERT), D ** -0.5),
        "w_exp_up": nrm(ks[23], (L, N_EXPERTS, D, D_EXPERT), D ** -0.5),
        "w_exp_down": nrm(ks[24], (L, N_EXPERTS, D_EXPERT, D), D_EXPERT ** -0.5),
    }


def reference(x, c, w_ada, b_ada, norm_mix_g, w_in, dw_kernel, dw_bias, conv_ln_g, conv_ln_b,
              w_conv_out, q_norm_g, k_norm_g, sinks, w_attn_out, w_out, rel_bias_table,
              norm_ffn_g, w_router_group, b_router_group, w_router_expert, b_router_expert,
              w_exp_gate, w_exp_up, w_exp_down):
    B, T, _ = x.shape
    splits = [D_CONV, 2 * D_CONV, 2 * D_CONV + D_Q, 2 * D_CONV + D_Q + D_KV,
              2 * D_CONV + D_Q + 2 * D_KV]
    for l in range(DEPTH):
        mod = jax.nn.silu(c) @ w_ada[l] + b_ada[l]
        sh1, sc1, g1, sh2, sc2, g2 = jnp.split(mod[:, None, :], 6, axis=-1)

        h = rms_norm(x, norm_mix_g[l]) * (1.0 + sc1) + sh1
        proj = h @ w_in[l]
        a_c, b_c, q, k, v, gate_logits = jnp.split(proj, splits, axis=-1)
        y_conv = conformer_conv(a_c, b_c, dw_kernel[l], dw_bias[l], conv_ln_g[l],
                                conv_ln_b[l], w_conv_out[l])
        y_attn = sliding_window_attention(
            q.reshape(B, T, N_Q_HEADS, HEAD_DIM), k.reshape(B, T, N_KV_HEADS, HEAD_DIM),
            v.reshape(B, T, N_KV_HEADS, HEAD_DIM), q_norm_g[l], k_norm_g[l],
            rel_bias_table, sinks[l]) @ w_attn_out[l]
        g_conv, g_attn = jnp.split(jax.nn.sigmoid(gate_logits), N_BRANCHES, axis=-1)
        x = x + g1 * ((g_conv * y_conv + g_attn * y_attn) @ w_out[l])

        h2 = rms_norm(x, norm_ffn_g[l]) * (1.0 + sc2) + sh2
        x = x + g2 * hierarchical_moe(h2, w_router_group[l], b_router_group[l],
                                      w_router_expert[l], b_router_expert[l],
                                      w_exp_gate[l], w_exp_up[l], w_exp_down[l])
    return x
```

```python
import math
import numpy as np
import concourse.bass as bass
import concourse.mybir as mybir
from concourse.bass_utils import run_bass_kernel_spmd

F32 = mybir.dt.float32
BF16 = mybir.dt.bfloat16
I32 = mybir.dt.int32
ALU = mybir.AluOpType
ACTF = mybir.ActivationFunctionType
AX = mybir.AxisListType

ENGS = ["tensor", "vector", "scalar", "gpsimd", "sync"]

D = 1024
DIN = 3840
TT = 256
NBS = TT // 128
EPS = 1e-6
NE = 32
DE = 256
SUB = 2
BLK = 128 * SUB


class Tl:
    def __init__(self, name, t):
        self.name = name
        self.t = t
        self.w = []
        self.r = []
        self.lg = None
        self.sg = None

    def __getitem__(self, k):
        return self.t[k]


class Grp:
    def __init__(self, name):
        self.name = name
        self.n = 0
        self.sem = None


class Op:
    __slots__ = ("eng", "fn", "deps", "grp", "signal", "count", "waits")

    def __init__(self, eng, fn, deps, grp=None):
        self.eng = eng
        self.fn = fn
        self.deps = deps
        self.grp = grp
        self.signal = False
        self.count = None
        self.waits = None


class Prog:
    def __init__(self, nc):
        self.nc = nc
        self.ops = {e: [] for e in ENGS}
        self.grps = []
        self.extra = {e: [] for e in ENGS}
        self.defer = None

    def grp(self, name):
        g = Grp(name)
        self.grps.append(g)
        return g

    def op(self, eng, fn, reads=(), writes=(), grp=None):
        if self.defer is not None:
            self.defer.append((eng, fn, list(reads), list(writes), grp))
            return None
        deps = []
        for t in reads:
            deps.extend(t.w)
        for t in writes:
            deps.extend(t.w)
            deps.extend(t.r)
        deps.extend(self.extra[eng])
        self.extra[eng] = []
        o = Op(eng, fn, deps, grp)
        self.ops[eng].append(o)
        idx = len(self.ops[eng]) - 1
        if grp is not None:
            grp.n += 1
            tok = ("d", grp, grp.n)
        else:
            tok = ("c", eng, idx)
        wset = set(id(t) for t in writes)
        for t in reads:
            if id(t) in wset:
                continue
            t.r = [x for x in t.r if not (x[0] == tok[0] and x[1] is tok[1])] + [tok]
        for t in writes:
            t.w = [tok]
            t.r = []
        return tok

    def barrier(self, exclude=()):
        toks = []
        for e in ENGS:
            if self.ops[e]:
                toks.append(("c", e, len(self.ops[e]) - 1))
        for g in self.grps:
            if g.n and not any(g is x for x in exclude):
                toks.append(("d", g, g.n))
        for e in ENGS:
            self.extra[e].extend(toks)

    def finalize(self):
        for e in ENGS:
            known_c = {}
            known_d = {}
            for i, o in enumerate(self.ops[e]):
                waits = []
                for d in o.deps:
                    if d[0] == "c":
                        _, e2, j = d
                        if e2 == e and (e == "tensor" or j < i - 2):
                            continue
                        if known_c.get(e2, -1) >= j:
                            continue
                        known_c[e2] = j
                        waits.append(d)
                    else:
                        _, g, n = d
                        if known_d.get(id(g), 0) >= n:
                            continue
                        known_d[id(g)] = n
                        waits.append(d)
                o.waits = waits
        for e in ENGS:
            for o in self.ops[e]:
                for d in o.waits:
                    if d[0] == "c":
                        self.ops[d[1]][d[2]].signal = True
        for e in ENGS:
            c = 0
            for o in self.ops[e]:
                if o.signal and o.grp is None:
                    c += 1
                    o.count = c

    def emit_engine(self, e, eng, sems):
        for o in self.ops[e]:
            for d in o.waits:
                if d[0] == "c":
                    tgt = self.ops[d[1]][d[2]]
                    if tgt.count is None:
                        continue
                    eng.wait_ge(sems[d[1]], tgt.count)
                else:
                    eng.wait_ge(d[1].sem, 16 * d[2])
            ins = o.fn(eng)
            if o.grp is not None:
                ins.then_inc(o.grp.sem, 16)
            elif o.signal:
                ins.then_inc(sems[e], 1)


def _dsize(dt):
    return 2 if dt == BF16 else 4


def build(T, phase1_only=False, debug=False, stop=None):
    NB = T // 128
    NS = T // TT
    NBLK = -(-2 * T // BLK) + NE
    NSLOT = NBLK * BLK
    nc = bass.Bass("TRN2", target_bir_lowering=False)
    P = Prog(nc)

    def din(name, shape, dt=F32):
        return nc.dram_tensor(name, shape, dt, kind="ExternalInput").ap()

    x = din("x", [T, D])
    c_col = din("c_col", [128, 8])
    w_ada = din("w_ada", [D, 6 * D])
    b_ada = din("b_ada", [1, 6 * D])
    gmix = din("gmix", [1, D])
    gffn = din("gffn", [1, D])
    w_in = din("w_in", [D, DIN])
    kern = din("kern", [128, 4, 31])
    cvec = din("cvec", [128, 4, 3])
    wco = din("wco", [512, D])
    wao = din("wao", [512, D])
    wout = din("wout", [D, D])
    gq = din("gq", [1, 64])
    gk = din("gk", [1, 64])
    sinks = din("sinks", [1, 8])
    bm = din("bm", [128, 8, 2, 128])
    wr = din("wr", [D, 36])
    br = din("br", [1, 36])
    weg = din("weg", [NE, D, DE])
    weu = din("weu", [NE, D, DE])
    wed = din("wed", [NE, DE, D])
    out = nc.dram_tensor("out", [T, D], F32, kind="ExternalOutput").ap()
    H2 = nc.dram_tensor("h2_scr", [T, D], BF16, kind="Internal").ap()
    Ldram = nc.dram_tensor("l_scr", [128, NB, 36], F32, kind="Internal").ap()
    WGU = nc.dram_tensor("wgu_scr", [NE * 128, 8, 2 * DE], BF16, kind="Internal").ap()
    WDS = nc.dram_tensor("wd_scr", [NE * 128, 2, D], BF16, kind="Internal").ap()
    XS = nc.dram_tensor("xs_scr", [NSLOT, D], BF16, kind="Internal").ap()
    YS = nc.dram_tensor("ys_scr", [NSLOT, D], F32, kind="Internal").ap()
    if debug:
        Ldbg = nc.dram_tensor("Ldbg", [128, NB, 36], F32, kind="ExternalOutput").ap()
        Ddbg = nc.dram_tensor("Ddbg", [128, 4, NB], F32, kind="ExternalOutput").ap()

    off = [16640]
    LIMIT = 229376 - 512

    def sb(name, shape, dt):
        nbytes = int(np.prod(shape[1:])) * _dsize(dt)
        nbytes = (nbytes + 63) // 64 * 64
        t = nc.alloc_sbuf_tensor_at(name, list(shape), dt, offset=off[0])
        off[0] += nbytes
        assert off[0] <= LIMIT, f"SBUF overflow at {name}: {off[0]}"
        return Tl(name, t)

    def ps(name, shape, dt):
        return Tl(name, nc.alloc_psum_tensor(name, list(shape), dt))

    tp = [ps(f"tp{i}", [128, 1024], BF16) for i in range(2)]
    mmb = [ps(f"mm{i}", [128, 512], F32) for i in range(4)]
    attA = ps("attA", [128, 512], F32)
    attB = ps("attB", [128, 512], F32)
    tpi = [0]
    mmi = [0]

    def gtp():
        tpi[0] += 1
        return tp[tpi[0] % 2]

    def gmm():
        mmi[0] += 1
        return mmb[mmi[0] % 4]

    def gmm_ab():
        mmi[0] += 1
        return mmb[mmi[0] % 2]

    def gmm_g():
        mmi[0] += 1
        return mmb[2 + mmi[0] % 2]

    def E(eng, fn, reads, writes, *a, **k):
        return P.op(eng, lambda e: getattr(e, fn)(*a, **k), reads, writes)

    def V(fn, reads, writes, *a, **k):
        return E("vector", fn, reads, writes, *a, **k)

    def A(fn, reads, writes, *a, **k):
        return E("scalar", fn, reads, writes, *a, **k)

    def G(fn, reads, writes, *a, **k):
        return E("gpsimd", fn, reads, writes, *a, **k)

    def MM(o_tl, o_ap, l_tl, l_ap, r_tl, r_ap, start, stop):
        return P.op("tensor", lambda e: e.matmul(o_ap, lhsT=l_ap, rhs=r_ap, start=start, stop=stop),
                    [l_tl, r_tl], [o_tl])

    def TR(o_tl, o_ap, i_tl, i_ap, id_tl):
        return P.op("tensor", lambda e: e.transpose(out=o_ap, in_=i_ap, identity=id_tl[:]),
                    [i_tl, id_tl], [o_tl])

    def load(q, tl, o_ap, i_ap):
        if tl.lg is None:
            tl.lg = P.grp("l_" + tl.name)
        return P.op(q, lambda e: e.dma_start(out=o_ap, in_=i_ap), [], [tl], grp=tl.lg)

    def store(q, tl, o_ap, i_ap):
        if tl.sg is None:
            tl.sg = P.grp("s_" + tl.name)
        return P.op(q, lambda e: e.dma_start(out=o_ap, in_=i_ap), [tl], [], grp=tl.sg)

    mod = sb("mod", [128, 6 * D], F32)
    B1, A1, G1 = mod[:, 0:D], mod[:, D:2 * D], mod[:, 2 * D:3 * D]
    B2, A2, G2 = mod[:, 3 * D:4 * D], mod[:, 4 * D:5 * D], mod[:, 5 * D:6 * D]
    ident_f = sb("ident_f", [128, 128], F32)
    ident_b = sb("ident_b", [128, 128], BF16)
    nhalf = sb("nhalf", [128, 1], F32)
    persist_end = off[0]

    G("memset", [], [ident_f], ident_f[:], 1.0)
    G("affine_select", [ident_f], [ident_f], out=ident_f[:], in_=ident_f[:], pattern=[[-1, 128]],
      compare_op=ALU.is_equal, fill=0.0, base=0, channel_multiplier=1)
    V("tensor_copy", [ident_f], [ident_b], out=ident_b[:], in_=ident_f[:])
    G("memset", [], [nhalf], nhalf[:], -0.5)

    w_in_sb = sb("w_in_sb", [128, 8, DIN], BF16)
    wco_sb = sb("wco_sb", [128, 4, D], BF16)
    wao_sb = sb("wao_sb", [128, 4, D], BF16)
    wout_sb = sb("wout_sb", [128, 8, D], BF16)
    kern_sb = sb("kern_sb", [128, 4, 31], F32)
    cvec_sb = sb("cvec_sb", [128, 4, 3], F32)
    bm_sb = sb("bm_sb", [128, 8, 2, 128], F32)
    wr_sb = sb("wr_sb", [128, 8, 36], F32)
    brb = sb("brb", [128, 36], F32)
    gqb = sb("gqb", [128, 64], F32)
    gkb = sb("gkb", [128, 64], F32)
    gq8 = sb("gq8", [128, 8, 64], F32)
    gk2 = sb("gk2", [128, 2, 64], F32)
    snk = sb("snk", [128, 8], F32)
    esink = sb("esink", [128, 8], F32)
    negc = sb("negc", [128, 1], F32)
    mq = sb("mq", [128, 1], F32)
    mk = sb("mk", [128, 1], F32)
    onesdiv = sb("onesdiv", [128, 128], BF16)

    cgrp = P.grp("wcast")

    def expert_casts(e_):
        rws = slice(e_ * 128, (e_ + 1) * 128)
        P.op("gpsimd", lambda e: e.dma_start(
            out=WGU[rws, :, 0:DE], in_=weg[e_, :, :].rearrange("(p kc) f -> p kc f", p=128)), [], [], grp=cgrp)
        P.op("gpsimd", lambda e: e.dma_start(
            out=WGU[rws, :, DE:2 * DE], in_=weu[e_, :, :].rearrange("(p kc) f -> p kc f", p=128)), [], [], grp=cgrp)
        P.op("gpsimd", lambda e: e.dma_start(
            out=WDS[rws, :, :], in_=wed[e_, :, :].rearrange("(p kc) n -> p kc n", p=128)), [], [], grp=cgrp)
    load("sync", kern_sb, kern_sb[:], kern[:, :, :])
    load("sync", cvec_sb, cvec_sb[:], cvec[:, :, :])
    load("sync", bm_sb, bm_sb[:], bm[:, :, :, :])
    load("sync", wr_sb, wr_sb[:], wr.rearrange("(kc p) n -> p kc n", p=128))
    load("sync", brb, brb[:], br[0, :].partition_broadcast(128))
    load("sync", gqb, gqb[:], gq[0, :].partition_broadcast(128))
    load("sync", gkb, gkb[:], gk[0, :].partition_broadcast(128))
    load("sync", snk, snk[:], sinks[0, :].partition_broadcast(128))
    resident_end = off[0]
    stgw = [sb(f"stgw{i}", [128, 4096], F32) for i in range(2)]
    w_in_v = w_in.rearrange("(kc p) n -> p kc n", p=128)
    wco_v = wco.rearrange("(kc p) n -> p kc n", p=128)
    wao_v = wao.rearrange("(kc p) n -> p kc n", p=128)
    wout_v = wout.rearrange("(kc p) n -> p kc n", p=128)
    jobs = []
    for kc in range(8):
        jobs.append((w_in_v[:, kc, :], w_in_sb, w_in_sb[:, kc, :], 3840))
    jobs.append((wco_v, wco_sb, wco_sb[:], 4096))
    jobs.append((wao_v, wao_sb, wao_sb[:], 4096))
    for hh_ in range(2):
        jobs.append((wout_v[:, hh_ * 4:(hh_ + 1) * 4, :], wout_sb, wout_sb[:, hh_ * 4:(hh_ + 1) * 4, :], 4096))
    for ji, (src, dtl, dap, nel) in enumerate(jobs):
        st_ = stgw[ji % 2]
        sview = st_[:, 0:nel]
        if len(src.shape) == 3:
            sview = sview.rearrange("p (k n) -> p k n", k=src.shape[1])
        load("scalar", st_, sview, src)
        if ji % 2 == 0:
            V("tensor_copy", [st_], [dtl], out=dap, in_=sview)
        else:
            A("copy", [st_], [dtl], out=dap, in_=sview)

    csb = sb("csb", [128, 8], F32)
    scs = sb("scs", [128, 8], F32)
    scb = sb("scb", [128, 8, 128], F32)
    gmb = sb("gmb", [128, D], F32)
    gfb = sb("gfb", [128, D], F32)
    wa = [sb(f"wa{i}", [128, 8, 512], F32) for i in range(2)]
    load("sync", csb, csb[:], c_col[:, :])
    load("sync", mod, mod[:], b_ada[0, :].partition_broadcast(128))
    load("sync", gmb, gmb[:], gmix[0, :].partition_broadcast(128))
    load("sync", gfb, gfb[:], gffn[0, :].partition_broadcast(128))
    A("activation", [csb], [scs], out=scs[:], in_=csb[:], func=ACTF.Silu)
    for kc in range(8):
        V("tensor_copy", [scs], [scb], out=scb[:, kc, :], in_=scs[:, kc:kc + 1].to_broadcast([128, 128]))
    w_ada_v = w_ada.rearrange("(kc p) n -> p kc n", p=128)
    for n in range(12):
        wt = wa[n % 2]
        load("sync", wt, wt[:], w_ada_v[:, :, n * 512:(n + 1) * 512])
        pm = gmm()
        for kc in range(8):
            MM(pm, pm[:], scb, scb[:, kc, :], wt, wt[:, kc, :], kc == 0, kc == 7)
        V("tensor_tensor", [pm, mod], [mod], out=mod[:, n * 512:(n + 1) * 512], in0=pm[:],
          in1=mod[:, n * 512:(n + 1) * 512], op=ALU.add)
    V("scalar_tensor_tensor", [mod, gmb], [mod], out=A1, in0=A1, scalar=1.0, in1=gmb[:],
      op0=ALU.add, op1=ALU.mult)
    V("scalar_tensor_tensor", [mod, gfb], [mod], out=A2, in0=A2, scalar=1.0, in1=gfb[:],
      op0=ALU.add, op1=ALU.mult)
    P.barrier()
    off[0] = resident_end
    NS_run = NS
    if stop == "p0":
        NS_run = 0

    G("memset", [], [onesdiv], onesdiv[:], 1.0 / 512.0)
    for kc in range(8):
        V("tensor_tensor", [wout_sb, mod], [wout_sb], out=wout_sb[:, kc, :], in0=wout_sb[:, kc, :], in1=G1, op=ALU.mult)
    V("tensor_scalar", [gqb], [gq8], out=gq8[:], in0=gqb[:, None, :].to_broadcast([128, 8, 64]),
      scalar1=0.125, scalar2=None, op0=ALU.mult)
    V("tensor_copy", [gkb], [gk2], out=gk2[:], in_=gkb[:, None, :].to_broadcast([128, 2, 64]))
    V("reduce_max", [gqb], [mq], out=mq[:], in_=gqb[:], axis=AX.X, apply_absolute_value=True)
    V("reduce_max", [gkb], [mk], out=mk[:], in_=gkb[:], axis=AX.X, apply_absolute_value=True)
    V("scalar_tensor_tensor", [mq, mk], [negc], out=negc[:], in0=mq[:], scalar=-8.0, in1=mk[:],
      op0=ALU.mult, op1=ALU.mult)
    A("activation", [snk, negc], [esink], out=esink[:], in_=snk[:], func=ACTF.Exp, bias=negc[:, 0:1])

    xin = [sb(f"xin{i}", [128, D], F32) for i in range(2)]
    xr = [sb(f"xr{i}", [128, D], F32) for i in range(2)]
    tmpf = sb("tmpf", [128, D], F32)
    h2f = tmpf
    Lrow = [sb(f"Lrow{i}", [128, 36], F32) for i in range(2)]
    h2Tt = sb("h2Tt", [128, 1024], F32)
    hb = [sb(f"hb{i}", [128, D], BF16) for i in range(1)]
    h2b = [sb(f"h2b{i}", [128, D], BF16) for i in range(1)]
    hTs = [sb(f"hT{i}", [128, 8, TT], BF16) for i in range(2)]
    ubuf = sb("ubuf", [128, 4, TT + 30], F32)
    acc = [sb(f"acc{c}", [128, TT], F32) for c in range(4)]
    cbf = sb("cbf", [128, 4, TT], BF16)
    sqb = sb("sqb", [128, 4, TT], BF16)
    uT = sb("uT", [128, 4, TT], BF16)
    oT = sb("oT", [128, 4, TT], BF16)
    mT = sb("mT", [128, 8, TT], BF16)
    sgb = [sb(f"sgb{i}", [128, TT], F32) for i in range(1)] * 2
    s1 = [sb(f"s1_{i}", [128, TT], F32) for i in range(2)]
    s2 = [sb(f"s2_{i}", [128, TT], F32) for i in range(2)]
    t1 = s1
    t2 = s2
    mean_sb = s1[0]
    m2 = s2[0]
    var = m2
    rln = m2
    ssq = sb("ssq", [128, 1], F32)
    msq = sb("msq", [128, 1], F32)
    rstd = sb("rstd", [128, 1], F32)
    ssq2 = sb("ssq2", [128, 1], F32)
    msq2 = sb("msq2", [128, 1], F32)
    rstd2 = sb("rstd2", [128, 1], F32)
    ssq10 = sb("ssq10", [128, 10], F32)
    ms10 = sb("ms10", [128, 10], F32)
    rs10 = sb("rs10", [128, 10], F32)
    qn = sb("qn", [128, 512], BF16)
    kpad = sb("kpad", [128, 2, 2, 128], BF16)
    vaug = [sb(f"vaug{i}", [128, 2, 65], BF16) for i in range(2)]
    qT = sb("qT", [128, 4, 128], BF16)
    kT = [sb(f"kT{i}", [128, 4, 128], BF16) for i in range(2)]
    lgT = sb("lgT", [128, 1024], F32)
    PT = sb("PT", [128, 4, 2, 128], BF16)
    den = sb("den", [128, 4], F32)
    rden = sb("rden", [128, 4], F32)
    onb = sb("onb", [128, 512], BF16)
    phase1_end = off[0]

    for i in range(2):
        G("memset", [], [vaug[i]], vaug[i][:], 1.0)
    G("memset", [], [kpad], kpad[:], 0.0)

    def stageA(s):
        hT = hTs[s % 2]
        for j in range(NBS):
            blk = s * NBS + j
            xi = xin[blk % 2]
            load("sync", xi, xi[:], x[blk * 128:(blk + 1) * 128, :])
            A("activation", [xi], [hb[0], ssq], out=hb[0][:], in_=xi[:], func=ACTF.Square, accum_out=ssq[:, 0:1])
            V("tensor_scalar", [ssq], [msq], out=msq[:], in0=ssq[:], scalar1=1.0 / D, scalar2=EPS,
              op0=ALU.mult, op1=ALU.add)
            G("tensor_tensor", [msq, nhalf], [rstd], out=rstd[:], in0=msq[:], in1=nhalf[:], op=ALU.pow)
            V("scalar_tensor_tensor", [xi, rstd, mod], [tmpf], out=tmpf[:], in0=xi[:], scalar=rstd[:, 0:1],
              in1=A1, op0=ALU.mult, op1=ALU.mult)
            hbt = hb[0]
            V("tensor_tensor", [tmpf, mod], [hbt], out=hbt[:], in0=tmpf[:], in1=B1, op=ALU.add)
            pt = gtp()
            for kc in range(8):
                TR(pt, pt[:, kc * 128:(kc + 1) * 128], hbt, hbt[:, kc * 128:(kc + 1) * 128], ident_b)
            A("copy", [pt], [hT], out=hT[:, :, j * 128:(j + 1) * 128],
              in_=pt[:].rearrange("p (k t) -> p k t", k=8))

    def stageB(s):
        hT = hTs[s % 2]
        if s == 0:
            G("memset", [], [ubuf], ubuf[:, :, 0:30], 0.0)
        else:
            A("copy", [ubuf], [ubuf], out=ubuf[:, :, 0:30], in_=ubuf[:, :, TT:TT + 30])
        for c in range(4):
            pa = gmm_ab()
            for kc in range(8):
                MM(pa, pa[:, 0:TT], w_in_sb, w_in_sb[:, kc, c * 128:(c + 1) * 128], hT, hT[:, kc, :], kc == 0, kc == 7)
            pb = gmm_ab()
            for kc in range(8):
                MM(pb, pb[:, 0:TT], w_in_sb, w_in_sb[:, kc, 512 + c * 128:512 + (c + 1) * 128], hT, hT[:, kc, :],
                   kc == 0, kc == 7)
            sg = sgb[c % 2]
            A("activation", [pb], [sg], out=sg[:], in_=pb[:, 0:TT], func=ACTF.Sigmoid)
            V("tensor_tensor", [pa, sg], [ubuf], out=ubuf[:, c, 30:30 + TT], in0=pa[:, 0:TT], in1=sg[:], op=ALU.mult)

    def stageC(s):
        for c in range(4):
            V("tensor_scalar", [ubuf, kern_sb, cvec_sb], [acc[c]], out=acc[c][:], in0=ubuf[:, c, 0:TT],
              scalar1=kern_sb[:, c, 0:1], scalar2=cvec_sb[:, c, 0:1], op0=ALU.mult, op1=ALU.add)
        for tap in range(1, 31):
            for c in range(4):
                V("scalar_tensor_tensor", [ubuf, kern_sb, acc[c]], [acc[c]], out=acc[c][:],
                  in0=ubuf[:, c, tap:tap + TT], scalar=kern_sb[:, c, tap:tap + 1], in1=acc[c][:],
                  op0=ALU.mult, op1=ALU.add)

    def stageD(s):
        sqv = sqb
        for c in range(4):
            A("copy", [acc[c]], [cbf], out=cbf[:, c, :], in_=acc[c][:])
            A("activation", [acc[c]], [sqb], out=sqv[:, c, :], in_=acc[c][:], func=ACTF.Square)
        pmn = mmb[1]
        for c in range(4):
            MM(pmn, pmn[:, 0:TT], onesdiv, onesdiv[:], cbf, cbf[:, c, :], c == 0, c == 3)
        pq2 = mmb[1]
        for c in range(4):
            MM(pq2, pq2[:, TT:2 * TT], onesdiv, onesdiv[:], sqb, sqv[:, c, :], c == 0, c == 3)
        A("copy", [pmn], [mean_sb], out=mean_sb[:], in_=pmn[:, 0:TT])
        V("tensor_tensor", [mean_sb], [m2], out=m2[:], in0=mean_sb[:], in1=mean_sb[:], op=ALU.mult)
        V("scalar_tensor_tensor", [pq2, m2], [m2], out=var[:], in0=pq2[:, TT:2 * TT], scalar=EPS, in1=m2[:],
          op0=ALU.add, op1=ALU.subtract)
        A("sqrt", [m2], [m2], out=m2[:], in_=m2[:])
        V("reciprocal", [m2], [m2], out=m2[:], in_=m2[:])
        for c in range(4):
            V("tensor_tensor", [acc[c], mean_sb], [acc[c]], out=acc[c][:], in0=acc[c][:], in1=mean_sb[:],
              op=ALU.subtract)
        for c in range(4):
            V("tensor_tensor", [acc[c], rln], [acc[c]], out=acc[c][:], in0=acc[c][:], in1=rln[:], op=ALU.mult)
        for c in range(4):
            A("activation", [acc[c], cvec_sb], [uT], out=uT[:, c, :], in_=acc[c][:], func=ACTF.Silu,
              bias=cvec_sb[:, c, 2:3], scale=cvec_sb[:, c, 1:2])

    def stageE(s):
        hT = hTs[s % 2]
        for j in range(NBS):
            blk = s * NBS + j
            tok = slice(j * 128, (j + 1) * 128)
            for kc in range(8):
                MM(attA, attA[:, 0:512], hT, hT[:, kc, tok], w_in_sb, w_in_sb[:, kc, 1024:1536], kc == 0, kc == 7)
            for kc in range(8):
                MM(attB, attB[:, 0:256], hT, hT[:, kc, tok], w_in_sb, w_in_sb[:, kc, 1536:1792], kc == 0, kc == 7)
            A("activation", [attA], [tmpf], out=tmpf[:, 0:512], in_=attA[:, 0:512], func=ACTF.Square)
            A("activation", [attB], [tmpf], out=tmpf[:, 512:640], in_=attB[:, 0:128], func=ACTF.Square)
            V("tensor_reduce", [tmpf], [ssq10], out=ssq10[:], in_=tmpf[:, 0:640].rearrange("p (h d) -> p h d", d=64),
              axis=AX.X, op=ALU.add)
            V("tensor_scalar", [ssq10], [ms10], out=ms10[:], in0=ssq10[:], scalar1=1.0 / 64, scalar2=EPS,
              op0=ALU.mult, op1=ALU.add)
            G("tensor_tensor", [ms10, nhalf], [rs10], out=rs10[:], in0=ms10[:],
              in1=nhalf[:, 0:1].to_broadcast([128, 10]), op=ALU.pow)
            V("tensor_tensor", [attA, rs10, ssq10], [tmpf], out=tmpf[:, 0:512].rearrange("p (h d) -> p h d", d=64), in0=attA[:, 0:512].rearrange("p (h d) -> p h d", d=64),
              in1=rs10[:, 0:8, None].to_broadcast([128, 8, 64]), op=ALU.mult)
            V("tensor_tensor", [tmpf, gq8], [qn], out=qn[:].rearrange("p (h d) -> p h d", d=64), in0=tmpf[:, 0:512].rearrange("p (h d) -> p h d", d=64),
              in1=gq8[:], op=ALU.mult)
            V("tensor_tensor", [attB, rs10], [tmpf], out=tmpf[:, 512:640].rearrange("p (h d) -> p h d", d=64), in0=attB[:, 0:128].rearrange("p (h d) -> p h d", d=64),
              in1=rs10[:, 8:10, None].to_broadcast([128, 2, 64]), op=ALU.mult)
            for dd in range(2):
                V("tensor_tensor", [tmpf, gk2], [kpad], out=kpad[:, :, dd, dd * 64:(dd + 1) * 64],
                  in0=tmpf[:, 512:640].rearrange("p (h d) -> p h d", d=64), in1=gk2[:], op=ALU.mult)
            va = vaug[blk % 2]
            A("copy", [attB], [va], out=va[:, :, 0:64], in_=attB[:, 128:256].rearrange("p (h d) -> p h d", d=64))
            pt = gtp()
            for c in range(4):
                TR(pt, pt[:, c * 128:(c + 1) * 128], qn, qn[:, c * 128:(c + 1) * 128], ident_b)
            kflat = kpad[:].rearrange("p a b d -> p (a b d)")
            for kv in range(4):
                TR(pt, pt[:, (4 + kv) * 128:(5 + kv) * 128], kpad, kflat[:, kv * 128:(kv + 1) * 128], ident_b)
            kTc = kT[blk % 2]
            kTp = kT[(blk - 1) % 2]
            A("copy", [pt], [qT], out=qT[:], in_=pt[:, 0:512].rearrange("p (c t) -> p c t", c=4))
            A("copy", [pt], [kTc], out=kTc[:], in_=pt[:, 512:1024].rearrange("p (c t) -> p c t", c=4))
            js = [1] if blk == 0 else [0, 1]
            jsl = slice(js[0], 2)
            attv = [(attA, attA[:].rearrange("p (h j q) -> p h j q", h=2, j=2)),
                    (attB, attB[:].rearrange("p (h j q) -> p h j q", h=2, j=2))]
            lg4 = lgT[:].rearrange("p (h j q) -> p h j q", h=4, j=2)
            for g in range(2):
                for hh in range(4):
                    h = 4 * g + hh
                    c, r = h // 2, h % 2
                    for jj in js:
                        kTt = kTp if jj == 0 else kTc
                        at_, av_ = attv[hh // 2]
                        MM(at_, av_[:, hh % 2, jj, :], kTt, kTt[:, 2 * g + r, :], qT, qT[:, c, :], True, True)
                for hp in range(2):
                    at_, av_ = attv[hp]
                    V("tensor_tensor", [at_, bm_sb], [lgT], out=lg4[:, 2 * hp:2 * hp + 2, jsl, :], in0=av_[:, :, jsl, :],
                      in1=bm_sb[:, 4 * g + 2 * hp:4 * g + 2 * hp + 2, jsl, :], op=ALU.add)
                A("activation", [lgT, negc], [PT], out=PT[:, :, jsl, :], in_=lg4[:, :, jsl, :], func=ACTF.Exp,
                  bias=negc[:, 0:1])
                po = mmb[0]
                po3 = po[:, 0:260].rearrange("p (h e) -> p h e", e=65)
                vp = vaug[(blk - 1) % 2]
                for hh in range(4):
                    for jj in js:
                        vt = vp if jj == 0 else va
                        MM(po, po3[:, hh, :], PT, PT[:, hh, jj, :], vt, vt[:, g, :], jj == js[0], jj == js[-1])
                V("tensor_tensor", [po, esink], [den], out=den[:], in0=po3[:, :, 64], in1=esink[:, 4 * g:4 * g + 4],
                  op=ALU.add)
                V("reciprocal", [den], [rden], out=rden[:], in_=den[:])
                V("tensor_tensor", [po, rden], [onb],
                  out=onb[:].rearrange("p (h d) -> p h d", d=64)[:, 4 * g:4 * g + 4, :], in0=po3[:, :, 0:64],
                  in1=rden[:, :, None].to_broadcast([128, 4, 64]), op=ALU.mult)
            pt = gtp()
            for c in range(4):
                TR(pt, pt[:, c * 128:(c + 1) * 128], onb, onb[:, c * 128:(c + 1) * 128], ident_b)
            A("copy", [pt], [oT], out=oT[:, :, tok], in_=pt[:, 0:512].rearrange("p (c t) -> p c t", c=4))

    def stageF(s):
        hT = hTs[s % 2]
        for mc in range(8):
            ms_ = slice(mc * 128, (mc + 1) * 128)
            i2 = mc % 2
            bx, by = (mmb[2], mmb[3]) if i2 == 0 else (attA, attB)
            for kc in range(8):
                MM(bx, bx[:, 0:TT], w_in_sb, w_in_sb[:, kc, 1792 + mc * 128:1792 + (mc + 1) * 128], hT, hT[:, kc, :],
                   kc == 0, kc == 7)
            A("activation", [bx], [s1[i2]], out=s1[i2][:], in_=bx[:, 0:TT], func=ACTF.Sigmoid)
            for kc in range(8):
                MM(by, by[:, 0:TT], w_in_sb, w_in_sb[:, kc, 2816 + mc * 128:2816 + (mc + 1) * 128], hT, hT[:, kc, :],
                   kc == 0, kc == 7)
            A("activation", [by], [s2[i2]], out=s2[i2][:], in_=by[:, 0:TT], func=ACTF.Sigmoid)
            for kc in range(4):
                MM(bx, bx[:, 0:TT], wco_sb, wco_sb[:, kc, ms_], uT, uT[:, kc, :], kc == 0, kc == 3)
            V("tensor_tensor", [bx, s1[i2]], [s1[i2]], out=s1[i2][:], in0=bx[:, 0:TT], in1=s1[i2][:], op=ALU.mult)
            for kc in range(4):
                MM(by, by[:, 0:TT], wao_sb, wao_sb[:, kc, ms_], oT, oT[:, kc, :], kc == 0, kc == 3)
            V("tensor_tensor", [by, s2[i2]], [s2[i2]], out=s2[i2][:], in0=by[:, 0:TT], in1=s2[i2][:], op=ALU.mult)
            V("tensor_tensor", [s1[i2], s2[i2]], [mT], out=mT[:, mc, :], in0=s1[i2][:], in1=s2[i2][:], op=ALU.add)

    def stageG(s):
        for j in range(NBS):
            blk = s * NBS + j
            tok = slice(j * 128, (j + 1) * 128)
            rows = slice(blk * 128, (blk + 1) * 128)
            xrt = xr[blk % 2]
            load("sync", xrt, xrt[:], x[rows, :])
            for nh in range(2):
                cs = slice(nh * 512, (nh + 1) * 512)
                pp = gmm_g()
                for kc in range(8):
                    MM(pp, pp[:], mT, mT[:, kc, tok], wout_sb, wout_sb[:, kc, cs], kc == 0, kc == 7)
                V("tensor_tensor", [pp, xrt], [xrt], out=xrt[:, cs], in0=pp[:], in1=xrt[:, cs], op=ALU.add)
            store("gpsimd", xrt, out[rows, :], xrt[:])
            A("activation", [xrt], [h2b[0], ssq2], out=h2b[0][:], in_=xrt[:], func=ACTF.Square, accum_out=ssq2[:, 0:1])
            V("tensor_scalar", [ssq2], [msq2], out=msq2[:], in0=ssq2[:], scalar1=1.0 / D, scalar2=EPS,
              op0=ALU.mult, op1=ALU.add)
            G("tensor_tensor", [msq2, nhalf], [rstd2], out=rstd2[:], in0=msq2[:], in1=nhalf[:], op=ALU.pow)
            h2f = xrt
            V("scalar_tensor_tensor", [xrt, rstd2, mod], [xrt], out=xrt[:], in0=xrt[:], scalar=rstd2[:, 0:1],
              in1=A2, op0=ALU.mult, op1=ALU.mult)
            V("tensor_tensor", [xrt, mod], [xrt], out=xrt[:], in0=xrt[:], in1=B2, op=ALU.add)
            hbt = h2b[0]
            A("copy", [h2f], [hbt], out=hbt[:], in_=h2f[:])
            store("gpsimd", hbt, H2[rows, :], hbt[:])
            h2T3 = h2Tt[:].rearrange("p (k t) -> p k t", k=8)
            for hf in range(2):
                pp = gmm_g()
                for i in range(4):
                    kc = hf * 4 + i
                    TR(pp, pp[:, i * 128:(i + 1) * 128], h2f, h2f[:, kc * 128:(kc + 1) * 128], ident_f)
                if hf == 0:
                    A("copy", [pp], [h2Tt], out=h2T3[:, 0:4, :], in_=pp[:].rearrange("p (k t) -> p k t", k=4))
                else:
                    V("tensor_copy", [pp], [h2Tt], out=h2T3[:, 4:8, :], in_=pp[:].rearrange("p (k t) -> p k t", k=4))
            pp = gmm_g()
            for kc in range(8):
                MM(pp, pp[:, 0:36], h2Tt, h2T3[:, kc, :], wr_sb, wr_sb[:, kc, :], kc == 0, kc == 7)
            lr = Lrow[blk % 2]
            V("tensor_tensor", [pp, brb], [lr], out=lr[:], in0=pp[:, 0:36], in1=brb[:], op=ALU.add)
            store("gpsimd", lr, Ldram[:, blk, :], lr[:])
        if not phase1_only:
            for e_ in range(s * NE // NS, (s + 1) * NE // NS):
                expert_casts(e_)

    def run_stage(fn, s):
        P.defer = []
        fn(s)
        lst = P.defer
        P.defer = None
        return lst

    def merge(la, lb):
        res = []
        ia = ib = 0
        na, nb = len(la), len(lb)
        while ia < na or ib < nb:
            if ib >= nb or (ia < na and ia * nb <= ib * na):
                res.append(la[ia]); ia += 1
            else:
                res.append(lb[ib]); ib += 1
        return res

    def play(lst):
        for a in lst:
            P.op(*a)

    if NS_run:
        play(run_stage(stageA, 0) + run_stage(stageB, 0))
    for s in range(NS_run):
        eg = run_stage(stageE, s)
        if s > 0:
            eg = merge(eg, run_stage(stageG, s - 1))
        play(merge(run_stage(stageC, s) + run_stage(stageD, s), eg))
        df = run_stage(stageF, s)
        if s + 1 < NS_run:
            df = merge(df, run_stage(stageA, s + 1) + run_stage(stageB, s + 1))
        play(df)
    if NS_run:
        play(run_stage(stageG, NS_run - 1))

    P.barrier()
    if debug:
        P.op("sync", lambda e: e.dma_start(out=Ldbg[:, :, :], in_=Ldram[:, :, :]), [], [], grp=P.grp("dbgL"))


    if not phase1_only:
        off[0] = persist_end
        KMAX = -(-T // BLK)
        w1 = sb("w1", [128, NB], F32)
        w2 = sb("w2", [128, NB], F32)
        d1i = sb("d1i", [128, NB], I32)
        d2i = sb("d2i", [128, NB], I32)
        blke_i = sb("blke_i", [128, NBLK], I32)
        Lt = sb("Lt", [128, NB, 36], F32)
        load("sync", Lt, Lt[:], Ldram[:, :, :])
        widx = sb("widx", [128, NBLK], I32)
        route_keep = off[0]
        ones_f = sb("ones_f", [128, 128], F32)
        ustr = sb("ustr", [128, 128], F32)
        gmax = sb("gmax", [128, NB], F32)
        ohg = sb("ohg", [128, NB, 4], F32)
        eg = sb("eg", [128, NB, 4], F32)
        sume = sb("sume", [128, NB], F32)
        ptop = sb("ptop", [128, NB], F32)
        tmp8 = sb("tmp8", [128, NB, 8], F32)
        elsel = sb("elsel", [128, NB, 8], F32)
        els2 = sb("els2", [128, NB, 8], F32)
        oh1 = sb("oh1", [128, NB, 8], F32)
        oh2 = sb("oh2", [128, NB, 8], F32)
        m1 = sb("m1", [128, NB], F32)
        m2v = sb("m2v", [128, NB], F32)
        ddv = sb("ddv", [128, NB], F32)
        e2v = sb("e2v", [128, NB], F32)
        OH1 = sb("OH1", [128, NB, 32], F32)
        OH2 = sb("OH2", [128, NB, 32], F32)
        TH = sb("TH", [128, NB, 32], F32)
        scn = [sb(f"scn{i}", [128, NB, 32], F32) for i in range(2)]
        cnt_p = sb("cnt_p", [128, 32], F32)
        tot_sb = sb("tot_sb", [128, 32], F32)
        pp_sb = sb("pp_sb", [128, 32], F32)
        thr_i = sb("thr_i", [128, KMAX], I32)
        thr_f = sb("thr_f", [128, KMAX], F32)
        cmpt = sb("cmpt", [128, 32, KMAX], F32)
        pc = sb("pc", [128, 32], F32)
        pe_ = [sb(f"pend{i}", [128, 32], F32) for i in range(2)]
        base = sb("base", [128, 32], F32)
        dst_f = sb("dst_f", [128, NB], F32)
        bthr_i = sb("bthr_i", [128, NBLK], I32)
        bthr_f = sb("bthr_f", [128, NBLK], F32)
        cmpb = sb("cmpb", [128, NBLK, 32], F32)
        blke_f = sb("blke_f", [128, NBLK], F32)
        hs = [sb(f"hs{i}", [128, D], BF16) for i in range(3)]
        pidx_i = sb("pidx_i", [128, 1], I32)
        pidx_f = sb("pidx_f", [128, 1], F32)
        widx_f = sb("widx_f", [128, NBLK], F32)

        G("memset", [], [ones_f], ones_f[:], 1.0)
        G("memset", [], [ustr], ustr[:], 1.0)
        G("affine_select", [ustr], [ustr], out=ustr[:], in_=ustr[:], pattern=[[1, 128]],
          compare_op=ALU.is_gt, fill=0.0, base=0, channel_multiplier=-1)
        G("iota", [], [thr_i], thr_i[:], pattern=[[BLK, KMAX]], base=0, channel_multiplier=0)
        G("iota", [], [bthr_i], bthr_i[:], pattern=[[BLK, NBLK]], base=0, channel_multiplier=0)
        V("tensor_copy", [thr_i], [thr_f], out=thr_f[:], in_=thr_i[:])
        V("tensor_copy", [bthr_i], [bthr_f], out=bthr_f[:], in_=bthr_i[:])

        gl = Lt[:, :, 0:4]
        el4 = Lt[:, :, 4:36].rearrange("p t (g e) -> p t g e", g=4)
        bc4 = lambda ap: ap[:, :, None].to_broadcast([128, NB, 4])
        bc8 = lambda ap: ap[:, :, None].to_broadcast([128, NB, 8])
        V("tensor_reduce", [Lt], [gmax], out=gmax[:], in_=gl, axis=AX.X, op=ALU.max)
        V("tensor_tensor", [Lt, gmax], [ohg], out=ohg[:], in0=gl, in1=bc4(gmax), op=ALU.is_equal)
        V("tensor_tensor", [Lt, gmax], [eg], out=eg[:], in0=gl, in1=bc4(gmax), op=ALU.subtract)
        A("activation", [eg], [eg], out=eg[:], in_=eg[:], func=ACTF.Exp)
        V("tensor_reduce", [eg], [sume], out=sume[:], in_=eg[:], axis=AX.X, op=ALU.add)
        V("reciprocal", [sume], [ptop], out=ptop[:], in_=sume[:])
        V("tensor_tensor", [Lt, ohg], [elsel], out=elsel[:], in0=el4[:, :, 0, :],
          in1=ohg[:, :, 0:1].to_broadcast([128, NB, 8]), op=ALU.mult)
        for g in range(1, 4):
            V("tensor_tensor", [Lt, ohg], [tmp8], out=tmp8[:], in0=el4[:, :, g, :],
              in1=ohg[:, :, g:g + 1].to_broadcast([128, NB, 8]), op=ALU.mult)
            V("tensor_tensor", [elsel, tmp8], [elsel], out=elsel[:], in0=elsel[:], in1=tmp8[:], op=ALU.add)
        V("tensor_reduce", [elsel], [m1], out=m1[:], in_=elsel[:], axis=AX.X, op=ALU.max)
        V("tensor_tensor", [elsel, m1], [oh1], out=oh1[:], in0=elsel[:], in1=bc8(m1), op=ALU.is_equal)
        V("scalar_tensor_tensor", [oh1, elsel], [els2], out=els2[:], in0=oh1[:], scalar=-1e30, in1=elsel[:],
          op0=ALU.mult, op1=ALU.add)
        V("tensor_reduce", [els2], [m2v], out=m2v[:], in_=els2[:], axis=AX.X, op=ALU.max)
        V("tensor_tensor", [els2, m2v], [oh2], out=oh2[:], in0=els2[:], in1=bc8(m2v), op=ALU.is_equal)
        V("tensor_tensor", [m2v, m1], [ddv], out=ddv[:], in0=m2v[:], in1=m1[:], op=ALU.subtract)
        A("activation", [ddv], [e2v], out=e2v[:], in_=ddv[:], func=ACTF.Exp)
        V("tensor_scalar", [e2v], [ddv], out=ddv[:], in0=e2v[:], scalar1=1.0, scalar2=None, op0=ALU.add)
        V("reciprocal", [ddv], [sume], out=sume[:], in_=ddv[:])
        V("tensor_tensor", [sume, ptop], [w1], out=w1[:], in0=sume[:], in1=ptop[:], op=ALU.mult)
        V("tensor_tensor", [w1, e2v], [w2], out=w2[:], in0=w1[:], in1=e2v[:], op=ALU.mult)
        for OHk, ohk in ((OH1, oh1), (OH2, oh2)):
            V("tensor_tensor", [ohg, ohk], [OHk], out=OHk[:].rearrange("p t (g e) -> p t g e", g=4),
              in0=ohg[:, :, :, None].to_broadcast([128, NB, 4, 8]),
              in1=ohk[:, :, None, :].to_broadcast([128, NB, 4, 8]), op=ALU.mult)
        V("tensor_tensor", [OH1, OH2], [TH], out=TH[:], in0=OH1[:], in1=OH2[:], op=ALU.add)
        V("tensor_reduce", [TH], [cnt_p], out=cnt_p[:], in_=TH[:].rearrange("p t e -> p e t"), axis=AX.X, op=ALU.add)
        pA = gmm()
        MM(pA, pA[:, 0:32], ustr, ustr[:], cnt_p, cnt_p[:], True, True)
        pB = gmm()
        MM(pB, pB[:, 0:32], ones_f, ones_f[:], cnt_p, cnt_p[:], True, True)
        A("copy", [pA], [pp_sb], out=pp_sb[:], in_=pA[:, 0:32])
        A("copy", [pB], [tot_sb], out=tot_sb[:], in_=pB[:, 0:32])
        V("tensor_tensor", [tot_sb, thr_f], [cmpt], out=cmpt[:], in0=tot_sb[:, :, None].to_broadcast([128, 32, KMAX]),
          in1=thr_f[:, None, :].to_broadcast([128, 32, KMAX]), op=ALU.is_gt)
        V("tensor_reduce", [cmpt], [pc], out=pc[:], in_=cmpt[:], axis=AX.X, op=ALU.add)
        V("tensor_scalar", [pc], [pc], out=pc[:], in0=pc[:], scalar1=float(BLK), scalar2=None, op0=ALU.mult)
        src = pc
        st = 1
        i = 0
        while st < 32:
            dstt = pe_[i % 2]
            V("tensor_tensor", [src], [dstt], out=dstt[:, st:], in0=src[:, st:], in1=src[:, 0:32 - st], op=ALU.add)
            V("tensor_copy", [src], [dstt], out=dstt[:, 0:st], in_=src[:, 0:st])
            src = dstt
            st *= 2
            i += 1
        pend = src
        V("tensor_tensor", [pend, pc], [base], out=base[:], in0=pend[:], in1=pc[:], op=ALU.subtract)
        V("tensor_tensor", [base, pp_sb], [base], out=base[:], in0=base[:], in1=pp_sb[:], op=ALU.add)
        src = TH
        st = 1
        i = 0
        while st < NB:
            dstt = scn[i % 2]
            V("tensor_tensor", [src], [dstt], out=dstt[:, st:, :], in0=src[:, st:, :], in1=src[:, 0:NB - st, :], op=ALU.add)
            G("tensor_copy", [src], [dstt], out=dstt[:, 0:st, :], in_=src[:, 0:st, :])
            src = dstt
            st *= 2
            i += 1
        pos = scn[i % 2] if src is not scn[i % 2] else scn[(i + 1) % 2]
        if src is TH:
            pos = scn[0]
        V("tensor_tensor", [src, TH], [pos], out=pos[:], in0=src[:], in1=TH[:], op=ALU.subtract)
        V("tensor_tensor", [pos, base], [pos], out=pos[:], in0=pos[:], in1=base[:, None, :].to_broadcast([128, NB, 32]),
          op=ALU.add)
        for OHk, dki in ((OH1, d1i), (OH2, d2i)):
            V("tensor_tensor", [OHk, pos], [OHk], out=OHk[:], in0=OHk[:], in1=pos[:], op=ALU.mult)
            V("tensor_reduce", [OHk], [dst_f], out=dst_f[:], in_=OHk[:], axis=AX.X, op=ALU.add)
            V("tensor_copy", [dst_f], [dki], out=dki[:], in_=dst_f[:])
        V("tensor_tensor", [pend, bthr_f], [cmpb], out=cmpb[:], in0=pend[:, None, :].to_broadcast([128, NBLK, 32]),
          in1=bthr_f[:, :, None].to_broadcast([128, NBLK, 32]), op=ALU.is_le)
        V("tensor_reduce", [cmpb], [blke_f], out=blke_f[:], in_=cmpb[:], axis=AX.X, op=ALU.add)
        V("tensor_scalar", [blke_f], [blke_f], out=blke_f[:], in0=blke_f[:], scalar1=float(NE - 1), scalar2=None,
          op0=ALU.min)
        V("tensor_copy", [blke_f], [blke_i], out=blke_i[:], in_=blke_f[:])
        G("iota", [], [pidx_i], pidx_i[:], pattern=[[0, 1]], base=0, channel_multiplier=1)
        V("tensor_copy", [pidx_i], [pidx_f], out=pidx_f[:], in_=pidx_i[:])
        V("tensor_scalar", [blke_f, pidx_f], [widx_f], out=widx_f[:], in0=blke_f[:], scalar1=128.0,
          scalar2=pidx_f[:, 0:1], op0=ALU.mult, op1=ALU.add)
        V("tensor_copy", [widx_f], [widx], out=widx[:], in_=widx_f[:])
        if debug:
            dbg = sb("dbg", [128, 4, NB], F32)
            V("tensor_copy", [w1], [dbg], out=dbg[:, 0, :], in_=w1[:])
            V("tensor_copy", [w2], [dbg], out=dbg[:, 1, :], in_=w2[:])
            V("tensor_copy", [d1i], [dbg], out=dbg[:, 2, :], in_=d1i[:])
            V("tensor_copy", [d2i], [dbg], out=dbg[:, 3, :], in_=d2i[:])
            store("sync", dbg, Ddbg[:, :, :], dbg[:])

        for t in range(0 if stop == "R" else NB):
            hst = hs[t % 3]
            load("sync", hst, hst[:], H2[t * 128:(t + 1) * 128, :])
            if hst.sg is None:
                hst.sg = P.grp("s_" + hst.name)
            for dki in (d1i, d2i):
                P.op("gpsimd", lambda e, hst=hst, dki=dki, t=t: e.indirect_dma_start(
                    out=XS[:, :], out_offset=bass.IndirectOffsetOnAxis(ap=dki[:, t:t + 1], axis=0),
                    in_=hst[:], in_offset=None), [hst, dki], [], grp=hst.sg)
        P.barrier()

        off[0] = route_keep
        Wgu = [sb(f"Wgu{i}", [128, 8, 2 * DE], BF16) for i in range(2)]
        Wd = [sb(f"Wd{i}", [128, 2, D], BF16) for i in range(2)]
        xs_in = [sb(f"xs_in{i}", [128, D], BF16) for i in range(3)]
        XTs = [sb(f"XT{i}", [128, 8, 128], BF16) for i in range(2)]
        sgls = [sb(f"sgl{i}", [128, DE], F32) for i in range(2)]
        hids = [sb(f"hid{i}", [128, DE], BF16) for i in range(2)]
        hidTs = [sb(f"hidT{i}", [128, 2, 128], BF16) for i in range(2)]
        ysb = [sb(f"ysb{i}", [128, D], F32) for i in range(3)]
        wgu_v = WGU.rearrange("r k f -> r (k f)")
        wds_v = WDS.rearrange("r k f -> r (k f)")

        def dyn_load(tl, src_v, b):
            if tl.lg is None:
                tl.lg = P.grp("l_" + tl.name)
            return P.op("gpsimd", lambda e: e.indirect_dma_start(
                out=tl[:].rearrange("p k f -> p (k f)"), out_offset=None, in_=src_v[:, :],
                in_offset=bass.IndirectOffsetOnAxis(ap=widx[:, b:b + 1], axis=0)), [widx], [tl], grp=tl.lg)

        def gathers(b):
            dyn_load(Wgu[b % 2], wgu_v, b)
            dyn_load(Wd[b % 2], wds_v, b)

        NBLK_run = 0 if stop in ("R", "S") else NBLK
        if NBLK_run:
            gathers(0)
        att_bf = [attA[:].bitcast(BF16), attB[:].bitcast(BF16)]
        att_tl = [attA, attB]

        def front(sl):
            b = sl // SUB
            i = b % 2
            k = sl % 2
            rows = slice(sl * 128, (sl + 1) * 128)
            xst = xs_in[sl % 3]
            XT, sgl, hid = XTs[k], sgls[k], hids[k]
            pt = tp[k]
            for kc in range(8):
                TR(pt, pt[:, kc * 128:(kc + 1) * 128], xst, xst[:].rearrange("p (f k) -> p k f", k=8)[:, kc, :], ident_b)
            A("copy", [pt], [XT], out=XT[:], in_=pt[:].rearrange("p (k t) -> p k t", k=8))
            pm = mmb[k]
            for kc in range(8):
                MM(pm, pm[:], XT, XT[:, kc, :], Wgu[i], Wgu[i][:, kc, :], kc == 0, kc == 7)
            A("activation", [pm], [sgl], out=sgl[:], in_=pm[:, 0:DE], func=ACTF.Silu)
            V("tensor_tensor", [pm, sgl], [hid], out=hid[:], in0=pm[:, DE:2 * DE], in1=sgl[:], op=ALU.mult)

        def back(sl):
            b = sl // SUB
            i = b % 2
            k = sl % 2
            rows = slice(sl * 128, (sl + 1) * 128)
            hid, hidT = hids[k], hidTs[k]
            pt2t, pt2 = att_tl[k], att_bf[k]
            for kc in range(2):
                TR(pt2t, pt2[:, kc * 128:(kc + 1) * 128], hid, hid[:].rearrange("p (f k) -> p k f", k=2)[:, kc, :], ident_b)
            A("copy", [pt2t], [hidT], out=hidT[:], in_=pt2[:, 0:256].rearrange("p (k t) -> p k t", k=2))
            yst = ysb[sl % 3]
            for nh in range(2):
                po = mmb[2 + nh]
                for kc in range(2):
                    MM(po, po[:], hidT, hidT[:, kc, :], Wd[i], Wd[i][:, kc, nh * 512:(nh + 1) * 512], kc == 0, kc == 1)
                V("tensor_tensor", [po, mod], [yst], out=yst[:, nh * 512:(nh + 1) * 512], in0=po[:],
                  in1=G2[:, nh * 512:(nh + 1) * 512], op=ALU.mult)
            store("sync", yst, YS[rows, :], yst[:])

        NSL = NBLK_run * SUB

        def xload(sl):
            xst = xs_in[sl % 3]
            load("sync", xst, xst[:], XS[sl * 128:(sl + 1) * 128, :])

        if NSL:
            xload(0)
            if NSL > 1:
                xload(1)
            play(run_stage(front, 0))
        for sl in range(NSL):
            if sl + 2 < NSL:
                xload(sl + 2)
            if sl % SUB == 0 and sl // SUB + 1 < NBLK_run:
                gathers(sl // SUB + 1)
            if sl + 1 < NSL:
                play(run_stage(front, sl + 1))
            play(run_stage(back, sl))
        P.barrier()

        y1r = [sb(f"y1r{i}", [128, D], F32) for i in range(2)]
        y2r = [sb(f"y2r{i}", [128, D], F32) for i in range(2)]
        xo = [sb(f"xo{i}", [128, D], F32) for i in range(2)]
        for t in range(0 if stop in ("R", "S", "X") else NB):
            rows = slice(t * 128, (t + 1) * 128)
            y1, y2, xot = y1r[t % 2], y2r[t % 2], xo[t % 2]
            for yt, dki in ((y1, d1i), (y2, d2i)):
                if yt.lg is None:
                    yt.lg = P.grp("l_" + yt.name)
                P.op("gpsimd", lambda e, yt=yt, dki=dki, t=t: e.indirect_dma_start(
                    out=yt[:], out_offset=None, in_=YS[:, :],
                    in_offset=bass.IndirectOffsetOnAxis(ap=dki[:, t:t + 1], axis=0)), [dki], [yt], grp=yt.lg)
            load("sync", xot, xot[:], out[rows, :])
            V("scalar_tensor_tensor", [y1, w1, xot], [xot], out=xot[:], in0=y1[:], scalar=w1[:, t:t + 1], in1=xot[:],
              op0=ALU.mult, op1=ALU.add)
            V("scalar_tensor_tensor", [y2, w2, xot], [xot], out=xot[:], in0=y2[:], scalar=w2[:, t:t + 1], in1=xot[:],
              op0=ALU.mult, op1=ALU.add)
            store("scalar", xot, out[rows, :], xot[:])

    P.barrier()
    P.op("sync", lambda e: e.nop())
    P.finalize()
    sems = {e: nc.alloc_semaphore("sem_" + e) for e in ENGS}
    for g in P.grps:
        g.sem = nc.alloc_semaphore("g_" + g.name)
    with nc.Block() as block:
        @block.tensor
        def _(e):
            P.emit_engine("tensor", e, sems)

        @block.vector
        def _(e):
            P.emit_engine("vector", e, sems)

        @block.scalar
        def _(e):
            P.emit_engine("scalar", e, sems)

        @block.gpsimd
        def _(e):
            P.emit_engine("gpsimd", e, sems)

        @block.sync
        def _(e):
            P.emit_engine("sync", e, sems)
    return nc


def _t5_bucket_table():
    W = 128
    qi = np.arange(W)[:, None]
    kj = np.arange(2 * W)[None, :]
    dist = qi + W - kj
    in_window = (dist >= 0) & (dist < W)
    dc = np.clip(dist, 0, 128)
    max_exact = 16
    d = np.maximum(dc, 1).astype(np.float32)
    large = max_exact + (np.log(d / max_exact) / math.log(128 / max_exact) * (32 - max_exact)).astype(np.int32)
    large = np.minimum(large, 31)
    bucket = np.where(dc < max_exact, dc, large)
    return bucket, in_window


def prep_shared(inp):
    f = lambda a: np.ascontiguousarray(np.asarray(a, dtype=np.float32))
    bucket, in_window = _t5_bucket_table()
    tab = f(inp["rel_bias_table"])
    bias = tab[bucket]
    bias = np.where(in_window[:, :, None], bias, np.float32(-1e30))
    bmh = bias.reshape(128, 2, 128, 8).transpose(2, 3, 1, 0)
    sh = {
        "w_ada": f(inp["w_ada"][0]),
        "b_ada": f(inp["b_ada"][0]).reshape(1, -1),
        "gmix": f(inp["norm_mix_g"][0]).reshape(1, -1),
        "gffn": f(inp["norm_ffn_g"][0]).reshape(1, -1),
        "w_in": f(inp["w_in"][0]),
        "kern": f(np.asarray(inp["dw_kernel"][0]).reshape(31, 4, 128).transpose(2, 1, 0)),
        "cvec": f(np.stack([np.asarray(inp["dw_bias"][0]).reshape(4, 128).T,
                            np.asarray(inp["conv_ln_g"][0]).reshape(4, 128).T,
                            np.asarray(inp["conv_ln_b"][0]).reshape(4, 128).T], axis=2)),
        "wco": f(inp["w_conv_out"][0]),
        "wao": f(inp["w_attn_out"][0]),
        "wout": f(inp["w_out"][0]),
        "gq": f(inp["q_norm_g"][0]).reshape(1, -1),
        "gk": f(inp["k_norm_g"][0]).reshape(1, -1),
        "sinks": f(inp["sinks"][0]).reshape(1, -1),
        "bm": f(bmh),
        "wr": f(np.concatenate([np.asarray(inp["w_router_group"][0]), np.asarray(inp["w_router_expert"][0])], axis=1)),
        "br": f(np.concatenate([np.asarray(inp["b_router_group"][0]), np.asarray(inp["b_router_expert"][0])])).reshape(1, -1),
        "weg": f(inp["w_exp_gate"][0]),
        "weu": f(inp["w_exp_up"][0]),
        "wed": f(inp["w_exp_down"][0]),
    }
    return sh


def kernel(**inputs):
    x = np.asarray(inputs["x"], dtype=np.float32)
    c = np.asarray(inputs["c"], dtype=np.float32)
    Bn, T, _ = x.shape
    sh = prep_shared(inputs)
    nc = build(T)
    in_maps = []
    for b in range(Bn):
        m = dict(sh)
        m["x"] = np.ascontiguousarray(x[b])
        m["c_col"] = np.ascontiguousarray(c[b].reshape(8, 128).T)
        in_maps.append(m)
    res = run_bass_kernel_spmd(nc, in_maps, core_ids=list(range(Bn)))
    return np.stack([np.asarray(r["out"]) for r in res.results], axis=0).astype(np.float32)
```

```python
import math
import numpy as np
import concourse.bass as bass
import concourse.mybir as mybir
from concourse.bass_utils import run_bass_kernel_spmd

F32 = mybir.dt.float32
BF16 = mybir.dt.bfloat16
I32 = mybir.dt.int32
ALU = mybir.AluOpType
ACTF = mybir.ActivationFunctionType
AX = mybir.AxisListType

ENGS = ["tensor", "vector", "scalar", "gpsimd", "sync"]

D = 1024
DIN = 3840
TT = 256
NBS = TT // 128
EPS = 1e-6
NE = 32
DE = 256
SUB = 2
BLK = 128 * SUB


class Tl:
    def __init__(self, name, t):
        self.name = name
        self.t = t
        self.w = []
        self.r = []
        self.lg = None
        self.sg = None

    def __getitem__(self, k):
        return self.t[k]


class Grp:
    def __init__(self, name):
        self.name = name
        self.n = 0
        self.sem = None


class Op:
    __slots__ = ("eng", "fn", "deps", "grp", "signal", "count", "waits")

    def __init__(self, eng, fn, deps, grp=None):
        self.eng = eng
        self.fn = fn
        self.deps = deps
        self.grp = grp
        self.signal = False
        self.count = None
        self.waits = None


class Prog:
    def __init__(self, nc):
        self.nc = nc
        self.ops = {e: [] for e in ENGS}
        self.grps = []
        self.extra = {e: [] for e in ENGS}
        self.defer = None

    def grp(self, name):
        g = Grp(name)
        self.grps.append(g)
        return g

    def op(self, eng, fn, reads=(), writes=(), grp=None):
        if self.defer is not None:
            self.defer.append((eng, fn, list(reads), list(writes), grp))
            return None
        deps = []
        for t in reads:
            deps.extend(t.w)
        for t in writes:
            deps.extend(t.w)
            deps.extend(t.r)
        deps.extend(self.extra[eng])
        self.extra[eng] = []
        o = Op(eng, fn, deps, grp)
        self.ops[eng].append(o)
        idx = len(self.ops[eng]) - 1
        if grp is not None:
            grp.n += 1
            tok = ("d", grp, grp.n)
        else:
            tok = ("c", eng, idx)
        wset = set(id(t) for t in writes)
        for t in reads:
            if id(t) in wset:
                continue
            t.r = [x for x in t.r if not (x[0] == tok[0] and x[1] is tok[1])] + [tok]
        for t in writes:
            t.w = [tok]
            t.r = []
        return tok

    def barrier(self, exclude=()):
        toks = []
        for e in ENGS:
            if self.ops[e]:
                toks.append(("c", e, len(self.ops[e]) - 1))
        for g in self.grps:
            if g.n and not any(g is x for x in exclude):
                toks.append(("d", g, g.n))
        for e in ENGS:
            self.extra[e].extend(toks)

    def finalize(self):
        for e in ENGS:
            known_c = {}
            known_d = {}
            for i, o in enumerate(self.ops[e]):
                waits = []
                for d in o.deps:
                    if d[0] == "c":
                        _, e2, j = d
                        if e2 == e and (e == "tensor" or j < i - 2):
                            continue
                        if known_c.get(e2, -1) >= j:
                            continue
                        known_c[e2] = j
                        waits.append(d)
                    else:
                        _, g, n = d
                        if known_d.get(id(g), 0) >= n:
                            continue
                        known_d[id(g)] = n
                        waits.append(d)
                o.waits = waits
        for e in ENGS:
            for o in self.ops[e]:
                for d in o.waits:
                    if d[0] == "c":
                        self.ops[d[1]][d[2]].signal = True
        for e in ENGS:
            c = 0
            for o in self.ops[e]:
                if o.signal and o.grp is None:
                    c += 1
                    o.count = c

    def emit_engine(self, e, eng, sems):
        for o in self.ops[e]:
            for d in o.waits:
                if d[0] == "c":
                    tgt = self.ops[d[1]][d[2]]
                    if tgt.count is None:
                        continue
                    eng.wait_ge(sems[d[1]], tgt.count)
                else:
                    eng.wait_ge(d[1].sem, 16 * d[2])
            ins = o.fn(eng)
            if o.grp is not None:
                ins.then_inc(o.grp.sem, 16)
            elif o.signal:
                ins.then_inc(sems[e], 1)


def _dsize(dt):
    return 2 if dt == BF16 else 4


def build(T, phase1_only=False, debug=False, stop=None):
    NB = T // 128
    NS = T // TT
    NBLK = -(-2 * T // BLK) + NE
    NSLOT = NBLK * BLK
    nc = bass.Bass("TRN2", target_bir_lowering=False)
    P = Prog(nc)

    def din(name, shape, dt=F32):
        return nc.dram_tensor(name, shape, dt, kind="ExternalInput").ap()

    x = din("x", [T, D])
    c_col = din("c_col", [128, 8])
    w_ada = din("w_ada", [D, 6 * D])
    b_ada = din("b_ada", [1, 6 * D])
    gmix = din("gmix", [1, D])
    gffn = din("gffn", [1, D])
    w_in = din("w_in", [D, DIN])
    kern = din("kern", [128, 4, 31])
    cvec = din("cvec", [128, 4, 3])
    wco = din("wco", [512, D])
    wao = din("wao", [512, D])
    wout = din("wout", [D, D])
    gq = din("gq", [1, 64])
    gk = din("gk", [1, 64])
    sinks = din("sinks", [1, 8])
    bm = din("bm", [128, 8, 2, 128])
    wr = din("wr", [D, 36])
    br = din("br", [1, 36])
    weg = din("weg", [NE, D, DE])
    weu = din("weu", [NE, D, DE])
    wed = din("wed", [NE, DE, D])
    out = nc.dram_tensor("out", [T, D], F32, kind="ExternalOutput").ap()
    H2 = nc.dram_tensor("h2_scr", [T, D], BF16, kind="Internal").ap()
    Ldram = nc.dram_tensor("l_scr", [128, NB, 36], F32, kind="Internal").ap()
    WGU = nc.dram_tensor("wgu_scr", [NE * 128, 8, 2 * DE], BF16, kind="Internal").ap()
    WDS = nc.dram_tensor("wd_scr", [NE * 128, 2, D], BF16, kind="Internal").ap()
    XS = nc.dram_tensor("xs_scr", [NSLOT, D], BF16, kind="Internal").ap()
    YS = nc.dram_tensor("ys_scr", [NSLOT, D], F32, kind="Internal").ap()
    if debug:
        Ldbg = nc.dram_tensor("Ldbg", [128, NB, 36], F32, kind="ExternalOutput").ap()
        Ddbg = nc.dram_tensor("Ddbg", [128, 4, NB], F32, kind="ExternalOutput").ap()

    off = [16640]
    LIMIT = 229376 - 512

    def sb(name, shape, dt):
        nbytes = int(np.prod(shape[1:])) * _dsize(dt)
        nbytes = (nbytes + 63) // 64 * 64
        t = nc.alloc_sbuf_tensor_at(name, list(shape), dt, offset=off[0])
        off[0] += nbytes
        assert off[0] <= LIMIT, f"SBUF overflow at {name}: {off[0]}"
        return Tl(name, t)

    def ps(name, shape, dt):
        return Tl(name, nc.alloc_psum_tensor(name, list(shape), dt))

    tp = [ps(f"tp{i}", [128, 1024], BF16) for i in range(2)]
    mmb = [ps(f"mm{i}", [128, 512], F32) for i in range(4)]
    attA = ps("attA", [128, 512], F32)
    attB = ps("attB", [128, 512], F32)
    tpi = [0]
    mmi = [0]

    def gtp():
        tpi[0] += 1
        return tp[tpi[0] % 2]

    def gmm():
        mmi[0] += 1
        return mmb[mmi[0] % 4]

    def gmm_ab():
        mmi[0] += 1
        return mmb[mmi[0] % 2]

    def gmm_g():
        mmi[0] += 1
        return mmb[2 + mmi[0] % 2]

    def E(eng, fn, reads, writes, *a, **k):
        return P.op(eng, lambda e: getattr(e, fn)(*a, **k), reads, writes)

    def V(fn, reads, writes, *a, **k):
        return E("vector", fn, reads, writes, *a, **k)

    def A(fn, reads, writes, *a, **k):
        return E("scalar", fn, reads, writes, *a, **k)

    def G(fn, reads, writes, *a, **k):
        return E("gpsimd", fn, reads, writes, *a, **k)

    def MM(o_tl, o_ap, l_tl, l_ap, r_tl, r_ap, start, stop):
        return P.op("tensor", lambda e: e.matmul(o_ap, lhsT=l_ap, rhs=r_ap, start=start, stop=stop),
                    [l_tl, r_tl], [o_tl])

    def TR(o_tl, o_ap, i_tl, i_ap, id_tl):
        return P.op("tensor", lambda e: e.transpose(out=o_ap, in_=i_ap, identity=id_tl[:]),
                    [i_tl, id_tl], [o_tl])

    def load(q, tl, o_ap, i_ap):
        if tl.lg is None:
            tl.lg = P.grp("l_" + tl.name)
        return P.op(q, lambda e: e.dma_start(out=o_ap, in_=i_ap), [], [tl], grp=tl.lg)

    def store(q, tl, o_ap, i_ap):
        if tl.sg is None:
            tl.sg = P.grp("s_" + tl.name)
        return P.op(q, lambda e: e.dma_start(out=o_ap, in_=i_ap), [tl], [], grp=tl.sg)

    mod = sb("mod", [128, 6 * D], F32)
    B1, A1, G1 = mod[:, 0:D], mod[:, D:2 * D], mod[:, 2 * D:3 * D]
    B2, A2, G2 = mod[:, 3 * D:4 * D], mod[:, 4 * D:5 * D], mod[:, 5 * D:6 * D]
    ident_f = sb("ident_f", [128, 128], F32)
    ident_b = sb("ident_b", [128, 128], BF16)
    nhalf = sb("nhalf", [128, 1], F32)
    persist_end = off[0]

    G("memset", [], [ident_f], ident_f[:], 1.0)
    G("affine_select", [ident_f], [ident_f], out=ident_f[:], in_=ident_f[:], pattern=[[-1, 128]],
      compare_op=ALU.is_equal, fill=0.0, base=0, channel_multiplier=1)
    V("tensor_copy", [ident_f], [ident_b], out=ident_b[:], in_=ident_f[:])
    G("memset", [], [nhalf], nhalf[:], -0.5)

    w_in_sb = sb("w_in_sb", [128, 8, DIN], BF16)
    wco_sb = sb("wco_sb", [128, 4, D], BF16)
    wao_sb = sb("wao_sb", [128, 4, D], BF16)
    wout_sb = sb("wout_sb", [128, 8, D], BF16)
    kern_sb = sb("kern_sb", [128, 4, 31], F32)
    cvec_sb = sb("cvec_sb", [128, 4, 3], F32)
    bm_sb = sb("bm_sb", [128, 8, 2, 128], F32)
    wr_sb = sb("wr_sb", [128, 8, 36], F32)
    brb = sb("brb", [128, 36], F32)
    gqb = sb("gqb", [128, 64], F32)
    gkb = sb("gkb", [128, 64], F32)
    gq8 = sb("gq8", [128, 8, 64], F32)
    gk2 = sb("gk2", [128, 2, 64], F32)
    snk = sb("snk", [128, 8], F32)
    esink = sb("esink", [128, 8], F32)
    negc = sb("negc", [128, 1], F32)
    mq = sb("mq", [128, 1], F32)
    mk = sb("mk", [128, 1], F32)
    onesdiv = sb("onesdiv", [128, 128], BF16)

    cgrp = P.grp("wcast")

    def expert_casts(e_):
        rws = slice(e_ * 128, (e_ + 1) * 128)
        P.op("gpsimd", lambda e: e.dma_start(
            out=WGU[rws, :, 0:DE], in_=weg[e_, :, :].rearrange("(p kc) f -> p kc f", p=128)), [], [], grp=cgrp)
        P.op("gpsimd", lambda e: e.dma_start(
            out=WGU[rws, :, DE:2 * DE], in_=weu[e_, :, :].rearrange("(p kc) f -> p kc f", p=128)), [], [], grp=cgrp)
        P.op("gpsimd", lambda e: e.dma_start(
            out=WDS[rws, :, :], in_=wed[e_, :, :].rearrange("(p kc) n -> p kc n", p=128)), [], [], grp=cgrp)
    load("sync", kern_sb, kern_sb[:], kern[:, :, :])
    load("sync", cvec_sb, cvec_sb[:], cvec[:, :, :])
    load("sync", bm_sb, bm_sb[:], bm[:, :, :, :])
    load("sync", wr_sb, wr_sb[:], wr.rearrange("(kc p) n -> p kc n", p=128))
    load("sync", brb, brb[:], br[0, :].partition_broadcast(128))
    load("sync", gqb, gqb[:], gq[0, :].partition_broadcast(128))
    load("sync", gkb, gkb[:], gk[0, :].partition_broadcast(128))
    load("sync", snk, snk[:], sinks[0, :].partition_broadcast(128))
    resident_end = off[0]
    stgw = [sb(f"stgw{i}", [128, 4096], F32) for i in range(2)]
    w_in_v = w_in.rearrange("(kc p) n -> p kc n", p=128)
    wco_v = wco.rearrange("(kc p) n -> p kc n", p=128)
    wao_v = wao.rearrange("(kc p) n -> p kc n", p=128)
    wout_v = wout.rearrange("(kc p) n -> p kc n", p=128)
    jobs = []
    for kc in range(8):
        jobs.append((w_in_v[:, kc, :], w_in_sb, w_in_sb[:, kc, :], 3840))
    jobs.append((wco_v, wco_sb, wco_sb[:], 4096))
    jobs.append((wao_v, wao_sb, wao_sb[:], 4096))
    for hh_ in range(2):
        jobs.append((wout_v[:, hh_ * 4:(hh_ + 1) * 4, :], wout_sb, wout_sb[:, hh_ * 4:(hh_ + 1) * 4, :], 4096))
    for ji, (src, dtl, dap, nel) in enumerate(jobs):
        st_ = stgw[ji % 2]
        sview = st_[:, 0:nel]
        if len(src.shape) == 3:
            sview = sview.rearrange("p (k n) -> p k n", k=src.shape[1])
        load("scalar", st_, sview, src)
        if ji % 2 == 0:
            V("tensor_copy", [st_], [dtl], out=dap, in_=sview)
        else:
            A("copy", [st_], [dtl], out=dap, in_=sview)

    csb = sb("csb", [128, 8], F32)
    scs = sb("scs", [128, 8], F32)
    scb = sb("scb", [128, 8, 128], F32)
    gmb = sb("gmb", [128, D], F32)
    gfb = sb("gfb", [128, D], F32)
    wa = [sb(f"wa{i}", [128, 8, 512], F32) for i in range(2)]
    load("sync", csb, csb[:], c_col[:, :])
    load("sync", mod, mod[:], b_ada[0, :].partition_broadcast(128))
    load("sync", gmb, gmb[:], gmix[0, :].partition_broadcast(128))
    load("sync", gfb, gfb[:], gffn[0, :].partition_broadcast(128))
    A("activation", [csb], [scs], out=scs[:], in_=csb[:], func=ACTF.Silu)
    for kc in range(8):
        V("tensor_copy", [scs], [scb], out=scb[:, kc, :], in_=scs[:, kc:kc + 1].to_broadcast([128, 128]))
    w_ada_v = w_ada.rearrange("(kc p) n -> p kc n", p=128)
    for n in range(12):
        wt = wa[n % 2]
        load("sync", wt, wt[:], w_ada_v[:, :, n * 512:(n + 1) * 512])
        pm = gmm()
        for kc in range(8):
            MM(pm, pm[:], scb, scb[:, kc, :], wt, wt[:, kc, :], kc == 0, kc == 7)
        V("tensor_tensor", [pm, mod], [mod], out=mod[:, n * 512:(n + 1) * 512], in0=pm[:],
          in1=mod[:, n * 512:(n + 1) * 512], op=ALU.add)
    V("scalar_tensor_tensor", [mod, gmb], [mod], out=A1, in0=A1, scalar=1.0, in1=gmb[:],
      op0=ALU.add, op1=ALU.mult)
    V("scalar_tensor_tensor", [mod, gfb], [mod], out=A2, in0=A2, scalar=1.0, in1=gfb[:],
      op0=ALU.add, op1=ALU.mult)
    P.barrier()
    off[0] = resident_end
    NS_run = NS
    if stop == "p0":
        NS_run = 0

    G("memset", [], [onesdiv], onesdiv[:], 1.0 / 512.0)
    for kc in range(8):
        V("tensor_tensor", [wout_sb, mod], [wout_sb], out=wout_sb[:, kc, :], in0=wout_sb[:, kc, :], in1=G1, op=ALU.mult)
    V("tensor_scalar", [gqb], [gq8], out=gq8[:], in0=gqb[:, None, :].to_broadcast([128, 8, 64]),
      scalar1=0.125, scalar2=None, op0=ALU.mult)
    V("tensor_copy", [gkb], [gk2], out=gk2[:], in_=gkb[:, None, :].to_broadcast([128, 2, 64]))
    V("reduce_max", [gqb], [mq], out=mq[:], in_=gqb[:], axis=AX.X, apply_absolute_value=True)
    V("reduce_max", [gkb], [mk], out=mk[:], in_=gkb[:], axis=AX.X, apply_absolute_value=True)
    V("scalar_tensor_tensor", [mq, mk], [negc], out=negc[:], in0=mq[:], scalar=-8.0, in1=mk[:],
      op0=ALU.mult, op1=ALU.mult)
    A("activation", [snk, negc], [esink], out=esink[:], in_=snk[:], func=ACTF.Exp, bias=negc[:, 0:1])

    xin = [sb(f"xin{i}", [128, D], F32) for i in range(2)]
    xr = [sb(f"xr{i}", [128, D], F32) for i in range(2)]
    tmpf = sb("tmpf", [128, D], F32)
    h2f = tmpf
    Lrow = [sb(f"Lrow{i}", [128, 36], F32) for i in range(2)]
    h2Tt = sb("h2Tt", [128, 1024], F32)
    hb = [sb(f"hb{i}", [128, D], BF16) for i in range(1)]
    h2b = [sb(f"h2b{i}", [128, D], BF16) for i in range(1)]
    hTs = [sb(f"hT{i}", [128, 8, TT], BF16) for i in range(2)]
    ubuf = sb("ubuf", [128, 4, TT + 30], F32)
    ub3 = sb("ub3", [128, TT + 30], BF16)
    dg = [sb(f"dg{i}", [128, 128], BF16) for i in range(4)]
    dgi = [0]
    acc = [sb(f"acc{c}", [128, TT], F32) for c in range(4)]
    cbf = sb("cbf", [128, 4, TT], BF16)
    sqb = sb("sqb", [128, 4, TT], BF16)
    uT = sb("uT", [128, 4, TT], BF16)
    oT = sb("oT", [128, 4, TT], BF16)
    mT = sb("mT", [128, 8, TT], BF16)
    sgb = [sb(f"sgb{i}", [128, TT], F32) for i in range(1)] * 2
    s1 = [sb(f"s1_{i}", [128, TT], F32) for i in range(2)]
    s2 = [sb(f"s2_{i}", [128, TT], F32) for i in range(2)]
    t1 = s1
    t2 = s2
    mean_sb = s1[0]
    m2 = s2[0]
    var = m2
    rln = m2
    ssq = sb("ssq", [128, 1], F32)
    msq = sb("msq", [128, 1], F32)
    rstd = sb("rstd", [128, 1], F32)
    ssq2 = sb("ssq2", [128, 1], F32)
    msq2 = sb("msq2", [128, 1], F32)
    rstd2 = sb("rstd2", [128, 1], F32)
    ssq10 = sb("ssq10", [128, 10], F32)
    ms10 = sb("ms10", [128, 10], F32)
    rs10 = sb("rs10", [128, 10], F32)
    qn = sb("qn", [128, 512], BF16)
    kpad = sb("kpad", [128, 2, 2, 128], BF16)
    vaug = [sb(f"vaug{i}", [128, 2, 65], BF16) for i in range(2)]
    qT = sb("qT", [128, 4, 128], BF16)
    kT = [sb(f"kT{i}", [128, 4, 128], BF16) for i in range(2)]
    lgT = sb("lgT", [128, 1024], F32)
    PT = sb("PT", [128, 4, 2, 128], BF16)
    den = sb("den", [128, 4], F32)
    rden = sb("rden", [128, 4], F32)
    onb = sb("onb", [128, 512], BF16)
    phase1_end = off[0]

    for i in range(2):
        G("memset", [], [vaug[i]], vaug[i][:], 1.0)
    G("memset", [], [kpad], kpad[:], 0.0)

    def stageA(s):
        hT = hTs[s % 2]
        for j in range(NBS):
            blk = s * NBS + j
            xi = xin[blk % 2]
            load("sync", xi, xi[:], x[blk * 128:(blk + 1) * 128, :])
            A("activation", [xi], [hb[0], ssq], out=hb[0][:], in_=xi[:], func=ACTF.Square, accum_out=ssq[:, 0:1])
            V("tensor_scalar", [ssq], [msq], out=msq[:], in0=ssq[:], scalar1=1.0 / D, scalar2=EPS,
              op0=ALU.mult, op1=ALU.add)
            G("tensor_tensor", [msq, nhalf], [rstd], out=rstd[:], in0=msq[:], in1=nhalf[:], op=ALU.pow)
            V("scalar_tensor_tensor", [xi, rstd, mod], [tmpf], out=tmpf[:], in0=xi[:], scalar=rstd[:, 0:1],
              in1=A1, op0=ALU.mult, op1=ALU.mult)
            hbt = hb[0]
            V("tensor_tensor", [tmpf, mod], [hbt], out=hbt[:], in0=tmpf[:], in1=B1, op=ALU.add)
            pt = gtp()
            for kc in range(8):
                TR(pt, pt[:, kc * 128:(kc + 1) * 128], hbt, hbt[:, kc * 128:(kc + 1) * 128], ident_b)
            A("copy", [pt], [hT], out=hT[:, :, j * 128:(j + 1) * 128],
              in_=pt[:].rearrange("p (k t) -> p k t", k=8))

    def stageB(s):
        hT = hTs[s % 2]
        if s == 0:
            G("memset", [], [ubuf], ubuf[:, :, 0:30], 0.0)
            G("memset", [], [ub3], ub3[:, 0:30], 0.0)
        else:
            A("copy", [ubuf], [ubuf], out=ubuf[:, 0:3, 0:30], in_=ubuf[:, 0:3, TT:TT + 30])
            A("copy", [ub3], [ub3], out=ub3[:, 0:30], in_=ub3[:, TT:TT + 30])
        for c in range(4):
            pa = gmm_ab()
            for kc in range(8):
                MM(pa, pa[:, 0:TT], w_in_sb, w_in_sb[:, kc, c * 128:(c + 1) * 128], hT, hT[:, kc, :], kc == 0, kc == 7)
            pb = gmm_ab()
            for kc in range(8):
                MM(pb, pb[:, 0:TT], w_in_sb, w_in_sb[:, kc, 512 + c * 128:512 + (c + 1) * 128], hT, hT[:, kc, :],
                   kc == 0, kc == 7)
            sg = sgb[c % 2]
            A("activation", [pb], [sg], out=sg[:], in_=pb[:, 0:TT], func=ACTF.Sigmoid)
            if c == 3:
                V("tensor_tensor", [pa, sg], [ub3], out=ub3[:, 30:30 + TT], in0=pa[:, 0:TT], in1=sg[:], op=ALU.mult)
            else:
                V("tensor_tensor", [pa, sg], [ubuf], out=ubuf[:, c, 30:30 + TT], in0=pa[:, 0:TT], in1=sg[:], op=ALU.mult)

    def stageC(s):
        bank = mmb[1]
        for tap in range(31):
            dgt = dg[dgi[0] % 4]
            dgi[0] += 1
            A("activation", [ident_f, kern_sb], [dgt], out=dgt[:], in_=ident_f[:], func=ACTF.Identity,
              scale=kern_sb[:, 3, tap:tap + 1])
            MM(bank, bank[:, 0:TT], dgt, dgt[:], ub3, ub3[:, tap:tap + TT], tap == 0, tap == 30)
        A("activation", [bank, cvec_sb], [acc[3]], out=acc[3][:], in_=bank[:, 0:TT], func=ACTF.Identity,
          bias=cvec_sb[:, 3, 0:1])
        pe_part = P.defer
        P.defer = []
        for c in range(3):
            V("tensor_scalar", [ubuf, kern_sb, cvec_sb], [acc[c]], out=acc[c][:], in0=ubuf[:, c, 0:TT],
              scalar1=kern_sb[:, c, 0:1], scalar2=cvec_sb[:, c, 0:1], op0=ALU.mult, op1=ALU.add)
        for tap in range(1, 31):
            for c in range(3):
                V("scalar_tensor_tensor", [ubuf, kern_sb, acc[c]], [acc[c]], out=acc[c][:],
                  in0=ubuf[:, c, tap:tap + TT], scalar=kern_sb[:, c, tap:tap + 1], in1=acc[c][:],
                  op0=ALU.mult, op1=ALU.add)
        dve_part = P.defer
        P.defer = merge(pe_part, dve_part)

    def stageD(s):
        sqv = sqb
        for c in range(4):
            A("copy", [acc[c]], [cbf], out=cbf[:, c, :], in_=acc[c][:])
            A("activation", [acc[c]], [sqb], out=sqv[:, c, :], in_=acc[c][:], func=ACTF.Square)
        pmn = mmb[1]
        for c in range(4):
            MM(pmn, pmn[:, 0:TT], onesdiv, onesdiv[:], cbf, cbf[:, c, :], c == 0, c == 3)
        pq2 = mmb[1]
        for c in range(4):
            MM(pq2, pq2[:, TT:2 * TT], onesdiv, onesdiv[:], sqb, sqv[:, c, :], c == 0, c == 3)
        A("copy", [pmn], [mean_sb], out=mean_sb[:], in_=pmn[:, 0:TT])
        V("tensor_tensor", [mean_sb], [m2], out=m2[:], in0=mean_sb[:], in1=mean_sb[:], op=ALU.mult)
        V("scalar_tensor_tensor", [pq2, m2], [m2], out=var[:], in0=pq2[:, TT:2 * TT], scalar=EPS, in1=m2[:],
          op0=ALU.add, op1=ALU.subtract)
        A("sqrt", [m2], [m2], out=m2[:], in_=m2[:])
        V("reciprocal", [m2], [m2], out=m2[:], in_=m2[:])
        for c in range(4):
            V("tensor_tensor", [acc[c], mean_sb], [acc[c]], out=acc[c][:], in0=acc[c][:], in1=mean_sb[:],
              op=ALU.subtract)
        for c in range(4):
            V("tensor_tensor", [acc[c], rln], [acc[c]], out=acc[c][:], in0=acc[c][:], in1=rln[:], op=ALU.mult)
        for c in range(4):
            A("activation", [acc[c], cvec_sb], [uT], out=uT[:, c, :], in_=acc[c][:], func=ACTF.Silu,
              bias=cvec_sb[:, c, 2:3], scale=cvec_sb[:, c, 1:2])

    def stageE(s):
        hT = hTs[s % 2]
        for j in range(NBS):
            blk = s * NBS + j
            tok = slice(j * 128, (j + 1) * 128)
            for kc in range(8):
                MM(attA, attA[:, 0:512], hT, hT[:, kc, tok], w_in_sb, w_in_sb[:, kc, 1024:1536], kc == 0, kc == 7)
            for kc in range(8):
                MM(attB, attB[:, 0:256], hT, hT[:, kc, tok], w_in_sb, w_in_sb[:, kc, 1536:1792], kc == 0, kc == 7)
            A("activation", [attA], [tmpf], out=tmpf[:, 0:512], in_=attA[:, 0:512], func=ACTF.Square)
            A("activation", [attB], [tmpf], out=tmpf[:, 512:640], in_=attB[:, 0:128], func=ACTF.Square)
            V("tensor_reduce", [tmpf], [ssq10], out=ssq10[:], in_=tmpf[:, 0:640].rearrange("p (h d) -> p h d", d=64),
              axis=AX.X, op=ALU.add)
            V("tensor_scalar", [ssq10], [ms10], out=ms10[:], in0=ssq10[:], scalar1=1.0 / 64, scalar2=EPS,
              op0=ALU.mult, op1=ALU.add)
            G("tensor_tensor", [ms10, nhalf], [rs10], out=rs10[:], in0=ms10[:],
              in1=nhalf[:, 0:1].to_broadcast([128, 10]), op=ALU.pow)
            V("tensor_tensor", [attA, rs10, ssq10], [tmpf], out=tmpf[:, 0:512].rearrange("p (h d) -> p h d", d=64), in0=attA[:, 0:512].rearrange("p (h d) -> p h d", d=64),
              in1=rs10[:, 0:8, None].to_broadcast([128, 8, 64]), op=ALU.mult)
            V("tensor_tensor", [tmpf, gq8], [qn], out=qn[:].rearrange("p (h d) -> p h d", d=64), in0=tmpf[:, 0:512].rearrange("p (h d) -> p h d", d=64),
              in1=gq8[:], op=ALU.mult)
            V("tensor_tensor", [attB, rs10], [tmpf], out=tmpf[:, 512:640].rearrange("p (h d) -> p h d", d=64), in0=attB[:, 0:128].rearrange("p (h d) -> p h d", d=64),
              in1=rs10[:, 8:10, None].to_broadcast([128, 2, 64]), op=ALU.mult)
            for dd in range(2):
                V("tensor_tensor", [tmpf, gk2], [kpad], out=kpad[:, :, dd, dd * 64:(dd + 1) * 64],
                  in0=tmpf[:, 512:640].rearrange("p (h d) -> p h d", d=64), in1=gk2[:], op=ALU.mult)
            va = vaug[blk % 2]
            A("copy", [attB], [va], out=va[:, :, 0:64], in_=attB[:, 128:256].rearrange("p (h d) -> p h d", d=64))
            pt = gtp()
            for c in range(4):
                TR(pt, pt[:, c * 128:(c + 1) * 128], qn, qn[:, c * 128:(c + 1) * 128], ident_b)
            kflat = kpad[:].rearrange("p a b d -> p (a b d)")
            for kv in range(4):
                TR(pt, pt[:, (4 + kv) * 128:(5 + kv) * 128], kpad, kflat[:, kv * 128:(kv + 1) * 128], ident_b)
            kTc = kT[blk % 2]
            kTp = kT[(blk - 1) % 2]
            A("copy", [pt], [qT], out=qT[:], in_=pt[:, 0:512].rearrange("p (c t) -> p c t", c=4))
            A("copy", [pt], [kTc], out=kTc[:], in_=pt[:, 512:1024].rearrange("p (c t) -> p c t", c=4))
            js = [1] if blk == 0 else [0, 1]
            jsl = slice(js[0], 2)
            attv = [(attA, attA[:].rearrange("p (h j q) -> p h j q", h=2, j=2)),
                    (attB, attB[:].rearrange("p (h j q) -> p h j q", h=2, j=2))]
            lg4 = lgT[:].rearrange("p (h j q) -> p h j q", h=4, j=2)
            for g in range(2):
                for hh in range(4):
                    h = 4 * g + hh
                    c, r = h // 2, h % 2
                    for jj in js:
                        kTt = kTp if jj == 0 else kTc
                        at_, av_ = attv[hh // 2]
                        MM(at_, av_[:, hh % 2, jj, :], kTt, kTt[:, 2 * g + r, :], qT, qT[:, c, :], True, True)
                for hp in range(2):
                    at_, av_ = attv[hp]
                    V("tensor_tensor", [at_, bm_sb], [lgT], out=lg4[:, 2 * hp:2 * hp + 2, jsl, :], in0=av_[:, :, jsl, :],
                      in1=bm_sb[:, 4 * g + 2 * hp:4 * g + 2 * hp + 2, jsl, :], op=ALU.add)
                A("activation", [lgT, negc], [PT], out=PT[:, :, jsl, :], in_=lg4[:, :, jsl, :], func=ACTF.Exp,
                  bias=negc[:, 0:1])
                po = mmb[0]
                po3 = po[:, 0:260].rearrange("p (h e) -> p h e", e=65)
                vp = vaug[(blk - 1) % 2]
                for hh in range(4):
                    for jj in js:
                        vt = vp if jj == 0 else va
                        MM(po, po3[:, hh, :], PT, PT[:, hh, jj, :], vt, vt[:, g, :], jj == js[0], jj == js[-1])
                V("tensor_tensor", [po, esink], [den], out=den[:], in0=po3[:, :, 64], in1=esink[:, 4 * g:4 * g + 4],
                  op=ALU.add)
                V("reciprocal", [den], [rden], out=rden[:], in_=den[:])
                V("tensor_tensor", [po, rden], [onb],
                  out=onb[:].rearrange("p (h d) -> p h d", d=64)[:, 4 * g:4 * g + 4, :], in0=po3[:, :, 0:64],
                  in1=rden[:, :, None].to_broadcast([128, 4, 64]), op=ALU.mult)
            pt = gtp()
            for c in range(4):
                TR(pt, pt[:, c * 128:(c + 1) * 128], onb, onb[:, c * 128:(c + 1) * 128], ident_b)
            A("copy", [pt], [oT], out=oT[:, :, tok], in_=pt[:, 0:512].rearrange("p (c t) -> p c t", c=4))

    def stageF(s):
        hT = hTs[s % 2]
        for mc in range(8):
            ms_ = slice(mc * 128, (mc + 1) * 128)
            i2 = mc % 2
            bx, by = (mmb[2], mmb[3]) if i2 == 0 else (attA, attB)
            for kc in range(8):
                MM(bx, bx[:, 0:TT], w_in_sb, w_in_sb[:, kc, 1792 + mc * 128:1792 + (mc + 1) * 128], hT, hT[:, kc, :],
                   kc == 0, kc == 7)
            A("activation", [bx], [s1[i2]], out=s1[i2][:], in_=bx[:, 0:TT], func=ACTF.Sigmoid)
            for kc in range(8):
                MM(by, by[:, 0:TT], w_in_sb, w_in_sb[:, kc, 2816 + mc * 128:2816 + (mc + 1) * 128], hT, hT[:, kc, :],
                   kc == 0, kc == 7)
            A("activation", [by], [s2[i2]], out=s2[i2][:], in_=by[:, 0:TT], func=ACTF.Sigmoid)
            for kc in range(4):
                MM(bx, bx[:, 0:TT], wco_sb, wco_sb[:, kc, ms_], uT, uT[:, kc, :], kc == 0, kc == 3)
            V("tensor_tensor", [bx, s1[i2]], [s1[i2]], out=s1[i2][:], in0=bx[:, 0:TT], in1=s1[i2][:], op=ALU.mult)
            for kc in range(4):
                MM(by, by[:, 0:TT], wao_sb, wao_sb[:, kc, ms_], oT, oT[:, kc, :], kc == 0, kc == 3)
            V("tensor_tensor", [by, s2[i2]], [s2[i2]], out=s2[i2][:], in0=by[:, 0:TT], in1=s2[i2][:], op=ALU.mult)
            V("tensor_tensor", [s1[i2], s2[i2]], [mT], out=mT[:, mc, :], in0=s1[i2][:], in1=s2[i2][:], op=ALU.add)

    def stageG(s):
        for j in range(NBS):
            blk = s * NBS + j
            tok = slice(j * 128, (j + 1) * 128)
            rows = slice(blk * 128, (blk + 1) * 128)
            xrt = xr[blk % 2]
            load("sync", xrt, xrt[:], x[rows, :])
            for nh in range(2):
                cs = slice(nh * 512, (nh + 1) * 512)
                pp = gmm_g()
                for kc in range(8):
                    MM(pp, pp[:], mT, mT[:, kc, tok], wout_sb, wout_sb[:, kc, cs], kc == 0, kc == 7)
                V("tensor_tensor", [pp, xrt], [xrt], out=xrt[:, cs], in0=pp[:], in1=xrt[:, cs], op=ALU.add)
            store("gpsimd", xrt, out[rows, :], xrt[:])
            A("activation", [xrt], [h2b[0], ssq2], out=h2b[0][:], in_=xrt[:], func=ACTF.Square, accum_out=ssq2[:, 0:1])
            V("tensor_scalar", [ssq2], [msq2], out=msq2[:], in0=ssq2[:], scalar1=1.0 / D, scalar2=EPS,
              op0=ALU.mult, op1=ALU.add)
            G("tensor_tensor", [msq2, nhalf], [rstd2], out=rstd2[:], in0=msq2[:], in1=nhalf[:], op=ALU.pow)
            h2f = xrt
            V("scalar_tensor_tensor", [xrt, rstd2, mod], [xrt], out=xrt[:], in0=xrt[:], scalar=rstd2[:, 0:1],
              in1=A2, op0=ALU.mult, op1=ALU.mult)
            V("tensor_tensor", [xrt, mod], [xrt], out=xrt[:], in0=xrt[:], in1=B2, op=ALU.add)
            hbt = h2b[0]
            A("copy", [h2f], [hbt], out=hbt[:], in_=h2f[:])
            store("gpsimd", hbt, H2[rows, :], hbt[:])
            h2T3 = h2Tt[:].rearrange("p (k t) -> p k t", k=8)
            for hf in range(2):
                pp = gmm_g()
                for i in range(4):
                    kc = hf * 4 + i
                    TR(pp, pp[:, i * 128:(i + 1) * 128], h2f, h2f[:, kc * 128:(kc + 1) * 128], ident_f)
                if hf == 0:
                    A("copy", [pp], [h2Tt], out=h2T3[:, 0:4, :], in_=pp[:].rearrange("p (k t) -> p k t", k=4))
                else:
                    V("tensor_copy", [pp], [h2Tt], out=h2T3[:, 4:8, :], in_=pp[:].rearrange("p (k t) -> p k t", k=4))
            pp = gmm_g()
            for kc in range(8):
                MM(pp, pp[:, 0:36], h2Tt, h2T3[:, kc, :], wr_sb, wr_sb[:, kc, :], kc == 0, kc == 7)
            lr = Lrow[blk % 2]
            V("tensor_tensor", [pp, brb], [lr], out=lr[:], in0=pp[:, 0:36], in1=brb[:], op=ALU.add)
            store("gpsimd", lr, Ldram[:, blk, :], lr[:])
        if not phase1_only:
            for e_ in range(s * NE // NS, (s + 1) * NE // NS):
                expert_casts(e_)

    def run_stage(fn, s):
        P.defer = []
        fn(s)
        lst = P.defer
        P.defer = None
        return lst

    def merge(la, lb):
        res = []
        ia = ib = 0
        na, nb = len(la), len(lb)
        while ia < na or ib < nb:
            if ib >= nb or (ia < na and ia * nb <= ib * na):
                res.append(la[ia]); ia += 1
            else:
                res.append(lb[ib]); ib += 1
        return res

    def play(lst):
        for a in lst:
            P.op(*a)

    if NS_run:
        play(run_stage(stageA, 0) + run_stage(stageB, 0))
    for s in range(NS_run):
        eg = run_stage(stageE, s)
        if s > 0:
            eg = merge(eg, run_stage(stageG, s - 1))
        play(merge(run_stage(stageC, s) + run_stage(stageD, s), eg))
        df = run_stage(stageF, s)
        if s + 1 < NS_run:
            df = merge(df, run_stage(stageA, s + 1) + run_stage(stageB, s + 1))
        play(df)
    if NS_run:
        play(run_stage(stageG, NS_run - 1))

    P.barrier()
    if debug:
        P.op("sync", lambda e: e.dma_start(out=Ldbg[:, :, :], in_=Ldram[:, :, :]), [], [], grp=P.grp("dbgL"))


    if not phase1_only:
        off[0] = persist_end
        KMAX = -(-T // BLK)
        w1 = sb("w1", [128, NB], F32)
        w2 = sb("w2", [128, NB], F32)
        d1i = sb("d1i", [128, NB], I32)
        d2i = sb("d2i", [128, NB], I32)
        blke_i = sb("blke_i", [128, NBLK], I32)
        Lt = sb("Lt", [128, NB, 36], F32)
        load("sync", Lt, Lt[:], Ldram[:, :, :])
        widx = sb("widx", [128, NBLK], I32)
        route_keep = off[0]
        ones_f = sb("ones_f", [128, 128], F32)
        ustr = sb("ustr", [128, 128], F32)
        gmax = sb("gmax", [128, NB], F32)
        ohg = sb("ohg", [128, NB, 4], F32)
        eg = sb("eg", [128, NB, 4], F32)
        sume = sb("sume", [128, NB], F32)
        ptop = sb("ptop", [128, NB], F32)
        tmp8 = sb("tmp8", [128, NB, 8], F32)
        elsel = sb("elsel", [128, NB, 8], F32)
        els2 = sb("els2", [128, NB, 8], F32)
        oh1 = sb("oh1", [128, NB, 8], F32)
        oh2 = sb("oh2", [128, NB, 8], F32)
        m1 = sb("m1", [128, NB], F32)
        m2v = sb("m2v", [128, NB], F32)
        ddv = sb("ddv", [128, NB], F32)
        e2v = sb("e2v", [128, NB], F32)
        OH1 = sb("OH1", [128, NB, 32], F32)
        OH2 = sb("OH2", [128, NB, 32], F32)
        TH = sb("TH", [128, NB, 32], F32)
        scn = [sb(f"scn{i}", [128, NB, 32], F32) for i in range(2)]
        cnt_p = sb("cnt_p", [128, 32], F32)
        tot_sb = sb("tot_sb", [128, 32], F32)
        pp_sb = sb("pp_sb", [128, 32], F32)
        thr_i = sb("thr_i", [128, KMAX], I32)
        thr_f = sb("thr_f", [128, KMAX], F32)
        cmpt = sb("cmpt", [128, 32, KMAX], F32)
        pc = sb("pc", [128, 32], F32)
        pe_ = [sb(f"pend{i}", [128, 32], F32) for i in range(2)]
        base = sb("base", [128, 32], F32)
        dst_f = sb("dst_f", [128, NB], F32)
        bthr_i = sb("bthr_i", [128, NBLK], I32)
        bthr_f = sb("bthr_f", [128, NBLK], F32)
        cmpb = sb("cmpb", [128, NBLK, 32], F32)
        blke_f = sb("blke_f", [128, NBLK], F32)
        hs = [sb(f"hs{i}", [128, D], BF16) for i in range(3)]
        pidx_i = sb("pidx_i", [128, 1], I32)
        pidx_f = sb("pidx_f", [128, 1], F32)
        widx_f = sb("widx_f", [128, NBLK], F32)

        G("memset", [], [ones_f], ones_f[:], 1.0)
        G("memset", [], [ustr], ustr[:], 1.0)
        G("affine_select", [ustr], [ustr], out=ustr[:], in_=ustr[:], pattern=[[1, 128]],
          compare_op=ALU.is_gt, fill=0.0, base=0, channel_multiplier=-1)
        G("iota", [], [thr_i], thr_i[:], pattern=[[BLK, KMAX]], base=0, channel_multiplier=0)
        G("iota", [], [bthr_i], bthr_i[:], pattern=[[BLK, NBLK]], base=0, channel_multiplier=0)
        V("tensor_copy", [thr_i], [thr_f], out=thr_f[:], in_=thr_i[:])
        V("tensor_copy", [bthr_i], [bthr_f], out=bthr_f[:], in_=bthr_i[:])

        gl = Lt[:, :, 0:4]
        el4 = Lt[:, :, 4:36].rearrange("p t (g e) -> p t g e", g=4)
        bc4 = lambda ap: ap[:, :, None].to_broadcast([128, NB, 4])
        bc8 = lambda ap: ap[:, :, None].to_broadcast([128, NB, 8])
        V("tensor_reduce", [Lt], [gmax], out=gmax[:], in_=gl, axis=AX.X, op=ALU.max)
        V("tensor_tensor", [Lt, gmax], [ohg], out=ohg[:], in0=gl, in1=bc4(gmax), op=ALU.is_equal)
        V("tensor_tensor", [Lt, gmax], [eg], out=eg[:], in0=gl, in1=bc4(gmax), op=ALU.subtract)
        A("activation", [eg], [eg], out=eg[:], in_=eg[:], func=ACTF.Exp)
        V("tensor_reduce", [eg], [sume], out=sume[:], in_=eg[:], axis=AX.X, op=ALU.add)
        V("reciprocal", [sume], [ptop], out=ptop[:], in_=sume[:])
        V("tensor_tensor", [Lt, ohg], [elsel], out=elsel[:], in0=el4[:, :, 0, :],
          in1=ohg[:, :, 0:1].to_broadcast([128, NB, 8]), op=ALU.mult)
        for g in range(1, 4):
            V("tensor_tensor", [Lt, ohg], [tmp8], out=tmp8[:], in0=el4[:, :, g, :],
              in1=ohg[:, :, g:g + 1].to_broadcast([128, NB, 8]), op=ALU.mult)
            V("tensor_tensor", [elsel, tmp8], [elsel], out=elsel[:], in0=elsel[:], in1=tmp8[:], op=ALU.add)
        V("tensor_reduce", [elsel], [m1], out=m1[:], in_=elsel[:], axis=AX.X, op=ALU.max)
        V("tensor_tensor", [elsel, m1], [oh1], out=oh1[:], in0=elsel[:], in1=bc8(m1), op=ALU.is_equal)
        V("scalar_tensor_tensor", [oh1, elsel], [els2], out=els2[:], in0=oh1[:], scalar=-1e30, in1=elsel[:],
          op0=ALU.mult, op1=ALU.add)
        V("tensor_reduce", [els2], [m2v], out=m2v[:], in_=els2[:], axis=AX.X, op=ALU.max)
        V("tensor_tensor", [els2, m2v], [oh2], out=oh2[:], in0=els2[:], in1=bc8(m2v), op=ALU.is_equal)
        V("tensor_tensor", [m2v, m1], [ddv], out=ddv[:], in0=m2v[:], in1=m1[:], op=ALU.subtract)
        A("activation", [ddv], [e2v], out=e2v[:], in_=ddv[:], func=ACTF.Exp)
        V("tensor_scalar", [e2v], [ddv], out=ddv[:], in0=e2v[:], scalar1=1.0, scalar2=None, op0=ALU.add)
        V("reciprocal", [ddv], [sume], out=sume[:], in_=ddv[:])
        V("tensor_tensor", [sume, ptop], [w1], out=w1[:], in0=sume[:], in1=ptop[:], op=ALU.mult)
        V("tensor_tensor", [w1, e2v], [w2], out=w2[:], in0=w1[:], in1=e2v[:], op=ALU.mult)
        for OHk, ohk in ((OH1, oh1), (OH2, oh2)):
            V("tensor_tensor", [ohg, ohk], [OHk], out=OHk[:].rearrange("p t (g e) -> p t g e", g=4),
              in0=ohg[:, :, :, None].to_broadcast([128, NB, 4, 8]),
              in1=ohk[:, :, None, :].to_broadcast([128, NB, 4, 8]), op=ALU.mult)
        V("tensor_tensor", [OH1, OH2], [TH], out=TH[:], in0=OH1[:], in1=OH2[:], op=ALU.add)
        V("tensor_reduce", [TH], [cnt_p], out=cnt_p[:], in_=TH[:].rearrange("p t e -> p e t"), axis=AX.X, op=ALU.add)
        pA = gmm()
        MM(pA, pA[:, 0:32], ustr, ustr[:], cnt_p, cnt_p[:], True, True)
        pB = gmm()
        MM(pB, pB[:, 0:32], ones_f, ones_f[:], cnt_p, cnt_p[:], True, True)
        A("copy", [pA], [pp_sb], out=pp_sb[:], in_=pA[:, 0:32])
        A("copy", [pB], [tot_sb], out=tot_sb[:], in_=pB[:, 0:32])
        V("tensor_tensor", [tot_sb, thr_f], [cmpt], out=cmpt[:], in0=tot_sb[:, :, None].to_broadcast([128, 32, KMAX]),
          in1=thr_f[:, None, :].to_broadcast([128, 32, KMAX]), op=ALU.is_gt)
        V("tensor_reduce", [cmpt], [pc], out=pc[:], in_=cmpt[:], axis=AX.X, op=ALU.add)
        V("tensor_scalar", [pc], [pc], out=pc[:], in0=pc[:], scalar1=float(BLK), scalar2=None, op0=ALU.mult)
        src = pc
        st = 1
        i = 0
        while st < 32:
            dstt = pe_[i % 2]
            V("tensor_tensor", [src], [dstt], out=dstt[:, st:], in0=src[:, st:], in1=src[:, 0:32 - st], op=ALU.add)
            V("tensor_copy", [src], [dstt], out=dstt[:, 0:st], in_=src[:, 0:st])
            src = dstt
            st *= 2
            i += 1
        pend = src
        V("tensor_tensor", [pend, pc], [base], out=base[:], in0=pend[:], in1=pc[:], op=ALU.subtract)
        V("tensor_tensor", [base, pp_sb], [base], out=base[:], in0=base[:], in1=pp_sb[:], op=ALU.add)
        src = TH
        st = 1
        i = 0
        while st < NB:
            dstt = scn[i % 2]
            V("tensor_tensor", [src], [dstt], out=dstt[:, st:, :], in0=src[:, st:, :], in1=src[:, 0:NB - st, :], op=ALU.add)
            G("tensor_copy", [src], [dstt], out=dstt[:, 0:st, :], in_=src[:, 0:st, :])
            src = dstt
            st *= 2
            i += 1
        pos = scn[i % 2] if src is not scn[i % 2] else scn[(i + 1) % 2]
        if src is TH:
            pos = scn[0]
        V("tensor_tensor", [src, TH], [pos], out=pos[:], in0=src[:], in1=TH[:], op=ALU.subtract)
        V("tensor_tensor", [pos, base], [pos], out=pos[:], in0=pos[:], in1=base[:, None, :].to_broadcast([128, NB, 32]),
          op=ALU.add)
        for OHk, dki in ((OH1, d1i), (OH2, d2i)):
            V("tensor_tensor", [OHk, pos], [OHk], out=OHk[:], in0=OHk[:], in1=pos[:], op=ALU.mult)
            V("tensor_reduce", [OHk], [dst_f], out=dst_f[:], in_=OHk[:], axis=AX.X, op=ALU.add)
            V("tensor_copy", [dst_f], [dki], out=dki[:], in_=dst_f[:])
        V("tensor_tensor", [pend, bthr_f], [cmpb], out=cmpb[:], in0=pend[:, None, :].to_broadcast([128, NBLK, 32]),
          in1=bthr_f[:, :, None].to_broadcast([128, NBLK, 32]), op=ALU.is_le)
        V("tensor_reduce", [cmpb], [blke_f], out=blke_f[:], in_=cmpb[:], axis=AX.X, op=ALU.add)
        V("tensor_scalar", [blke_f], [blke_f], out=blke_f[:], in0=blke_f[:], scalar1=float(NE - 1), scalar2=None,
          op0=ALU.min)
        V("tensor_copy", [blke_f], [blke_i], out=blke_i[:], in_=blke_f[:])
        G("iota", [], [pidx_i], pidx_i[:], pattern=[[0, 1]], base=0, channel_multiplier=1)
        V("tensor_copy", [pidx_i], [pidx_f], out=pidx_f[:], in_=pidx_i[:])
        V("tensor_scalar", [blke_f, pidx_f], [widx_f], out=widx_f[:], in0=blke_f[:], scalar1=128.0,
          scalar2=pidx_f[:, 0:1], op0=ALU.mult, op1=ALU.add)
        V("tensor_copy", [widx_f], [widx], out=widx[:], in_=widx_f[:])
        if debug:
            dbg = sb("dbg", [128, 4, NB], F32)
            V("tensor_copy", [w1], [dbg], out=dbg[:, 0, :], in_=w1[:])
            V("tensor_copy", [w2], [dbg], out=dbg[:, 1, :], in_=w2[:])
            V("tensor_copy", [d1i], [dbg], out=dbg[:, 2, :], in_=d1i[:])
            V("tensor_copy", [d2i], [dbg], out=dbg[:, 3, :], in_=d2i[:])
            store("sync", dbg, Ddbg[:, :, :], dbg[:])

        for t in range(0 if stop == "R" else NB):
            hst = hs[t % 3]
            load("sync", hst, hst[:], H2[t * 128:(t + 1) * 128, :])
            if hst.sg is None:
                hst.sg = P.grp("s_" + hst.name)
            for dki in (d1i, d2i):
                P.op("gpsimd", lambda e, hst=hst, dki=dki, t=t: e.indirect_dma_start(
                    out=XS[:, :], out_offset=bass.IndirectOffsetOnAxis(ap=dki[:, t:t + 1], axis=0),
                    in_=hst[:], in_offset=None), [hst, dki], [], grp=hst.sg)
        P.barrier()

        off[0] = route_keep
        Wgu = [sb(f"Wgu{i}", [128, 8, 2 * DE], BF16) for i in range(2)]
        Wd = [sb(f"Wd{i}", [128, 2, D], BF16) for i in range(2)]
        xs_in = [sb(f"xs_in{i}", [128, D], BF16) for i in range(3)]
        XTs = [sb(f"XT{i}", [128, 8, 128], BF16) for i in range(2)]
        sgls = [sb(f"sgl{i}", [128, DE], F32) for i in range(2)]
        hids = [sb(f"hid{i}", [128, DE], BF16) for i in range(2)]
        hidTs = [sb(f"hidT{i}", [128, 2, 128], BF16) for i in range(2)]
        ysb = [sb(f"ysb{i}", [128, D], F32) for i in range(3)]
        wgu_v = WGU.rearrange("r k f -> r (k f)")
        wds_v = WDS.rearrange("r k f -> r (k f)")

        def dyn_load(tl, src_v, b):
            if tl.lg is None:
                tl.lg = P.grp("l_" + tl.name)
            return P.op("gpsimd", lambda e: e.indirect_dma_start(
                out=tl[:].rearrange("p k f -> p (k f)"), out_offset=None, in_=src_v[:, :],
                in_offset=bass.IndirectOffsetOnAxis(ap=widx[:, b:b + 1], axis=0)), [widx], [tl], grp=tl.lg)

        def gathers(b):
            dyn_load(Wgu[b % 2], wgu_v, b)
            dyn_load(Wd[b % 2], wds_v, b)

        NBLK_run = 0 if stop in ("R", "S") else NBLK
        if NBLK_run:
            gathers(0)
        att_bf = [attA[:].bitcast(BF16), attB[:].bitcast(BF16)]
        att_tl = [attA, attB]

        def front(sl):
            b = sl // SUB
            i = b % 2
            k = sl % 2
            rows = slice(sl * 128, (sl + 1) * 128)
            xst = xs_in[sl % 3]
            XT, sgl, hid = XTs[k], sgls[k], hids[k]
            pt = tp[k]
            for kc in range(8):
                TR(pt, pt[:, kc * 128:(kc + 1) * 128], xst, xst[:].rearrange("p (f k) -> p k f", k=8)[:, kc, :], ident_b)
            A("copy", [pt], [XT], out=XT[:], in_=pt[:].rearrange("p (k t) -> p k t", k=8))
            pm = mmb[k]
            for kc in range(8):
                MM(pm, pm[:], XT, XT[:, kc, :], Wgu[i], Wgu[i][:, kc, :], kc == 0, kc == 7)
            A("activation", [pm], [sgl], out=sgl[:], in_=pm[:, 0:DE], func=ACTF.Silu)
            V("tensor_tensor", [pm, sgl], [hid], out=hid[:], in0=pm[:, DE:2 * DE], in1=sgl[:], op=ALU.mult)

        def back(sl):
            b = sl // SUB
            i = b % 2
            k = sl % 2
            rows = slice(sl * 128, (sl + 1) * 128)
            hid, hidT = hids[k], hidTs[k]
            pt2t, pt2 = att_tl[k], att_bf[k]
            for kc in range(2):
                TR(pt2t, pt2[:, kc * 128:(kc + 1) * 128], hid, hid[:].rearrange("p (f k) -> p k f", k=2)[:, kc, :], ident_b)
            A("copy", [pt2t], [hidT], out=hidT[:], in_=pt2[:, 0:256].rearrange("p (k t) -> p k t", k=2))
            yst = ysb[sl % 3]
            for nh in range(2):
                po = mmb[2 + nh]
                for kc in range(2):
                    MM(po, po[:], hidT, hidT[:, kc, :], Wd[i], Wd[i][:, kc, nh * 512:(nh + 1) * 512], kc == 0, kc == 1)
                V("tensor_tensor", [po, mod], [yst], out=yst[:, nh * 512:(nh + 1) * 512], in0=po[:],
                  in1=G2[:, nh * 512:(nh + 1) * 512], op=ALU.mult)
            store("sync", yst, YS[rows, :], yst[:])

        NSL = NBLK_run * SUB

        def xload(sl):
            xst = xs_in[sl % 3]
            load("sync", xst, xst[:], XS[sl * 128:(sl + 1) * 128, :])

        if NSL:
            xload(0)
            if NSL > 1:
                xload(1)
            play(run_stage(front, 0))
        for sl in range(NSL):
            if sl + 2 < NSL:
                xload(sl + 2)
            if sl % SUB == 0 and sl // SUB + 1 < NBLK_run:
                gathers(sl // SUB + 1)
            if sl + 1 < NSL:
                play(run_stage(front, sl + 1))
            play(run_stage(back, sl))
        P.barrier()

        y1r = [sb(f"y1r{i}", [128, D], F32) for i in range(2)]
        y2r = [sb(f"y2r{i}", [128, D], F32) for i in range(2)]
        xo = [sb(f"xo{i}", [128, D], F32) for i in range(2)]
        for t in range(0 if stop in ("R", "S", "X") else NB):
            rows = slice(t * 128, (t + 1) * 128)
            y1, y2, xot = y1r[t % 2], y2r[t % 2], xo[t % 2]
            for yt, dki in ((y1, d1i), (y2, d2i)):
                if yt.lg is None:
                    yt.lg = P.grp("l_" + yt.name)
                P.op("gpsimd", lambda e, yt=yt, dki=dki, t=t: e.indirect_dma_start(
                    out=yt[:], out_offset=None, in_=YS[:, :],
                    in_offset=bass.IndirectOffsetOnAxis(ap=dki[:, t:t + 1], axis=0)), [dki], [yt], grp=yt.lg)
            load("sync", xot, xot[:], out[rows, :])
            V("scalar_tensor_tensor", [y1, w1, xot], [xot], out=xot[:], in0=y1[:], scalar=w1[:, t:t + 1], in1=xot[:],
              op0=ALU.mult, op1=ALU.add)
            V("scalar_tensor_tensor", [y2, w2, xot], [xot], out=xot[:], in0=y2[:], scalar=w2[:, t:t + 1], in1=xot[:],
              op0=ALU.mult, op1=ALU.add)
            store("scalar", xot, out[rows, :], xot[:])

    P.barrier()
    P.op("sync", lambda e: e.nop())
    P.finalize()
    sems = {e: nc.alloc_semaphore("sem_" + e) for e in ENGS}
    for g in P.grps:
        g.sem = nc.alloc_semaphore("g_" + g.name)
    with nc.Block() as block:
        @block.tensor
        def _(e):
            P.emit_engine("tensor", e, sems)

        @block.vector
        def _(e):
            P.emit_engine("vector", e, sems)

        @block.scalar
        def _(e):
            P.emit_engine("scalar", e, sems)

        @block.gpsimd
        def _(e):
            P.emit_engine("gpsimd", e, sems)

        @block.sync
        def _(e):
            P.emit_engine("sync", e, sems)
    return nc


def _t5_bucket_table():
    W = 128
    qi = np.arange(W)[:, None]
    kj = np.arange(2 * W)[None, :]
    dist = qi + W - kj
    in_window = (dist >= 0) & (dist < W)
    dc = np.clip(dist, 0, 128)
    max_exact = 16
    d = np.maximum(dc, 1).astype(np.float32)
    large = max_exact + (np.log(d / max_exact) / math.log(128 / max_exact) * (32 - max_exact)).astype(np.int32)
    large = np.minimum(large, 31)
    bucket = np.where(dc < max_exact, dc, large)
    return bucket, in_window


def prep_shared(inp):
    f = lambda a: np.ascontiguousarray(np.asarray(a, dtype=np.float32))
    bucket, in_window = _t5_bucket_table()
    tab = f(inp["rel_bias_table"])
    bias = tab[bucket]
    bias = np.where(in_window[:, :, None], bias, np.float32(-1e30))
    bmh = bias.reshape(128, 2, 128, 8).transpose(2, 3, 1, 0)
    sh = {
        "w_ada": f(inp["w_ada"][0]),
        "b_ada": f(inp["b_ada"][0]).reshape(1, -1),
        "gmix": f(inp["norm_mix_g"][0]).reshape(1, -1),
        "gffn": f(inp["norm_ffn_g"][0]).reshape(1, -1),
        "w_in": f(inp["w_in"][0]),
        "kern": f(np.asarray(inp["dw_kernel"][0]).reshape(31, 4, 128).transpose(2, 1, 0)),
        "cvec": f(np.stack([np.asarray(inp["dw_bias"][0]).reshape(4, 128).T,
                            np.asarray(inp["conv_ln_g"][0]).reshape(4, 128).T,
                            np.asarray(inp["conv_ln_b"][0]).reshape(4, 128).T], axis=2)),
        "wco": f(inp["w_conv_out"][0]),
        "wao": f(inp["w_attn_out"][0]),
        "wout": f(inp["w_out"][0]),
        "gq": f(inp["q_norm_g"][0]).reshape(1, -1),
        "gk": f(inp["k_norm_g"][0]).reshape(1, -1),
        "sinks": f(inp["sinks"][0]).reshape(1, -1),
        "bm": f(bmh),
        "wr": f(np.concatenate([np.asarray(inp["w_router_group"][0]), np.asarray(inp["w_router_expert"][0])], axis=1)),
        "br": f(np.concatenate([np.asarray(inp["b_router_group"][0]), np.asarray(inp["b_router_expert"][0])])).reshape(1, -1),
        "weg": f(inp["w_exp_gate"][0]),
        "weu": f(inp["w_exp_up"][0]),
        "wed": f(inp["w_exp_down"][0]),
    }
    return sh


def kernel(**inputs):
    x = np.asarray(inputs["x"], dtype=np.float32)
    c = np.asarray(inputs["c"], dtype=np.float32)
    Bn, T, _ = x.shape
    sh = prep_shared(inputs)
    nc = build(T)
    in_maps = []
    for b in range(Bn):
        m = dict(sh)
        m["x"] = np.ascontiguousarray(x[b])
        m["c_col"] = np.ascontiguousarray(c[b].reshape(8, 128).T)
        in_maps.append(m)
    res = run_bass_kernel_spmd(nc, in_maps, core_ids=list(range(Bn)))
    return np.stack([np.asarray(r["out"]) for r in res.results], axis=0).astype(np.float32)
```

```python
import math
import numpy as np
import concourse.bass as bass
import concourse.mybir as mybir
from concourse.bass_utils import run_bass_kernel_spmd

F32 = mybir.dt.float32
BF16 = mybir.dt.bfloat16
I32 = mybir.dt.int32
ALU = mybir.AluOpType
ACTF = mybir.ActivationFunctionType
AX = mybir.AxisListType

ENGS = ["tensor", "vector", "scalar", "gpsimd", "sync"]

D = 1024
DIN = 3840
TT = 256
NBS = TT // 128
EPS = 1e-6
NE = 32
DE = 256
SUB = 2
BLK = 128 * SUB


class Tl:
    def __init__(self, name, t):
        self.name = name
        self.t = t
        self.w = []
        self.r = []
        self.lg = None
        self.sg = None

    def __getitem__(self, k):
        return self.t[k]


class Grp:
    def __init__(self, name):
        self.name = name
        self.n = 0
        self.sem = None


class Op:
    __slots__ = ("eng", "fn", "deps", "grp", "signal", "count", "waits")

    def __init__(self, eng, fn, deps, grp=None):
        self.eng = eng
        self.fn = fn
        self.deps = deps
        self.grp = grp
        self.signal = False
        self.count = None
        self.waits = None


class Prog:
    def __init__(self, nc):
        self.nc = nc
        self.ops = {e: [] for e in ENGS}
        self.grps = []
        self.extra = {e: [] for e in ENGS}
        self.defer = None

    def grp(self, name):
        g = Grp(name)
        self.grps.append(g)
        return g

    def op(self, eng, fn, reads=(), writes=(), grp=None):
        if self.defer is not None:
            self.defer.append((eng, fn, list(reads), list(writes), grp))
            return None
        deps = []
        for t in reads:
            deps.extend(t.w)
        for t in writes:
            deps.extend(t.w)
            deps.extend(t.r)
        deps.extend(self.extra[eng])
        self.extra[eng] = []
        o = Op(eng, fn, deps, grp)
        self.ops[eng].append(o)
        idx = len(self.ops[eng]) - 1
        if grp is not None:
            grp.n += 1
            tok = ("d", grp, grp.n)
        else:
            tok = ("c", eng, idx)
        wset = set(id(t) for t in writes)
        for t in reads:
            if id(t) in wset:
                continue
            t.r = [x for x in t.r if not (x[0] == tok[0] and x[1] is tok[1])] + [tok]
        for t in writes:
            t.w = [tok]
            t.r = []
        return tok

    def barrier(self, exclude=()):
        toks = []
        for e in ENGS:
            if self.ops[e]:
                toks.append(("c", e, len(self.ops[e]) - 1))
        for g in self.grps:
            if g.n and not any(g is x for x in exclude):
                toks.append(("d", g, g.n))
        for e in ENGS:
            self.extra[e].extend(toks)

    def finalize(self):
        for e in ENGS:
            known_c = {}
            known_d = {}
            for i, o in enumerate(self.ops[e]):
                waits = []
                for d in o.deps:
                    if d[0] == "c":
                        _, e2, j = d
                        if e2 == e and (e == "tensor" or j < i - 2):
                            continue
                        if known_c.get(e2, -1) >= j:
                            continue
                        known_c[e2] = j
                        waits.append(d)
                    else:
                        _, g, n = d
                        if known_d.get(id(g), 0) >= n:
                            continue
                        known_d[id(g)] = n
                        waits.append(d)
                o.waits = waits
        for e in ENGS:
            for o in self.ops[e]:
                for d in o.waits:
                    if d[0] == "c":
                        self.ops[d[1]][d[2]].signal = True
        for e in ENGS:
            c = 0
            for o in self.ops[e]:
                if o.signal and o.grp is None:
                    c += 1
                    o.count = c

    def emit_engine(self, e, eng, sems):
        for o in self.ops[e]:
            for d in o.waits:
                if d[0] == "c":
                    tgt = self.ops[d[1]][d[2]]
                    if tgt.count is None:
                        continue
                    eng.wait_ge(sems[d[1]], tgt.count)
                else:
                    eng.wait_ge(d[1].sem, 16 * d[2])
            ins = o.fn(eng)
            if o.grp is not None:
                ins.then_inc(o.grp.sem, 16)
            elif o.signal:
                ins.then_inc(sems[e], 1)


def _dsize(dt):
    return 2 if dt == BF16 else 4


def build(T, phase1_only=False, debug=False, stop=None):
    NB = T // 128
    NS = T // TT
    NBLK = -(-2 * T // BLK) + NE
    NSLOT = NBLK * BLK
    nc = bass.Bass("TRN2", target_bir_lowering=False)
    P = Prog(nc)

    def din(name, shape, dt=F32):
        return nc.dram_tensor(name, shape, dt, kind="ExternalInput").ap()

    x = din("x", [T, D])
    c_col = din("c_col", [128, 8])
    w_ada = din("w_ada", [D, 6 * D])
    b_ada = din("b_ada", [1, 6 * D])
    gmix = din("gmix", [1, D])
    gffn = din("gffn", [1, D])
    w_in = din("w_in", [D, DIN])
    kern = din("kern", [128, 4, 31])
    cvec = din("cvec", [128, 4, 3])
    wco = din("wco", [512, D])
    wao = din("wao", [512, D])
    wout = din("wout", [D, D])
    gq = din("gq", [1, 64])
    gk = din("gk", [1, 64])
    sinks = din("sinks", [1, 8])
    bm = din("bm", [128, 8, 2, 128])
    wr = din("wr", [D, 36])
    br = din("br", [1, 36])
    weg = din("weg", [NE, D, DE])
    weu = din("weu", [NE, D, DE])
    wed = din("wed", [NE, DE, D])
    out = nc.dram_tensor("out", [T, D], F32, kind="ExternalOutput").ap()
    H2 = nc.dram_tensor("h2_scr", [T, D], BF16, kind="Internal").ap()
    Ldram = nc.dram_tensor("l_scr", [128, NB, 36], F32, kind="Internal").ap()
    G2d = nc.dram_tensor("g2_scr", [128, D], F32, kind="Internal").ap()
    WGU = nc.dram_tensor("wgu_scr", [NE * 128, 8, 2 * DE], BF16, kind="Internal").ap()
    WDS = nc.dram_tensor("wd_scr", [NE * 128, 2, D], BF16, kind="Internal").ap()
    XS = nc.dram_tensor("xs_scr", [NSLOT, D], BF16, kind="Internal").ap()
    YS = nc.dram_tensor("ys_scr", [NSLOT, D], F32, kind="Internal").ap()
    if debug:
        Ldbg = nc.dram_tensor("Ldbg", [128, NB, 36], F32, kind="ExternalOutput").ap()
        Ddbg = nc.dram_tensor("Ddbg", [128, 4, NB], F32, kind="ExternalOutput").ap()

    off = [16640]
    LIMIT = 229376 - 512

    def sb(name, shape, dt):
        nbytes = int(np.prod(shape[1:])) * _dsize(dt)
        nbytes = (nbytes + 63) // 64 * 64
        t = nc.alloc_sbuf_tensor_at(name, list(shape), dt, offset=off[0])
        off[0] += nbytes
        assert off[0] <= LIMIT, f"SBUF overflow at {name}: {off[0]}"
        return Tl(name, t)

    def ps(name, shape, dt):
        return Tl(name, nc.alloc_psum_tensor(name, list(shape), dt))

    tp = [ps(f"tp{i}", [128, 1024], BF16) for i in range(2)]
    mmb = [ps(f"mm{i}", [128, 512], F32) for i in range(4)]
    attA = ps("attA", [128, 512], F32)
    attB = ps("attB", [128, 512], F32)
    tpi = [0]
    mmi = [0]

    def gtp():
        tpi[0] += 1
        return tp[tpi[0] % 2]

    def gmm():
        mmi[0] += 1
        return mmb[mmi[0] % 4]

    def gmm_ab():
        mmi[0] += 1
        return mmb[mmi[0] % 2]

    def gmm_g():
        mmi[0] += 1
        return mmb[2 + mmi[0] % 2]

    def E(eng, fn, reads, writes, *a, **k):
        return P.op(eng, lambda e: getattr(e, fn)(*a, **k), reads, writes)

    def V(fn, reads, writes, *a, **k):
        return E("vector", fn, reads, writes, *a, **k)

    def A(fn, reads, writes, *a, **k):
        return E("scalar", fn, reads, writes, *a, **k)

    def G(fn, reads, writes, *a, **k):
        return E("gpsimd", fn, reads, writes, *a, **k)

    def MM(o_tl, o_ap, l_tl, l_ap, r_tl, r_ap, start, stop):
        return P.op("tensor", lambda e: e.matmul(o_ap, lhsT=l_ap, rhs=r_ap, start=start, stop=stop),
                    [l_tl, r_tl], [o_tl])

    def TR(o_tl, o_ap, i_tl, i_ap, id_tl):
        return P.op("tensor", lambda e: e.transpose(out=o_ap, in_=i_ap, identity=id_tl[:]),
                    [i_tl, id_tl], [o_tl])

    def load(q, tl, o_ap, i_ap):
        if tl.lg is None:
            tl.lg = P.grp("l_" + tl.name)
        return P.op(q, lambda e: e.dma_start(out=o_ap, in_=i_ap), [], [tl], grp=tl.lg)

    def store(q, tl, o_ap, i_ap):
        if tl.sg is None:
            tl.sg = P.grp("s_" + tl.name)
        return P.op(q, lambda e: e.dma_start(out=o_ap, in_=i_ap), [tl], [], grp=tl.sg)

    mod = sb("mod", [128, 4 * D], F32)
    B1, A1 = mod[:, 0:D], mod[:, D:2 * D]
    B2, A2 = mod[:, 2 * D:3 * D], mod[:, 3 * D:4 * D]
    ident_f = sb("ident_f", [128, 128], F32)
    ident_b = sb("ident_b", [128, 128], BF16)
    nhalf = sb("nhalf", [128, 1], F32)
    persist_end = off[0]

    G("memset", [], [ident_f], ident_f[:], 1.0)
    G("affine_select", [ident_f], [ident_f], out=ident_f[:], in_=ident_f[:], pattern=[[-1, 128]],
      compare_op=ALU.is_equal, fill=0.0, base=0, channel_multiplier=1)
    V("tensor_copy", [ident_f], [ident_b], out=ident_b[:], in_=ident_f[:])
    G("memset", [], [nhalf], nhalf[:], -0.5)

    w_in_sb = sb("w_in_sb", [128, 8, DIN], BF16)
    wco_sb = sb("wco_sb", [128, 4, D], BF16)
    wao_sb = sb("wao_sb", [128, 4, D], BF16)
    wout_sb = sb("wout_sb", [128, 8, D], BF16)
    kern_sb = sb("kern_sb", [128, 4, 31], F32)
    cvec_sb = sb("cvec_sb", [128, 4, 3], F32)
    bm_sb = sb("bm_sb", [128, 8, 2, 128], F32)
    wr_sb = sb("wr_sb", [128, 8, 36], F32)
    brb = sb("brb", [128, 36], F32)
    gqb = sb("gqb", [128, 64], F32)
    gkb = sb("gkb", [128, 64], F32)
    gq8 = sb("gq8", [128, 8, 64], F32)
    gk2 = sb("gk2", [128, 2, 64], F32)
    snk = sb("snk", [128, 8], F32)
    esink = sb("esink", [128, 8], F32)
    negc = sb("negc", [128, 1], F32)
    mq = sb("mq", [128, 1], F32)
    mk = sb("mk", [128, 1], F32)
    onesdiv = sb("onesdiv", [128, 128], BF16)

    cgrp = P.grp("wcast")

    def expert_casts(e_):
        rws = slice(e_ * 128, (e_ + 1) * 128)
        P.op("gpsimd", lambda e: e.dma_start(
            out=WGU[rws, :, 0:DE], in_=weg[e_, :, :].rearrange("(p kc) f -> p kc f", p=128)), [], [], grp=cgrp)
        P.op("gpsimd", lambda e: e.dma_start(
            out=WGU[rws, :, DE:2 * DE], in_=weu[e_, :, :].rearrange("(p kc) f -> p kc f", p=128)), [], [], grp=cgrp)
        P.op("gpsimd", lambda e: e.dma_start(
            out=WDS[rws, :, :], in_=wed[e_, :, :].rearrange("(p kc) n -> p kc n", p=128)), [], [], grp=cgrp)
    load("sync", kern_sb, kern_sb[:], kern[:, :, :])
    load("sync", cvec_sb, cvec_sb[:], cvec[:, :, :])
    load("sync", bm_sb, bm_sb[:], bm[:, :, :, :])
    load("sync", wr_sb, wr_sb[:], wr.rearrange("(kc p) n -> p kc n", p=128))
    load("sync", brb, brb[:], br[0, :].partition_broadcast(128))
    load("sync", gqb, gqb[:], gq[0, :].partition_broadcast(128))
    load("sync", gkb, gkb[:], gk[0, :].partition_broadcast(128))
    load("sync", snk, snk[:], sinks[0, :].partition_broadcast(128))
    resident_end = off[0]
    stgw = [sb(f"stgw{i}", [128, 4096], F32) for i in range(2)]
    w_in_v = w_in.rearrange("(kc p) n -> p kc n", p=128)
    wco_v = wco.rearrange("(kc p) n -> p kc n", p=128)
    wao_v = wao.rearrange("(kc p) n -> p kc n", p=128)
    wout_v = wout.rearrange("(kc p) n -> p kc n", p=128)
    jobs = []
    for kc in range(8):
        jobs.append((w_in_v[:, kc, :], w_in_sb, w_in_sb[:, kc, :], 3840))
    jobs.append((wco_v, wco_sb, wco_sb[:], 4096))
    jobs.append((wao_v, wao_sb, wao_sb[:], 4096))
    for hh_ in range(2):
        jobs.append((wout_v[:, hh_ * 4:(hh_ + 1) * 4, :], wout_sb, wout_sb[:, hh_ * 4:(hh_ + 1) * 4, :], 4096))
    for ji, (src, dtl, dap, nel) in enumerate(jobs):
        st_ = stgw[ji % 2]
        sview = st_[:, 0:nel]
        if len(src.shape) == 3:
            sview = sview.rearrange("p (k n) -> p k n", k=src.shape[1])
        load("scalar", st_, sview, src)
        if ji % 2 == 0:
            V("tensor_copy", [st_], [dtl], out=dap, in_=sview)
        else:
            A("copy", [st_], [dtl], out=dap, in_=sview)

    csb = sb("csb", [128, 8], F32)
    scs = sb("scs", [128, 8], F32)
    scb = sb("scb", [128, 8, 128], F32)
    gmb = sb("gmb", [128, D], F32)
    gfb = sb("gfb", [128, D], F32)
    wa = [sb(f"wa{i}", [128, 8, 256], F32) for i in range(2)]
    modT = sb("modT", [128, 6 * D], F32)
    G1 = modT[:, 2 * D:3 * D]
    load("sync", csb, csb[:], c_col[:, :])
    load("sync", modT, modT[:], b_ada[0, :].partition_broadcast(128))
    load("sync", gmb, gmb[:], gmix[0, :].partition_broadcast(128))
    load("sync", gfb, gfb[:], gffn[0, :].partition_broadcast(128))
    A("activation", [csb], [scs], out=scs[:], in_=csb[:], func=ACTF.Silu)
    for kc in range(8):
        V("tensor_copy", [scs], [scb], out=scb[:, kc, :], in_=scs[:, kc:kc + 1].to_broadcast([128, 128]))
    w_ada_v = w_ada.rearrange("(kc p) n -> p kc n", p=128)
    for n in range(24):
        wt = wa[n % 2]
        load("sync", wt, wt[:], w_ada_v[:, :, n * 256:(n + 1) * 256])
        pm = gmm()
        for kc in range(8):
            MM(pm, pm[:, 0:256], scb, scb[:, kc, :], wt, wt[:, kc, :], kc == 0, kc == 7)
        V("tensor_tensor", [pm, modT], [modT], out=modT[:, n * 256:(n + 1) * 256], in0=pm[:, 0:256],
          in1=modT[:, n * 256:(n + 1) * 256], op=ALU.add)
    V("scalar_tensor_tensor", [modT, gmb], [modT], out=modT[:, D:2 * D], in0=modT[:, D:2 * D], scalar=1.0, in1=gmb[:],
      op0=ALU.add, op1=ALU.mult)
    V("scalar_tensor_tensor", [modT, gfb], [modT], out=modT[:, 4 * D:5 * D], in0=modT[:, 4 * D:5 * D], scalar=1.0,
      in1=gfb[:], op0=ALU.add, op1=ALU.mult)
    V("tensor_copy", [modT], [mod], out=mod[:, 0:2 * D], in_=modT[:, 0:2 * D])
    V("tensor_copy", [modT], [mod], out=mod[:, 2 * D:4 * D], in_=modT[:, 3 * D:5 * D])
    for kc in range(8):
        V("scalar_tensor_tensor", [wout_sb, modT], [wout_sb], out=wout_sb[:, kc, :], in0=wout_sb[:, kc, :], scalar=0.5,
          in1=G1, op0=ALU.mult, op1=ALU.mult)
    store("sync", modT, G2d[:, :], modT[:, 5 * D:6 * D])
    P.barrier()
    off[0] = resident_end
    NS_run = NS
    if stop == "p0":
        NS_run = 0

    G("memset", [], [onesdiv], onesdiv[:], 1.0 / 512.0)
    V("tensor_scalar", [gqb], [gq8], out=gq8[:], in0=gqb[:, None, :].to_broadcast([128, 8, 64]),
      scalar1=0.125, scalar2=None, op0=ALU.mult)
    V("tensor_copy", [gkb], [gk2], out=gk2[:], in_=gkb[:, None, :].to_broadcast([128, 2, 64]))
    V("reduce_max", [gqb], [mq], out=mq[:], in_=gqb[:], axis=AX.X, apply_absolute_value=True)
    V("reduce_max", [gkb], [mk], out=mk[:], in_=gkb[:], axis=AX.X, apply_absolute_value=True)
    V("scalar_tensor_tensor", [mq, mk], [negc], out=negc[:], in0=mq[:], scalar=-8.0, in1=mk[:],
      op0=ALU.mult, op1=ALU.mult)
    A("activation", [snk, negc], [esink], out=esink[:], in_=snk[:], func=ACTF.Exp, bias=negc[:, 0:1])

    xin = [sb(f"xin{i}", [128, D], F32) for i in range(2)]
    xr = [sb(f"xr{i}", [128, D], F32) for i in range(2)]
    tmpf = sb("tmpf", [128, D], F32)
    h2f = tmpf
    Lrow = [sb(f"Lrow{i}", [128, 36], F32) for i in range(2)]
    h2Tt = sb("h2Tt", [128, 1024], F32)
    hb = [sb(f"hb{i}", [128, D], BF16) for i in range(1)]
    h2b = [sb(f"h2b{i}", [128, D], BF16) for i in range(1)]
    hTs = [sb(f"hT{i}", [128, 8, TT], BF16) for i in range(2)]
    ubuf = sb("ubuf", [128, 4, TT + 30], F32)
    ub3 = sb("ub3", [128, TT + 30], BF16)
    dg = [sb(f"dg{i}", [128, 128], BF16) for i in range(4)]
    dgi = [0]
    acc = [sb(f"acc{c}", [128, TT], F32) for c in range(4)]
    cbf = sb("cbf", [128, 4, TT], BF16)
    sqb = sb("sqb", [128, 4, TT], BF16)
    uT = sb("uT", [128, 4, TT], BF16)
    oT = sb("oT", [128, 4, TT], BF16)
    mT = sb("mT", [128, 8, TT], BF16)
    sgc = sb("sgc", [128, 8, TT], BF16)
    sga = sb("sga", [128, 8, TT], BF16)
    fgA = mmb[2]
    fgB = mmb[2]
    sgb = [sb(f"sgb{i}", [128, TT], F32) for i in range(1)] * 2
    s1 = [sb(f"s1_{i}", [128, TT], F32) for i in range(2)]
    s2 = [sb(f"s2_{i}", [128, TT], F32) for i in range(2)]
    t1 = s1
    t2 = s2
    mean_sb = s1[0]
    m2 = s2[0]
    var = m2
    rln = m2
    ssq = sb("ssq", [128, 1], F32)
    msq = sb("msq", [128, 1], F32)
    rstd = sb("rstd", [128, 1], F32)
    ssq2 = sb("ssq2", [128, 1], F32)
    msq2 = sb("msq2", [128, 1], F32)
    rstd2 = sb("rstd2", [128, 1], F32)
    ssq10 = sb("ssq10", [128, 10], F32)
    ms10 = sb("ms10", [128, 10], F32)
    rs10 = sb("rs10", [128, 10], F32)
    qn = sb("qn", [128, 512], BF16)
    kpad = sb("kpad", [128, 2, 2, 128], BF16)
    vaug = [sb(f"vaug{i}", [128, 2, 65], BF16) for i in range(2)]
    qT = sb("qT", [128, 4, 128], BF16)
    kT = [sb(f"kT{i}", [128, 4, 128], BF16) for i in range(2)]
    lgT = sb("lgT", [128, 1024], F32)
    PT = sb("PT", [128, 4, 2, 128], BF16)
    den = sb("den", [128, 4], F32)
    rden = sb("rden", [128, 4], F32)
    onb = sb("onb", [128, 512], BF16)
    phase1_end = off[0]

    for i in range(2):
        G("memset", [], [vaug[i]], vaug[i][:], 1.0)
    G("memset", [], [kpad], kpad[:], 0.0)

    def stageA(s):
        hT = hTs[s % 2]
        for j in range(NBS):
            blk = s * NBS + j
            xi = xin[blk % 2]
            load("sync", xi, xi[:], x[blk * 128:(blk + 1) * 128, :])
            A("activation", [xi], [hb[0], ssq], out=hb[0][:], in_=xi[:], func=ACTF.Square, accum_out=ssq[:, 0:1])
            V("tensor_scalar", [ssq], [msq], out=msq[:], in0=ssq[:], scalar1=1.0 / D, scalar2=EPS,
              op0=ALU.mult, op1=ALU.add)
            G("tensor_tensor", [msq, nhalf], [rstd], out=rstd[:], in0=msq[:], in1=nhalf[:], op=ALU.pow)
            V("scalar_tensor_tensor", [xi, rstd, mod], [tmpf], out=tmpf[:], in0=xi[:], scalar=rstd[:, 0:1],
              in1=A1, op0=ALU.mult, op1=ALU.mult)
            hbt = hb[0]
            V("tensor_tensor", [tmpf, mod], [hbt], out=hbt[:], in0=tmpf[:], in1=B1, op=ALU.add)
            pt = gtp()
            for kc in range(8):
                TR(pt, pt[:, kc * 128:(kc + 1) * 128], hbt, hbt[:, kc * 128:(kc + 1) * 128], ident_b)
            A("copy", [pt], [hT], out=hT[:, :, j * 128:(j + 1) * 128],
              in_=pt[:].rearrange("p (k t) -> p k t", k=8))

    def stageB(s):
        hT = hTs[s % 2]
        if s == 0:
            G("memset", [], [ubuf], ubuf[:, :, 0:30], 0.0)
            G("memset", [], [ub3], ub3[:, 0:30], 0.0)
        else:
            A("copy", [ubuf], [ubuf], out=ubuf[:, 0:3, 0:30], in_=ubuf[:, 0:3, TT:TT + 30])
            A("copy", [ub3], [ub3], out=ub3[:, 0:30], in_=ub3[:, TT:TT + 30])
        for c in range(4):
            pa = gmm_ab()
            for kc in range(8):
                MM(pa, pa[:, 0:TT], w_in_sb, w_in_sb[:, kc, c * 128:(c + 1) * 128], hT, hT[:, kc, :], kc == 0, kc == 7)
            pb = gmm_ab()
            for kc in range(8):
                MM(pb, pb[:, 0:TT], w_in_sb, w_in_sb[:, kc, 512 + c * 128:512 + (c + 1) * 128], hT, hT[:, kc, :],
                   kc == 0, kc == 7)
            sg = sgb[c % 2]
            A("activation", [pb], [sg], out=sg[:], in_=pb[:, 0:TT], func=ACTF.Sigmoid)
            if c == 3:
                V("tensor_tensor", [pa, sg], [ub3], out=ub3[:, 30:30 + TT], in0=pa[:, 0:TT], in1=sg[:], op=ALU.mult)
            else:
                V("tensor_tensor", [pa, sg], [ubuf], out=ubuf[:, c, 30:30 + TT], in0=pa[:, 0:TT], in1=sg[:], op=ALU.mult)

    def stageC(s):
        bank = mmb[1]
        for tap in range(31):
            dgt = dg[dgi[0] % 4]
            dgi[0] += 1
            A("activation", [ident_f, kern_sb], [dgt], out=dgt[:], in_=ident_f[:], func=ACTF.Identity,
              scale=kern_sb[:, 3, tap:tap + 1])
            MM(bank, bank[:, 0:TT], dgt, dgt[:], ub3, ub3[:, tap:tap + TT], tap == 0, tap == 30)
        A("activation", [bank, cvec_sb], [acc[3]], out=acc[3][:], in_=bank[:, 0:TT], func=ACTF.Identity,
          bias=cvec_sb[:, 3, 0:1])
        pe_part = P.defer
        P.defer = []
        for c in range(3):
            V("tensor_scalar", [ubuf, kern_sb, cvec_sb], [acc[c]], out=acc[c][:], in0=ubuf[:, c, 0:TT],
              scalar1=kern_sb[:, c, 0:1], scalar2=cvec_sb[:, c, 0:1], op0=ALU.mult, op1=ALU.add)
        for tap in range(1, 31):
            for c in range(3):
                V("scalar_tensor_tensor", [ubuf, kern_sb, acc[c]], [acc[c]], out=acc[c][:],
                  in0=ubuf[:, c, tap:tap + TT], scalar=kern_sb[:, c, tap:tap + 1], in1=acc[c][:],
                  op0=ALU.mult, op1=ALU.add)
        dve_part = P.defer
        P.defer = merge(pe_part, dve_part)

    def stageD(s):
        sqv = sqb
        for c in range(4):
            A("copy", [acc[c]], [cbf], out=cbf[:, c, :], in_=acc[c][:])
            A("activation", [acc[c]], [sqb], out=sqv[:, c, :], in_=acc[c][:], func=ACTF.Square)
        pmn = mmb[1]
        for c in range(4):
            MM(pmn, pmn[:, 0:TT], onesdiv, onesdiv[:], cbf, cbf[:, c, :], c == 0, c == 3)
        pq2 = mmb[1]
        for c in range(4):
            MM(pq2, pq2[:, TT:2 * TT], onesdiv, onesdiv[:], sqb, sqv[:, c, :], c == 0, c == 3)
        A("copy", [pmn], [mean_sb], out=mean_sb[:], in_=pmn[:, 0:TT])
        V("tensor_tensor", [mean_sb], [m2], out=m2[:], in0=mean_sb[:], in1=mean_sb[:], op=ALU.mult)
        V("scalar_tensor_tensor", [pq2, m2], [m2], out=var[:], in0=pq2[:, TT:2 * TT], scalar=EPS, in1=m2[:],
          op0=ALU.add, op1=ALU.subtract)
        A("sqrt", [m2], [m2], out=m2[:], in_=m2[:])
        V("reciprocal", [m2], [m2], out=m2[:], in_=m2[:])
        for c in range(4):
            V("tensor_tensor", [acc[c], mean_sb], [acc[c]], out=acc[c][:], in0=acc[c][:], in1=mean_sb[:],
              op=ALU.subtract)
        for c in range(4):
            V("tensor_tensor", [acc[c], rln], [acc[c]], out=acc[c][:], in0=acc[c][:], in1=rln[:], op=ALU.mult)
        for c in range(4):
            A("activation", [acc[c], cvec_sb], [uT], out=uT[:, c, :], in_=acc[c][:], func=ACTF.Silu,
              bias=cvec_sb[:, c, 2:3], scale=cvec_sb[:, c, 1:2])

    def stageE(s):
        hT = hTs[s % 2]
        for j in range(NBS):
            blk = s * NBS + j
            tok = slice(j * 128, (j + 1) * 128)
            for kc in range(8):
                MM(attA, attA[:, 0:512], hT, hT[:, kc, tok], w_in_sb, w_in_sb[:, kc, 1024:1536], kc == 0, kc == 7)
            for kc in range(8):
                MM(attB, attB[:, 0:256], hT, hT[:, kc, tok], w_in_sb, w_in_sb[:, kc, 1536:1792], kc == 0, kc == 7)
            A("activation", [attA], [tmpf], out=tmpf[:, 0:512], in_=attA[:, 0:512], func=ACTF.Square)
            A("activation", [attB], [tmpf], out=tmpf[:, 512:640], in_=attB[:, 0:128], func=ACTF.Square)
            V("tensor_reduce", [tmpf], [ssq10], out=ssq10[:], in_=tmpf[:, 0:640].rearrange("p (h d) -> p h d", d=64),
              axis=AX.X, op=ALU.add)
            V("tensor_scalar", [ssq10], [ms10], out=ms10[:], in0=ssq10[:], scalar1=1.0 / 64, scalar2=EPS,
              op0=ALU.mult, op1=ALU.add)
            G("tensor_tensor", [ms10, nhalf], [rs10], out=rs10[:], in0=ms10[:],
              in1=nhalf[:, 0:1].to_broadcast([128, 10]), op=ALU.pow)
            V("tensor_tensor", [attA, rs10, ssq10], [tmpf], out=tmpf[:, 0:512].rearrange("p (h d) -> p h d", d=64), in0=attA[:, 0:512].rearrange("p (h d) -> p h d", d=64),
              in1=rs10[:, 0:8, None].to_broadcast([128, 8, 64]), op=ALU.mult)
            V("tensor_tensor", [tmpf, gq8], [qn], out=qn[:].rearrange("p (h d) -> p h d", d=64), in0=tmpf[:, 0:512].rearrange("p (h d) -> p h d", d=64),
              in1=gq8[:], op=ALU.mult)
            V("tensor_tensor", [attB, rs10], [tmpf], out=tmpf[:, 512:640].rearrange("p (h d) -> p h d", d=64), in0=attB[:, 0:128].rearrange("p (h d) -> p h d", d=64),
              in1=rs10[:, 8:10, None].to_broadcast([128, 2, 64]), op=ALU.mult)
            for dd in range(2):
                V("tensor_tensor", [tmpf, gk2], [kpad], out=kpad[:, :, dd, dd * 64:(dd + 1) * 64],
                  in0=tmpf[:, 512:640].rearrange("p (h d) -> p h d", d=64), in1=gk2[:], op=ALU.mult)
            va = vaug[blk % 2]
            A("copy", [attB], [va], out=va[:, :, 0:64], in_=attB[:, 128:256].rearrange("p (h d) -> p h d", d=64))
            pt = gtp()
            for c in range(4):
                TR(pt, pt[:, c * 128:(c + 1) * 128], qn, qn[:, c * 128:(c + 1) * 128], ident_b)
            kflat = kpad[:].rearrange("p a b d -> p (a b d)")
            for kv in range(4):
                TR(pt, pt[:, (4 + kv) * 128:(5 + kv) * 128], kpad, kflat[:, kv * 128:(kv + 1) * 128], ident_b)
            kTc = kT[blk % 2]
            kTp = kT[(blk - 1) % 2]
            A("copy", [pt], [qT], out=qT[:], in_=pt[:, 0:512].rearrange("p (c t) -> p c t", c=4))
            A("copy", [pt], [kTc], out=kTc[:], in_=pt[:, 512:1024].rearrange("p (c t) -> p c t", c=4))
            js = [1] if blk == 0 else [0, 1]
            jsl = slice(js[0], 2)
            attv = [(attA, attA[:].rearrange("p (h j q) -> p h j q", h=2, j=2)),
                    (attB, attB[:].rearrange("p (h j q) -> p h j q", h=2, j=2))]
            lg4 = lgT[:].rearrange("p (h j q) -> p h j q", h=4, j=2)
            for g in range(2):
                for hh in range(4):
                    h = 4 * g + hh
                    c, r = h // 2, h % 2
                    for jj in js:
                        kTt = kTp if jj == 0 else kTc
                        at_, av_ = attv[hh // 2]
                        MM(at_, av_[:, hh % 2, jj, :], kTt, kTt[:, 2 * g + r, :], qT, qT[:, c, :], True, True)
                for hp in range(2):
                    at_, av_ = attv[hp]
                    V("tensor_tensor", [at_, bm_sb], [lgT], out=lg4[:, 2 * hp:2 * hp + 2, jsl, :], in0=av_[:, :, jsl, :],
                      in1=bm_sb[:, 4 * g + 2 * hp:4 * g + 2 * hp + 2, jsl, :], op=ALU.add)
                A("activation", [lgT, negc], [PT], out=PT[:, :, jsl, :], in_=lg4[:, :, jsl, :], func=ACTF.Exp,
                  bias=negc[:, 0:1])
                po = mmb[0]
                po3 = po[:, 0:260].rearrange("p (h e) -> p h e", e=65)
                vp = vaug[(blk - 1) % 2]
                for hh in range(4):
                    for jj in js:
                        vt = vp if jj == 0 else va
                        MM(po, po3[:, hh, :], PT, PT[:, hh, jj, :], vt, vt[:, g, :], jj == js[0], jj == js[-1])
                V("tensor_tensor", [po, esink], [den], out=den[:], in0=po3[:, :, 64], in1=esink[:, 4 * g:4 * g + 4],
                  op=ALU.add)
                V("reciprocal", [den], [rden], out=rden[:], in_=den[:])
                V("tensor_tensor", [po, rden], [onb],
                  out=onb[:].rearrange("p (h d) -> p h d", d=64)[:, 4 * g:4 * g + 4, :], in0=po3[:, :, 0:64],
                  in1=rden[:, :, None].to_broadcast([128, 4, 64]), op=ALU.mult)
            pt = gtp()
            for c in range(4):
                TR(pt, pt[:, c * 128:(c + 1) * 128], onb, onb[:, c * 128:(c + 1) * 128], ident_b)
            A("copy", [pt], [oT], out=oT[:, :, tok], in_=pt[:, 0:512].rearrange("p (c t) -> p c t", c=4))

    def stageFg(s):
        hT = hTs[s % 2]
        for mc in range(8):
            for kc in range(8):
                MM(fgA, fgA[:, 0:TT], w_in_sb, w_in_sb[:, kc, 1792 + mc * 128:1792 + (mc + 1) * 128], hT, hT[:, kc, :],
                   kc == 0, kc == 7)
            A("activation", [fgA], [sgc], out=sgc[:, mc, :], in_=fgA[:, 0:TT], func=ACTF.Tanh, scale=0.5)
            for kc in range(8):
                MM(fgB, fgB[:, TT:2 * TT], w_in_sb, w_in_sb[:, kc, 2816 + mc * 128:2816 + (mc + 1) * 128], hT, hT[:, kc, :],
                   kc == 0, kc == 7)
            A("activation", [fgB], [sga], out=sga[:, mc, :], in_=fgB[:, TT:2 * TT], func=ACTF.Tanh, scale=0.5)

    def stageF(s):
        for mc in range(8):
            ms_ = slice(mc * 128, (mc + 1) * 128)
            i2 = mc % 2
            bx, by = (mmb[2], mmb[3]) if i2 == 0 else (attA, attB)
            for kc in range(4):
                MM(bx, bx[:, 0:TT], wco_sb, wco_sb[:, kc, ms_], uT, uT[:, kc, :], kc == 0, kc == 3)
            V("scalar_tensor_tensor", [sgc, bx], [s1[i2]], out=s1[i2][:], in0=sgc[:, mc, :], scalar=1.0, in1=bx[:, 0:TT],
              op0=ALU.add, op1=ALU.mult)
            for kc in range(4):
                MM(by, by[:, 0:TT], wao_sb, wao_sb[:, kc, ms_], oT, oT[:, kc, :], kc == 0, kc == 3)
            V("scalar_tensor_tensor", [sga, by], [s2[i2]], out=s2[i2][:], in0=sga[:, mc, :], scalar=1.0, in1=by[:, 0:TT],
              op0=ALU.add, op1=ALU.mult)
            V("tensor_tensor", [s1[i2], s2[i2]], [mT], out=mT[:, mc, :], in0=s1[i2][:], in1=s2[i2][:], op=ALU.add)

    def stageG(s):
        for j in range(NBS):
            blk = s * NBS + j
            tok = slice(j * 128, (j + 1) * 128)
            rows = slice(blk * 128, (blk + 1) * 128)
            xrt = xr[blk % 2]
            load("sync", xrt, xrt[:], x[rows, :])
            for nh in range(2):
                cs = slice(nh * 512, (nh + 1) * 512)
                pp = mmb[3]
                for kc in range(8):
                    MM(pp, pp[:], mT, mT[:, kc, tok], wout_sb, wout_sb[:, kc, cs], kc == 0, kc == 7)
                V("tensor_tensor", [pp, xrt], [xrt], out=xrt[:, cs], in0=pp[:], in1=xrt[:, cs], op=ALU.add)
            store("gpsimd", xrt, out[rows, :], xrt[:])
            A("activation", [xrt], [h2b[0], ssq2], out=h2b[0][:], in_=xrt[:], func=ACTF.Square, accum_out=ssq2[:, 0:1])
            V("tensor_scalar", [ssq2], [msq2], out=msq2[:], in0=ssq2[:], scalar1=1.0 / D, scalar2=EPS,
              op0=ALU.mult, op1=ALU.add)
            G("tensor_tensor", [msq2, nhalf], [rstd2], out=rstd2[:], in0=msq2[:], in1=nhalf[:], op=ALU.pow)
            h2f = xrt
            V("scalar_tensor_tensor", [xrt, rstd2, mod], [xrt], out=xrt[:], in0=xrt[:], scalar=rstd2[:, 0:1],
              in1=A2, op0=ALU.mult, op1=ALU.mult)
            V("tensor_tensor", [xrt, mod], [xrt], out=xrt[:], in0=xrt[:], in1=B2, op=ALU.add)
            hbt = h2b[0]
            A("copy", [h2f], [hbt], out=hbt[:], in_=h2f[:])
            store("gpsimd", hbt, H2[rows, :], hbt[:])
            h2T3 = h2Tt[:].rearrange("p (k t) -> p k t", k=8)
            for hf in range(2):
                pp = mmb[3]
                for i in range(4):
                    kc = hf * 4 + i
                    TR(pp, pp[:, i * 128:(i + 1) * 128], h2f, h2f[:, kc * 128:(kc + 1) * 128], ident_f)
                if hf == 0:
                    A("copy", [pp], [h2Tt], out=h2T3[:, 0:4, :], in_=pp[:].rearrange("p (k t) -> p k t", k=4))
                else:
                    V("tensor_copy", [pp], [h2Tt], out=h2T3[:, 4:8, :], in_=pp[:].rearrange("p (k t) -> p k t", k=4))
            pp = mmb[3]
            for kc in range(8):
                MM(pp, pp[:, 0:36], h2Tt, h2T3[:, kc, :], wr_sb, wr_sb[:, kc, :], kc == 0, kc == 7)
            lr = Lrow[blk % 2]
            V("tensor_tensor", [pp, brb], [lr], out=lr[:], in0=pp[:, 0:36], in1=brb[:], op=ALU.add)
            store("gpsimd", lr, Ldram[:, blk, :], lr[:])
        if not phase1_only:
            for e_ in range(s * NE // NS, (s + 1) * NE // NS):
                expert_casts(e_)

    def run_stage(fn, s):
        P.defer = []
        fn(s)
        lst = P.defer
        P.defer = None
        return lst

    def merge(la, lb):
        res = []
        ia = ib = 0
        na, nb = len(la), len(lb)
        while ia < na or ib < nb:
            if ib >= nb or (ia < na and ia * nb <= ib * na):
                res.append(la[ia]); ia += 1
            else:
                res.append(lb[ib]); ib += 1
        return res

    def play(lst):
        for a in lst:
            P.op(*a)

    if NS_run:
        play(run_stage(stageA, 0) + run_stage(stageB, 0))
    for s in range(NS_run):
        eg = run_stage(stageE, s)
        if s > 0:
            eg = merge(eg, run_stage(stageG, s - 1))
        eg = merge(eg, run_stage(stageFg, s))
        play(merge(run_stage(stageC, s) + run_stage(stageD, s), eg))
        df = run_stage(stageF, s)
        if s + 1 < NS_run:
            df = merge(df, run_stage(stageA, s + 1) + run_stage(stageB, s + 1))
        play(df)
    if NS_run:
        play(run_stage(stageG, NS_run - 1))

    P.barrier()
    if debug:
        P.op("sync", lambda e: e.dma_start(out=Ldbg[:, :, :], in_=Ldram[:, :, :]), [], [], grp=P.grp("dbgL"))


    if not phase1_only:
        off[0] = persist_end
        KMAX = -(-T // BLK)
        w1 = sb("w1", [128, NB], F32)
        w2 = sb("w2", [128, NB], F32)
        d1i = sb("d1i", [128, NB], I32)
        d2i = sb("d2i", [128, NB], I32)
        blke_i = sb("blke_i", [128, NBLK], I32)
        Lt = sb("Lt", [128, NB, 36], F32)
        load("sync", Lt, Lt[:], Ldram[:, :, :])
        widx = sb("widx", [128, NBLK], I32)
        route_keep = off[0]
        ones_f = sb("ones_f", [128, 128], F32)
        ustr = sb("ustr", [128, 128], F32)
        gmax = sb("gmax", [128, NB], F32)
        ohg = sb("ohg", [128, NB, 4], F32)
        eg = sb("eg", [128, NB, 4], F32)
        sume = sb("sume", [128, NB], F32)
        ptop = sb("ptop", [128, NB], F32)
        tmp8 = sb("tmp8", [128, NB, 8], F32)
        elsel = sb("elsel", [128, NB, 8], F32)
        els2 = sb("els2", [128, NB, 8], F32)
        oh1 = sb("oh1", [128, NB, 8], F32)
        oh2 = sb("oh2", [128, NB, 8], F32)
        m1 = sb("m1", [128, NB], F32)
        m2v = sb("m2v", [128, NB], F32)
        ddv = sb("ddv", [128, NB], F32)
        e2v = sb("e2v", [128, NB], F32)
        OH1 = sb("OH1", [128, NB, 32], F32)
        OH2 = sb("OH2", [128, NB, 32], F32)
        TH = sb("TH", [128, NB, 32], F32)
        scn = [sb(f"scn{i}", [128, NB, 32], F32) for i in range(2)]
        cnt_p = sb("cnt_p", [128, 32], F32)
        tot_sb = sb("tot_sb", [128, 32], F32)
        pp_sb = sb("pp_sb", [128, 32], F32)
        thr_i = sb("thr_i", [128, KMAX], I32)
        thr_f = sb("thr_f", [128, KMAX], F32)
        cmpt = sb("cmpt", [128, 32, KMAX], F32)
        pc = sb("pc", [128, 32], F32)
        pe_ = [sb(f"pend{i}", [128, 32], F32) for i in range(2)]
        base = sb("base", [128, 32], F32)
        dst_f = sb("dst_f", [128, NB], F32)
        bthr_i = sb("bthr_i", [128, NBLK], I32)
        bthr_f = sb("bthr_f", [128, NBLK], F32)
        cmpb = sb("cmpb", [128, NBLK, 32], F32)
        blke_f = sb("blke_f", [128, NBLK], F32)
        hs = [sb(f"hs{i}", [128, D], BF16) for i in range(3)]
        pidx_i = sb("pidx_i", [128, 1], I32)
        pidx_f = sb("pidx_f", [128, 1], F32)
        widx_f = sb("widx_f", [128, NBLK], F32)

        G("memset", [], [ones_f], ones_f[:], 1.0)
        G("memset", [], [ustr], ustr[:], 1.0)
        G("affine_select", [ustr], [ustr], out=ustr[:], in_=ustr[:], pattern=[[1, 128]],
          compare_op=ALU.is_gt, fill=0.0, base=0, channel_multiplier=-1)
        G("iota", [], [thr_i], thr_i[:], pattern=[[BLK, KMAX]], base=0, channel_multiplier=0)
        G("iota", [], [bthr_i], bthr_i[:], pattern=[[BLK, NBLK]], base=0, channel_multiplier=0)
        V("tensor_copy", [thr_i], [thr_f], out=thr_f[:], in_=thr_i[:])
        V("tensor_copy", [bthr_i], [bthr_f], out=bthr_f[:], in_=bthr_i[:])

        gl = Lt[:, :, 0:4]
        el4 = Lt[:, :, 4:36].rearrange("p t (g e) -> p t g e", g=4)
        bc4 = lambda ap: ap[:, :, None].to_broadcast([128, NB, 4])
        bc8 = lambda ap: ap[:, :, None].to_broadcast([128, NB, 8])
        V("tensor_reduce", [Lt], [gmax], out=gmax[:], in_=gl, axis=AX.X, op=ALU.max)
        V("tensor_tensor", [Lt, gmax], [ohg], out=ohg[:], in0=gl, in1=bc4(gmax), op=ALU.is_equal)
        V("tensor_tensor", [Lt, gmax], [eg], out=eg[:], in0=gl, in1=bc4(gmax), op=ALU.subtract)
        A("activation", [eg], [eg], out=eg[:], in_=eg[:], func=ACTF.Exp)
        V("tensor_reduce", [eg], [sume], out=sume[:], in_=eg[:], axis=AX.X, op=ALU.add)
        V("reciprocal", [sume], [ptop], out=ptop[:], in_=sume[:])
        V("tensor_tensor", [Lt, ohg], [elsel], out=elsel[:], in0=el4[:, :, 0, :],
          in1=ohg[:, :, 0:1].to_broadcast([128, NB, 8]), op=ALU.mult)
        for g in range(1, 4):
            V("tensor_tensor", [Lt, ohg], [tmp8], out=tmp8[:], in0=el4[:, :, g, :],
              in1=ohg[:, :, g:g + 1].to_broadcast([128, NB, 8]), op=ALU.mult)
            V("tensor_tensor", [elsel, tmp8], [elsel], out=elsel[:], in0=elsel[:], in1=tmp8[:], op=ALU.add)
        V("tensor_reduce", [elsel], [m1], out=m1[:], in_=elsel[:], axis=AX.X, op=ALU.max)
        V("tensor_tensor", [elsel, m1], [oh1], out=oh1[:], in0=elsel[:], in1=bc8(m1), op=ALU.is_equal)
        V("scalar_tensor_tensor", [oh1, elsel], [els2], out=els2[:], in0=oh1[:], scalar=-1e30, in1=elsel[:],
          op0=ALU.mult, op1=ALU.add)
        V("tensor_reduce", [els2], [m2v], out=m2v[:], in_=els2[:], axis=AX.X, op=ALU.max)
        V("tensor_tensor", [els2, m2v], [oh2], out=oh2[:], in0=els2[:], in1=bc8(m2v), op=ALU.is_equal)
        V("tensor_tensor", [m2v, m1], [ddv], out=ddv[:], in0=m2v[:], in1=m1[:], op=ALU.subtract)
        A("activation", [ddv], [e2v], out=e2v[:], in_=ddv[:], func=ACTF.Exp)
        V("tensor_scalar", [e2v], [ddv], out=ddv[:], in0=e2v[:], scalar1=1.0, scalar2=None, op0=ALU.add)
        V("reciprocal", [ddv], [sume], out=sume[:], in_=ddv[:])
        V("tensor_tensor", [sume, ptop], [w1], out=w1[:], in0=sume[:], in1=ptop[:], op=ALU.mult)
        V("tensor_tensor", [w1, e2v], [w2], out=w2[:], in0=w1[:], in1=e2v[:], op=ALU.mult)
        for OHk, ohk in ((OH1, oh1), (OH2, oh2)):
            V("tensor_tensor", [ohg, ohk], [OHk], out=OHk[:].rearrange("p t (g e) -> p t g e", g=4),
              in0=ohg[:, :, :, None].to_broadcast([128, NB, 4, 8]),
              in1=ohk[:, :, None, :].to_broadcast([128, NB, 4, 8]), op=ALU.mult)
        V("tensor_tensor", [OH1, OH2], [TH], out=TH[:], in0=OH1[:], in1=OH2[:], op=ALU.add)
        V("tensor_reduce", [TH], [cnt_p], out=cnt_p[:], in_=TH[:].rearrange("p t e -> p e t"), axis=AX.X, op=ALU.add)
        pA = gmm()
        MM(pA, pA[:, 0:32], ustr, ustr[:], cnt_p, cnt_p[:], True, True)
        pB = gmm()
        MM(pB, pB[:, 0:32], ones_f, ones_f[:], cnt_p, cnt_p[:], True, True)
        A("copy", [pA], [pp_sb], out=pp_sb[:], in_=pA[:, 0:32])
        A("copy", [pB], [tot_sb], out=tot_sb[:], in_=pB[:, 0:32])
        V("tensor_tensor", [tot_sb, thr_f], [cmpt], out=cmpt[:], in0=tot_sb[:, :, None].to_broadcast([128, 32, KMAX]),
          in1=thr_f[:, None, :].to_broadcast([128, 32, KMAX]), op=ALU.is_gt)
        V("tensor_reduce", [cmpt], [pc], out=pc[:], in_=cmpt[:], axis=AX.X, op=ALU.add)
        V("tensor_scalar", [pc], [pc], out=pc[:], in0=pc[:], scalar1=float(BLK), scalar2=None, op0=ALU.mult)
        src = pc
        st = 1
        i = 0
        while st < 32:
            dstt = pe_[i % 2]
            V("tensor_tensor", [src], [dstt], out=dstt[:, st:], in0=src[:, st:], in1=src[:, 0:32 - st], op=ALU.add)
            V("tensor_copy", [src], [dstt], out=dstt[:, 0:st], in_=src[:, 0:st])
            src = dstt
            st *= 2
            i += 1
        pend = src
        V("tensor_tensor", [pend, pc], [base], out=base[:], in0=pend[:], in1=pc[:], op=ALU.subtract)
        V("tensor_tensor", [base, pp_sb], [base], out=base[:], in0=base[:], in1=pp_sb[:], op=ALU.add)
        src = TH
        st = 1
        i = 0
        while st < NB:
            dstt = scn[i % 2]
            V("tensor_tensor", [src], [dstt], out=dstt[:, st:, :], in0=src[:, st:, :], in1=src[:, 0:NB - st, :], op=ALU.add)
            G("tensor_copy", [src], [dstt], out=dstt[:, 0:st, :], in_=src[:, 0:st, :])
            src = dstt
            st *= 2
            i += 1
        pos = scn[i % 2] if src is not scn[i % 2] else scn[(i + 1) % 2]
        if src is TH:
            pos = scn[0]
        V("tensor_tensor", [src, TH], [pos], out=pos[:], in0=src[:], in1=TH[:], op=ALU.subtract)
        V("tensor_tensor", [pos, base], [pos], out=pos[:], in0=pos[:], in1=base[:, None, :].to_broadcast([128, NB, 32]),
          op=ALU.add)
        for OHk, dki in ((OH1, d1i), (OH2, d2i)):
            V("tensor_tensor", [OHk, pos], [OHk], out=OHk[:], in0=OHk[:], in1=pos[:], op=ALU.mult)
            V("tensor_reduce", [OHk], [dst_f], out=dst_f[:], in_=OHk[:], axis=AX.X, op=ALU.add)
            V("tensor_copy", [dst_f], [dki], out=dki[:], in_=dst_f[:])
        V("tensor_tensor", [pend, bthr_f], [cmpb], out=cmpb[:], in0=pend[:, None, :].to_broadcast([128, NBLK, 32]),
          in1=bthr_f[:, :, None].to_broadcast([128, NBLK, 32]), op=ALU.is_le)
        V("tensor_reduce", [cmpb], [blke_f], out=blke_f[:], in_=cmpb[:], axis=AX.X, op=ALU.add)
        V("tensor_scalar", [blke_f], [blke_f], out=blke_f[:], in0=blke_f[:], scalar1=float(NE - 1), scalar2=None,
          op0=ALU.min)
        V("tensor_copy", [blke_f], [blke_i], out=blke_i[:], in_=blke_f[:])
        G("iota", [], [pidx_i], pidx_i[:], pattern=[[0, 1]], base=0, channel_multiplier=1)
        V("tensor_copy", [pidx_i], [pidx_f], out=pidx_f[:], in_=pidx_i[:])
        V("tensor_scalar", [blke_f, pidx_f], [widx_f], out=widx_f[:], in0=blke_f[:], scalar1=128.0,
          scalar2=pidx_f[:, 0:1], op0=ALU.mult, op1=ALU.add)
        V("tensor_copy", [widx_f], [widx], out=widx[:], in_=widx_f[:])
        if debug:
            dbg = sb("dbg", [128, 4, NB], F32)
            V("tensor_copy", [w1], [dbg], out=dbg[:, 0, :], in_=w1[:])
            V("tensor_copy", [w2], [dbg], out=dbg[:, 1, :], in_=w2[:])
            V("tensor_copy", [d1i], [dbg], out=dbg[:, 2, :], in_=d1i[:])
            V("tensor_copy", [d2i], [dbg], out=dbg[:, 3, :], in_=d2i[:])
            store("sync", dbg, Ddbg[:, :, :], dbg[:])

        for t in range(0 if stop == "R" else NB):
            hst = hs[t % 3]
            load("sync", hst, hst[:], H2[t * 128:(t + 1) * 128, :])
            if hst.sg is None:
                hst.sg = P.grp("s_" + hst.name)
            for dki in (d1i, d2i):
                P.op("gpsimd", lambda e, hst=hst, dki=dki, t=t: e.indirect_dma_start(
                    out=XS[:, :], out_offset=bass.IndirectOffsetOnAxis(ap=dki[:, t:t + 1], axis=0),
                    in_=hst[:], in_offset=None), [hst, dki], [], grp=hst.sg)
        P.barrier()

        off[0] = route_keep
        g2t = sb("g2t", [128, D], F32)
        load("sync", g2t, g2t[:], G2d[:, :])
        Wgu = [sb(f"Wgu{i}", [128, 8, 2 * DE], BF16) for i in range(2)]
        Wd = [sb(f"Wd{i}", [128, 2, D], BF16) for i in range(2)]
        xs_in = [sb(f"xs_in{i}", [128, D], BF16) for i in range(3)]
        XTs = [sb(f"XT{i}", [128, 8, 128], BF16) for i in range(2)]
        sgls = [sb(f"sgl{i}", [128, DE], F32) for i in range(2)]
        hids = [sb(f"hid{i}", [128, DE], BF16) for i in range(2)]
        hidTs = [sb(f"hidT{i}", [128, 2, 128], BF16) for i in range(2)]
        ysb = [sb(f"ysb{i}", [128, D], F32) for i in range(3)]
        wgu_v = WGU.rearrange("r k f -> r (k f)")
        wds_v = WDS.rearrange("r k f -> r (k f)")

        def dyn_load(tl, src_v, b):
            if tl.lg is None:
                tl.lg = P.grp("l_" + tl.name)
            return P.op("gpsimd", lambda e: e.indirect_dma_start(
                out=tl[:].rearrange("p k f -> p (k f)"), out_offset=None, in_=src_v[:, :],
                in_offset=bass.IndirectOffsetOnAxis(ap=widx[:, b:b + 1], axis=0)), [widx], [tl], grp=tl.lg)

        def gathers(b):
            dyn_load(Wgu[b % 2], wgu_v, b)
            dyn_load(Wd[b % 2], wds_v, b)

        NBLK_run = 0 if stop in ("R", "S") else NBLK
        if NBLK_run:
            gathers(0)
        att_bf = [attA[:].bitcast(BF16), attB[:].bitcast(BF16)]
        att_tl = [attA, attB]

        def front(sl):
            b = sl // SUB
            i = b % 2
            k = sl % 2
            rows = slice(sl * 128, (sl + 1) * 128)
            xst = xs_in[sl % 3]
            XT, sgl, hid = XTs[k], sgls[k], hids[k]
            pt = tp[k]
            for kc in range(8):
                TR(pt, pt[:, kc * 128:(kc + 1) * 128], xst, xst[:].rearrange("p (f k) -> p k f", k=8)[:, kc, :], ident_b)
            A("copy", [pt], [XT], out=XT[:], in_=pt[:].rearrange("p (k t) -> p k t", k=8))
            pm = mmb[k]
            for kc in range(8):
                MM(pm, pm[:], XT, XT[:, kc, :], Wgu[i], Wgu[i][:, kc, :], kc == 0, kc == 7)
            A("activation", [pm], [sgl], out=sgl[:], in_=pm[:, 0:DE], func=ACTF.Silu)
            V("tensor_tensor", [pm, sgl], [hid], out=hid[:], in0=pm[:, DE:2 * DE], in1=sgl[:], op=ALU.mult)

        def back(sl):
            b = sl // SUB
            i = b % 2
            k = sl % 2
            rows = slice(sl * 128, (sl + 1) * 128)
            hid, hidT = hids[k], hidTs[k]
            pt2t, pt2 = att_tl[k], att_bf[k]
            for kc in range(2):
                TR(pt2t, pt2[:, kc * 128:(kc + 1) * 128], hid, hid[:].rearrange("p (f k) -> p k f", k=2)[:, kc, :], ident_b)
            A("copy", [pt2t], [hidT], out=hidT[:], in_=pt2[:, 0:256].rearrange("p (k t) -> p k t", k=2))
            yst = ysb[sl % 3]
            for nh in range(2):
                po = mmb[2 + nh]
                for kc in range(2):
                    MM(po, po[:], hidT, hidT[:, kc, :], Wd[i], Wd[i][:, kc, nh * 512:(nh + 1) * 512], kc == 0, kc == 1)
                V("tensor_tensor", [po, g2t], [yst], out=yst[:, nh * 512:(nh + 1) * 512], in0=po[:],
                  in1=g2t[:, nh * 512:(nh + 1) * 512], op=ALU.mult)
            store("sync", yst, YS[rows, :], yst[:])

        NSL = NBLK_run * SUB

        def xload(sl):
            xst = xs_in[sl % 3]
            load("sync", xst, xst[:], XS[sl * 128:(sl + 1) * 128, :])

        if NSL:
            xload(0)
            if NSL > 1:
                xload(1)
            play(run_stage(front, 0))
        for sl in range(NSL):
            if sl + 2 < NSL:
                xload(sl + 2)
            if sl % SUB == 0 and sl // SUB + 1 < NBLK_run:
                gathers(sl // SUB + 1)
            if sl + 1 < NSL:
                play(run_stage(front, sl + 1))
            play(run_stage(back, sl))
        P.barrier()

        y1r = [sb(f"y1r{i}", [128, D], F32) for i in range(2)]
        y2r = [sb(f"y2r{i}", [128, D], F32) for i in range(2)]
        xo = [sb(f"xo{i}", [128, D], F32) for i in range(2)]
        for t in range(0 if stop in ("R", "S", "X") else NB):
            rows = slice(t * 128, (t + 1) * 128)
            y1, y2, xot = y1r[t % 2], y2r[t % 2], xo[t % 2]
            for yt, dki in ((y1, d1i), (y2, d2i)):
                if yt.lg is None:
                    yt.lg = P.grp("l_" + yt.name)
                P.op("gpsimd", lambda e, yt=yt, dki=dki, t=t: e.indirect_dma_start(
                    out=yt[:], out_offset=None, in_=YS[:, :],
                    in_offset=bass.IndirectOffsetOnAxis(ap=dki[:, t:t + 1], axis=0)), [dki], [yt], grp=yt.lg)
            load("sync", xot, xot[:], out[rows, :])
            V("scalar_tensor_tensor", [y1, w1, xot], [xot], out=xot[:], in0=y1[:], scalar=w1[:, t:t + 1], in1=xot[:],
              op0=ALU.mult, op1=ALU.add)
            V("scalar_tensor_tensor", [y2, w2, xot], [xot], out=xot[:], in0=y2[:], scalar=w2[:, t:t + 1], in1=xot[:],
              op0=ALU.mult, op1=ALU.add)
            store("scalar", xot, out[rows, :], xot[:])

    P.barrier()
    P.op("sync", lambda e: e.nop())
    P.finalize()
    sems = {e: nc.alloc_semaphore("sem_" + e) for e in ENGS}
    for g in P.grps:
        g.sem = nc.alloc_semaphore("g_" + g.name)
    with nc.Block() as block:
        @block.tensor
        def _(e):
            P.emit_engine("tensor", e, sems)

        @block.vector
        def _(e):
            P.emit_engine("vector", e, sems)

        @block.scalar
        def _(e):
            P.emit_engine("scalar", e, sems)

        @block.gpsimd
        def _(e):
            P.emit_engine("gpsimd", e, sems)

        @block.sync
        def _(e):
            P.emit_engine("sync", e, sems)
    return nc


def _t5_bucket_table():
    W = 128
    qi = np.arange(W)[:, None]
    kj = np.arange(2 * W)[None, :]
    dist = qi + W - kj
    in_window = (dist >= 0) & (dist < W)
    dc = np.clip(dist, 0, 128)
    max_exact = 16
    d = np.maximum(dc, 1).astype(np.float32)
    large = max_exact + (np.log(d / max_exact) / math.log(128 / max_exact) * (32 - max_exact)).astype(np.int32)
    large = np.minimum(large, 31)
    bucket = np.where(dc < max_exact, dc, large)
    return bucket, in_window


def prep_shared(inp):
    f = lambda a: np.ascontiguousarray(np.asarray(a, dtype=np.float32))
    bucket, in_window = _t5_bucket_table()
    tab = f(inp["rel_bias_table"])
    bias = tab[bucket]
    bias = np.where(in_window[:, :, None], bias, np.float32(-1e30))
    bmh = bias.reshape(128, 2, 128, 8).transpose(2, 3, 1, 0)
    sh = {
        "w_ada": f(inp["w_ada"][0]),
        "b_ada": f(inp["b_ada"][0]).reshape(1, -1),
        "gmix": f(inp["norm_mix_g"][0]).reshape(1, -1),
        "gffn": f(inp["norm_ffn_g"][0]).reshape(1, -1),
        "w_in": f(inp["w_in"][0]),
        "kern": f(np.asarray(inp["dw_kernel"][0]).reshape(31, 4, 128).transpose(2, 1, 0)),
        "cvec": f(np.stack([np.asarray(inp["dw_bias"][0]).reshape(4, 128).T,
                            np.asarray(inp["conv_ln_g"][0]).reshape(4, 128).T,
                            np.asarray(inp["conv_ln_b"][0]).reshape(4, 128).T], axis=2)),
        "wco": f(inp["w_conv_out"][0]),
        "wao": f(inp["w_attn_out"][0]),
        "wout": f(inp["w_out"][0]),
        "gq": f(inp["q_norm_g"][0]).reshape(1, -1),
        "gk": f(inp["k_norm_g"][0]).reshape(1, -1),
        "sinks": f(inp["sinks"][0]).reshape(1, -1),
        "bm": f(bmh),
        "wr": f(np.concatenate([np.asarray(inp["w_router_group"][0]), np.asarray(inp["w_router_expert"][0])], axis=1)),
        "br": f(np.concatenate([np.asarray(inp["b_router_group"][0]), np.asarray(inp["b_router_expert"][0])])).reshape(1, -1),
        "weg": f(inp["w_exp_gate"][0]),
        "weu": f(inp["w_exp_up"][0]),
        "wed": f(inp["w_exp_down"][0]),
    }
    return sh


def kernel(**inputs):
    x = np.asarray(inputs["x"], dtype=np.float32)
    c = np.asarray(inputs["c"], dtype=np.float32)
    Bn, T, _ = x.shape
    sh = prep_shared(inputs)
    nc = build(T)
    in_maps = []
    for b in range(Bn):
        m = dict(sh)
        m["x"] = np.ascontiguousarray(x[b])
        m["c_col"] = np.ascontiguousarray(c[b].reshape(8, 128).T)
        in_maps.append(m)
    res = run_bass_kernel_spmd(nc, in_maps, core_ids=list(range(Bn)))
    return np.stack([np.asarray(r["out"]) for r in res.results], axis=0).astype(np.float32)
```

```python
import math
import numpy as np
import concourse.bass as bass
import concourse.mybir as mybir
from concourse.bass_utils import run_bass_kernel_spmd

F32 = mybir.dt.float32
BF16 = mybir.dt.bfloat16
I32 = mybir.dt.int32
ALU = mybir.AluOpType
ACTF = mybir.ActivationFunctionType
AX = mybir.AxisListType

ENGS = ["tensor", "vector", "scalar", "gpsimd", "sync"]

D = 1024
DIN = 3840
TT = 256
NBS = TT // 128
EPS = 1e-6
NE = 32
DE = 256
SUB = 2
BLK = 128 * SUB


class Tl:
    def __init__(self, name, t):
        self.name = name
        self.t = t
        self.w = []
        self.r = []
        self.lg = None
        self.sg = None

    def __getitem__(self, k):
        return self.t[k]


class Grp:
    def __init__(self, name):
        self.name = name
        self.n = 0
        self.sem = None


class Op:
    __slots__ = ("eng", "fn", "deps", "grp", "signal", "count", "waits")

    def __init__(self, eng, fn, deps, grp=None):
        self.eng = eng
        self.fn = fn
        self.deps = deps
        self.grp = grp
        self.signal = False
        self.count = None
        self.waits = None


class Prog:
    def __init__(self, nc):
        self.nc = nc
        self.ops = {e: [] for e in ENGS}
        self.grps = []
        self.extra = {e: [] for e in ENGS}
        self.defer = None

    def grp(self, name):
        g = Grp(name)
        self.grps.append(g)
        return g

    def op(self, eng, fn, reads=(), writes=(), grp=None):
        if self.defer is not None:
            self.defer.append((eng, fn, list(reads), list(writes), grp))
            return None
        deps = []
        for t in reads:
            deps.extend(t.w)
        for t in writes:
            deps.extend(t.w)
            deps.extend(t.r)
        deps.extend(self.extra[eng])
        self.extra[eng] = []
        o = Op(eng, fn, deps, grp)
        self.ops[eng].append(o)
        idx = len(self.ops[eng]) - 1
        if grp is not None:
            grp.n += 1
            tok = ("d", grp, grp.n)
        else:
            tok = ("c", eng, idx)
        wset = set(id(t) for t in writes)
        for t in reads:
            if id(t) in wset:
                continue
            t.r = [x for x in t.r if not (x[0] == tok[0] and x[1] is tok[1])] + [tok]
        for t in writes:
            t.w = [tok]
            t.r = []
        return tok

    def barrier(self, exclude=()):
        toks = []
        for e in ENGS:
            if self.ops[e]:
                toks.append(("c", e, len(self.ops[e]) - 1))
        for g in self.grps:
            if g.n and not any(g is x for x in exclude):
                toks.append(("d", g, g.n))
        for e in ENGS:
            self.extra[e].extend(toks)

    def finalize(self):
        for e in ENGS:
            known_c = {}
            known_d = {}
            for i, o in enumerate(self.ops[e]):
                waits = []
                for d in o.deps:
                    if d[0] == "c":
                        _, e2, j = d
                        if e2 == e and (e == "tensor" or j < i - 2):
                            continue
                        if known_c.get(e2, -1) >= j:
                            continue
                        known_c[e2] = j
                        waits.append(d)
                    else:
                        _, g, n = d
                        if known_d.get(id(g), 0) >= n:
                            continue
                        known_d[id(g)] = n
                        waits.append(d)
                o.waits = waits
        for e in ENGS:
            for o in self.ops[e]:
                for d in o.waits:
                    if d[0] == "c":
                        self.ops[d[1]][d[2]].signal = True
        for e in ENGS:
            c = 0
            for o in self.ops[e]:
                if o.signal and o.grp is None:
                    c += 1
                    o.count = c

    def emit_engine(self, e, eng, sems):
        for o in self.ops[e]:
            for d in o.waits:
                if d[0] == "c":
                    tgt = self.ops[d[1]][d[2]]
                    if tgt.count is None:
                        continue
                    eng.wait_ge(sems[d[1]], tgt.count)
                else:
                    eng.wait_ge(d[1].sem, 16 * d[2])
            ins = o.fn(eng)
            if o.grp is not None:
                ins.then_inc(o.grp.sem, 16)
            elif o.signal:
                ins.then_inc(sems[e], 1)


def _dsize(dt):
    return 2 if dt == BF16 else 4


def build(T, phase1_only=False, debug=False, stop=None):
    NB = T // 128
    NS = T // TT
    NBLK = -(-2 * T // BLK) + NE
    NSLOT = NBLK * BLK
    nc = bass.Bass("TRN2", target_bir_lowering=False)
    P = Prog(nc)

    def din(name, shape, dt=F32):
        return nc.dram_tensor(name, shape, dt, kind="ExternalInput").ap()

    x = din("x", [T, D])
    c_col = din("c_col", [128, 8])
    w_ada = din("w_ada", [D, 6 * D])
    b_ada = din("b_ada", [1, 6 * D])
    gmix = din("gmix", [1, D])
    gffn = din("gffn", [1, D])
    w_in = din("w_in", [D, DIN])
    kern = din("kern", [128, 4, 31])
    cvec = din("cvec", [128, 4, 3])
    wco = din("wco", [512, D])
    wao = din("wao", [512, D])
    wout = din("wout", [D, D])
    gq = din("gq", [1, 64])
    gk = din("gk", [1, 64])
    sinks = din("sinks", [1, 8])
    bm = din("bm", [128, 8, 2, 128])
    wr = din("wr", [D, 36])
    br = din("br", [1, 36])
    weg = din("weg", [NE, D, DE])
    weu = din("weu", [NE, D, DE])
    wed = din("wed", [NE, DE, D])
    out = nc.dram_tensor("out", [T, D], F32, kind="ExternalOutput").ap()
    H2 = nc.dram_tensor("h2_scr", [T, D], BF16, kind="Internal").ap()
    Ldram = nc.dram_tensor("l_scr", [128, NB, 36], F32, kind="Internal").ap()
    G2d = nc.dram_tensor("g2_scr", [128, D], F32, kind="Internal").ap()
    WGU = nc.dram_tensor("wgu_scr", [NE * 128, 8, 2 * DE], BF16, kind="Internal").ap()
    WDS = nc.dram_tensor("wd_scr", [NE * 128, 2, D], BF16, kind="Internal").ap()
    XS = nc.dram_tensor("xs_scr", [NSLOT, D], BF16, kind="Internal").ap()
    YS = nc.dram_tensor("ys_scr", [NSLOT, D], BF16, kind="Internal").ap()
    if debug:
        Ldbg = nc.dram_tensor("Ldbg", [128, NB, 36], F32, kind="ExternalOutput").ap()
        Ddbg = nc.dram_tensor("Ddbg", [128, 4, NB], F32, kind="ExternalOutput").ap()

    off = [16640]
    LIMIT = 229376 - 512

    def sb(name, shape, dt):
        nbytes = int(np.prod(shape[1:])) * _dsize(dt)
        nbytes = (nbytes + 63) // 64 * 64
        t = nc.alloc_sbuf_tensor_at(name, list(shape), dt, offset=off[0])
        off[0] += nbytes
        assert off[0] <= LIMIT, f"SBUF overflow at {name}: {off[0]}"
        return Tl(name, t)

    def ps(name, shape, dt):
        return Tl(name, nc.alloc_psum_tensor(name, list(shape), dt))

    tp = [ps(f"tp{i}", [128, 1024], BF16) for i in range(2)]
    mmb = [ps(f"mm{i}", [128, 512], F32) for i in range(4)]
    attA = ps("attA", [128, 512], F32)
    attB = ps("attB", [128, 512], F32)
    tpi = [0]
    mmi = [0]

    def gtp():
        tpi[0] += 1
        return tp[tpi[0] % 2]

    def gmm():
        mmi[0] += 1
        return mmb[mmi[0] % 4]

    def gmm_ab():
        mmi[0] += 1
        return mmb[mmi[0] % 2]

    def gmm_g():
        mmi[0] += 1
        return mmb[2 + mmi[0] % 2]

    def E(eng, fn, reads, writes, *a, **k):
        return P.op(eng, lambda e: getattr(e, fn)(*a, **k), reads, writes)

    def V(fn, reads, writes, *a, **k):
        return E("vector", fn, reads, writes, *a, **k)

    def A(fn, reads, writes, *a, **k):
        return E("scalar", fn, reads, writes, *a, **k)

    def G(fn, reads, writes, *a, **k):
        return E("gpsimd", fn, reads, writes, *a, **k)

    def MM(o_tl, o_ap, l_tl, l_ap, r_tl, r_ap, start, stop):
        return P.op("tensor", lambda e: e.matmul(o_ap, lhsT=l_ap, rhs=r_ap, start=start, stop=stop),
                    [l_tl, r_tl], [o_tl])

    def TR(o_tl, o_ap, i_tl, i_ap, id_tl):
        return P.op("tensor", lambda e: e.transpose(out=o_ap, in_=i_ap, identity=id_tl[:]),
                    [i_tl, id_tl], [o_tl])

    def load(q, tl, o_ap, i_ap):
        if tl.lg is None:
            tl.lg = P.grp("l_" + tl.name)
        return P.op(q, lambda e: e.dma_start(out=o_ap, in_=i_ap), [], [tl], grp=tl.lg)

    def store(q, tl, o_ap, i_ap):
        if tl.sg is None:
            tl.sg = P.grp("s_" + tl.name)
        return P.op(q, lambda e: e.dma_start(out=o_ap, in_=i_ap), [tl], [], grp=tl.sg)

    mod = sb("mod", [128, 4 * D], F32)
    B1, A1 = mod[:, 0:D], mod[:, D:2 * D]
    B2, A2 = mod[:, 2 * D:3 * D], mod[:, 3 * D:4 * D]
    ident_f = sb("ident_f", [128, 128], F32)
    ident_b = sb("ident_b", [128, 128], BF16)
    nhalf = sb("nhalf", [128, 1], F32)
    persist_end = off[0]

    G("memset", [], [ident_f], ident_f[:], 1.0)
    G("affine_select", [ident_f], [ident_f], out=ident_f[:], in_=ident_f[:], pattern=[[-1, 128]],
      compare_op=ALU.is_equal, fill=0.0, base=0, channel_multiplier=1)
    V("tensor_copy", [ident_f], [ident_b], out=ident_b[:], in_=ident_f[:])
    G("memset", [], [nhalf], nhalf[:], -0.5)

    w_in_sb = sb("w_in_sb", [128, 8, DIN], BF16)
    wco_sb = sb("wco_sb", [128, 4, D], BF16)
    wao_sb = sb("wao_sb", [128, 4, D], BF16)
    wout_sb = sb("wout_sb", [128, 8, D], BF16)
    kern_sb = sb("kern_sb", [128, 4, 31], F32)
    cvec_sb = sb("cvec_sb", [128, 4, 3], F32)
    bm_sb = sb("bm_sb", [128, 8, 2, 128], F32)
    wr_sb = sb("wr_sb", [128, 8, 36], F32)
    brb = sb("brb", [128, 36], F32)
    gqb = sb("gqb", [128, 64], F32)
    gkb = sb("gkb", [128, 64], F32)
    gq8 = sb("gq8", [128, 8, 64], F32)
    gk2 = sb("gk2", [128, 2, 64], F32)
    snk = sb("snk", [128, 8], F32)
    esink = sb("esink", [128, 8], F32)
    negc = sb("negc", [128, 1], F32)
    mq = sb("mq", [128, 1], F32)
    mk = sb("mk", [128, 1], F32)
    onesdiv = sb("onesdiv", [128, 128], BF16)

    cgrp = P.grp("wcast")

    def expert_casts(e_):
        rws = slice(e_ * 128, (e_ + 1) * 128)
        P.op("gpsimd", lambda e: e.dma_start(
            out=WGU[rws, :, 0:DE], in_=weg[e_, :, :].rearrange("(p kc) f -> p kc f", p=128)), [], [], grp=cgrp)
        P.op("gpsimd", lambda e: e.dma_start(
            out=WGU[rws, :, DE:2 * DE], in_=weu[e_, :, :].rearrange("(p kc) f -> p kc f", p=128)), [], [], grp=cgrp)
        P.op("gpsimd", lambda e: e.dma_start(
            out=WDS[rws, :, :], in_=wed[e_, :, :].rearrange("(p kc) n -> p kc n", p=128)), [], [], grp=cgrp)
    load("sync", kern_sb, kern_sb[:], kern[:, :, :])
    load("sync", cvec_sb, cvec_sb[:], cvec[:, :, :])
    load("sync", bm_sb, bm_sb[:], bm[:, :, :, :])
    load("sync", wr_sb, wr_sb[:], wr.rearrange("(kc p) n -> p kc n", p=128))
    load("sync", brb, brb[:], br[0, :].partition_broadcast(128))
    load("sync", gqb, gqb[:], gq[0, :].partition_broadcast(128))
    load("sync", gkb, gkb[:], gk[0, :].partition_broadcast(128))
    load("sync", snk, snk[:], sinks[0, :].partition_broadcast(128))
    resident_end = off[0]
    stgw = [sb(f"stgw{i}", [128, 4096], F32) for i in range(2)]
    w_in_v = w_in.rearrange("(kc p) n -> p kc n", p=128)
    wco_v = wco.rearrange("(kc p) n -> p kc n", p=128)
    wao_v = wao.rearrange("(kc p) n -> p kc n", p=128)
    wout_v = wout.rearrange("(kc p) n -> p kc n", p=128)
    jobs = []
    for kc in range(8):
        jobs.append((w_in_v[:, kc, :], w_in_sb, w_in_sb[:, kc, :], 3840))
    jobs.append((wco_v, wco_sb, wco_sb[:], 4096))
    jobs.append((wao_v, wao_sb, wao_sb[:], 4096))
    for hh_ in range(2):
        jobs.append((wout_v[:, hh_ * 4:(hh_ + 1) * 4, :], wout_sb, wout_sb[:, hh_ * 4:(hh_ + 1) * 4, :], 4096))
    for ji, (src, dtl, dap, nel) in enumerate(jobs):
        st_ = stgw[ji % 2]
        sview = st_[:, 0:nel]
        if len(src.shape) == 3:
            sview = sview.rearrange("p (k n) -> p k n", k=src.shape[1])
        load("scalar", st_, sview, src)
        if ji % 2 == 0:
            V("tensor_copy", [st_], [dtl], out=dap, in_=sview)
        else:
            A("copy", [st_], [dtl], out=dap, in_=sview)

    csb = sb("csb", [128, 8], F32)
    scs = sb("scs", [128, 8], F32)
    scb = sb("scb", [128, 8, 128], F32)
    gmb = sb("gmb", [128, D], F32)
    gfb = sb("gfb", [128, D], F32)
    wa = [sb(f"wa{i}", [128, 8, 256], F32) for i in range(2)]
    modT = sb("modT", [128, 6 * D], F32)
    G1 = modT[:, 2 * D:3 * D]
    load("sync", csb, csb[:], c_col[:, :])
    load("sync", modT, modT[:], b_ada[0, :].partition_broadcast(128))
    load("sync", gmb, gmb[:], gmix[0, :].partition_broadcast(128))
    load("sync", gfb, gfb[:], gffn[0, :].partition_broadcast(128))
    A("activation", [csb], [scs], out=scs[:], in_=csb[:], func=ACTF.Silu)
    for kc in range(8):
        V("tensor_copy", [scs], [scb], out=scb[:, kc, :], in_=scs[:, kc:kc + 1].to_broadcast([128, 128]))
    w_ada_v = w_ada.rearrange("(kc p) n -> p kc n", p=128)
    for n in range(24):
        wt = wa[n % 2]
        load("sync", wt, wt[:], w_ada_v[:, :, n * 256:(n + 1) * 256])
        pm = gmm()
        for kc in range(8):
            MM(pm, pm[:, 0:256], scb, scb[:, kc, :], wt, wt[:, kc, :], kc == 0, kc == 7)
        V("tensor_tensor", [pm, modT], [modT], out=modT[:, n * 256:(n + 1) * 256], in0=pm[:, 0:256],
          in1=modT[:, n * 256:(n + 1) * 256], op=ALU.add)
    V("scalar_tensor_tensor", [modT, gmb], [modT], out=modT[:, D:2 * D], in0=modT[:, D:2 * D], scalar=1.0, in1=gmb[:],
      op0=ALU.add, op1=ALU.mult)
    V("scalar_tensor_tensor", [modT, gfb], [modT], out=modT[:, 4 * D:5 * D], in0=modT[:, 4 * D:5 * D], scalar=1.0,
      in1=gfb[:], op0=ALU.add, op1=ALU.mult)
    V("tensor_copy", [modT], [mod], out=mod[:, 0:2 * D], in_=modT[:, 0:2 * D])
    V("tensor_copy", [modT], [mod], out=mod[:, 2 * D:4 * D], in_=modT[:, 3 * D:5 * D])
    for kc in range(8):
        V("scalar_tensor_tensor", [wout_sb, modT], [wout_sb], out=wout_sb[:, kc, :], in0=wout_sb[:, kc, :], scalar=0.5,
          in1=G1, op0=ALU.mult, op1=ALU.mult)
    store("sync", modT, G2d[:, :], modT[:, 5 * D:6 * D])
    P.barrier()
    off[0] = resident_end
    NS_run = NS
    if stop == "p0":
        NS_run = 0

    G("memset", [], [onesdiv], onesdiv[:], 1.0 / 512.0)
    V("tensor_scalar", [gqb], [gq8], out=gq8[:], in0=gqb[:, None, :].to_broadcast([128, 8, 64]),
      scalar1=0.125, scalar2=None, op0=ALU.mult)
    V("tensor_copy", [gkb], [gk2], out=gk2[:], in_=gkb[:, None, :].to_broadcast([128, 2, 64]))
    V("reduce_max", [gqb], [mq], out=mq[:], in_=gqb[:], axis=AX.X, apply_absolute_value=True)
    V("reduce_max", [gkb], [mk], out=mk[:], in_=gkb[:], axis=AX.X, apply_absolute_value=True)
    V("scalar_tensor_tensor", [mq, mk], [negc], out=negc[:], in0=mq[:], scalar=-8.0, in1=mk[:],
      op0=ALU.mult, op1=ALU.mult)
    A("activation", [snk, negc], [esink], out=esink[:], in_=snk[:], func=ACTF.Exp, bias=negc[:, 0:1])

    xin = [sb(f"xin{i}", [128, D], F32) for i in range(2)]
    xr = [sb(f"xr{i}", [128, D], F32) for i in range(2)]
    tmpf = sb("tmpf", [128, D], F32)
    h2f = tmpf
    Lrow = [sb(f"Lrow{i}", [128, 36], F32) for i in range(2)]
    h2Tt = sb("h2Tt", [128, 1024], F32)
    hb = [sb(f"hb{i}", [128, D], BF16) for i in range(1)]
    h2b = [sb(f"h2b{i}", [128, D], BF16) for i in range(1)]
    hTs = [sb(f"hT{i}", [128, 8, TT], BF16) for i in range(2)]
    ubuf = sb("ubuf", [128, 4, TT + 30], F32)
    ub3 = sb("ub3", [128, TT + 30], BF16)
    dg = [sb(f"dg{i}", [128, 128], BF16) for i in range(4)]
    dgi = [0]
    acc = [sb(f"acc{c}", [128, TT], F32) for c in range(4)]
    cbf = sb("cbf", [128, 4, TT], BF16)
    sqb = sb("sqb", [128, 4, TT], BF16)
    uT = sb("uT", [128, 4, TT], BF16)
    oT = sb("oT", [128, 4, TT], BF16)
    mT = sb("mT", [128, 8, TT], BF16)
    sgc = sb("sgc", [128, 8, TT], BF16)
    sga = sb("sga", [128, 8, TT], BF16)
    fgA = mmb[2]
    fgB = mmb[2]
    sgb = [sb(f"sgb{i}", [128, TT], F32) for i in range(1)] * 2
    s1 = [sb(f"s1_{i}", [128, TT], F32) for i in range(2)]
    s2 = [sb(f"s2_{i}", [128, TT], F32) for i in range(2)]
    t1 = s1
    t2 = s2
    mean_sb = s1[0]
    m2 = s2[0]
    var = m2
    rln = m2
    ssq = sb("ssq", [128, 1], F32)
    msq = sb("msq", [128, 1], F32)
    rstd = sb("rstd", [128, 1], F32)
    ssq2 = sb("ssq2", [128, 1], F32)
    msq2 = sb("msq2", [128, 1], F32)
    rstd2 = sb("rstd2", [128, 1], F32)
    ssq10 = sb("ssq10", [128, 10], F32)
    ms10 = sb("ms10", [128, 10], F32)
    rs10 = sb("rs10", [128, 10], F32)
    qn = sb("qn", [128, 512], BF16)
    kpad = sb("kpad", [128, 2, 2, 128], BF16)
    vaug = [sb(f"vaug{i}", [128, 2, 65], BF16) for i in range(2)]
    qT = sb("qT", [128, 4, 128], BF16)
    kT = [sb(f"kT{i}", [128, 4, 128], BF16) for i in range(2)]
    lgT = sb("lgT", [128, 1024], F32)
    PT = sb("PT", [128, 4, 2, 128], BF16)
    den = sb("den", [128, 4], F32)
    rden = sb("rden", [128, 4], F32)
    onb = sb("onb", [128, 512], BF16)
    phase1_end = off[0]

    for i in range(2):
        G("memset", [], [vaug[i]], vaug[i][:], 1.0)
    G("memset", [], [kpad], kpad[:], 0.0)

    def stageA(s):
        hT = hTs[s % 2]
        for j in range(NBS):
            blk = s * NBS + j
            xi = xin[blk % 2]
            load("sync", xi, xi[:], x[blk * 128:(blk + 1) * 128, :])
            A("activation", [xi], [hb[0], ssq], out=hb[0][:], in_=xi[:], func=ACTF.Square, accum_out=ssq[:, 0:1])
            V("tensor_scalar", [ssq], [msq], out=msq[:], in0=ssq[:], scalar1=1.0 / D, scalar2=EPS,
              op0=ALU.mult, op1=ALU.add)
            G("tensor_tensor", [msq, nhalf], [rstd], out=rstd[:], in0=msq[:], in1=nhalf[:], op=ALU.pow)
            V("scalar_tensor_tensor", [xi, rstd, mod], [tmpf], out=tmpf[:], in0=xi[:], scalar=rstd[:, 0:1],
              in1=A1, op0=ALU.mult, op1=ALU.mult)
            hbt = hb[0]
            V("tensor_tensor", [tmpf, mod], [hbt], out=hbt[:], in0=tmpf[:], in1=B1, op=ALU.add)
            pt = gtp()
            for kc in range(8):
                TR(pt, pt[:, kc * 128:(kc + 1) * 128], hbt, hbt[:, kc * 128:(kc + 1) * 128], ident_b)
            A("copy", [pt], [hT], out=hT[:, :, j * 128:(j + 1) * 128],
              in_=pt[:].rearrange("p (k t) -> p k t", k=8))

    def stageB(s):
        hT = hTs[s % 2]
        if s == 0:
            G("memset", [], [ubuf], ubuf[:, :, 0:30], 0.0)
            G("memset", [], [ub3], ub3[:, 0:30], 0.0)
        else:
            A("copy", [ubuf], [ubuf], out=ubuf[:, 0:3, 0:30], in_=ubuf[:, 0:3, TT:TT + 30])
            A("copy", [ub3], [ub3], out=ub3[:, 0:30], in_=ub3[:, TT:TT + 30])
        for c in range(4):
            pa = gmm_ab()
            for kc in range(8):
                MM(pa, pa[:, 0:TT], w_in_sb, w_in_sb[:, kc, c * 128:(c + 1) * 128], hT, hT[:, kc, :], kc == 0, kc == 7)
            pb = gmm_ab()
            for kc in range(8):
                MM(pb, pb[:, 0:TT], w_in_sb, w_in_sb[:, kc, 512 + c * 128:512 + (c + 1) * 128], hT, hT[:, kc, :],
                   kc == 0, kc == 7)
            sg = sgb[c % 2]
            A("activation", [pb], [sg], out=sg[:], in_=pb[:, 0:TT], func=ACTF.Sigmoid)
            if c == 3:
                V("tensor_tensor", [pa, sg], [ub3], out=ub3[:, 30:30 + TT], in0=pa[:, 0:TT], in1=sg[:], op=ALU.mult)
            else:
                V("tensor_tensor", [pa, sg], [ubuf], out=ubuf[:, c, 30:30 + TT], in0=pa[:, 0:TT], in1=sg[:], op=ALU.mult)

    def stageC(s):
        bank = mmb[1]
        for tap in range(31):
            dgt = dg[dgi[0] % 4]
            dgi[0] += 1
            A("activation", [ident_f, kern_sb], [dgt], out=dgt[:], in_=ident_f[:], func=ACTF.Identity,
              scale=kern_sb[:, 3, tap:tap + 1])
            MM(bank, bank[:, 0:TT], dgt, dgt[:], ub3, ub3[:, tap:tap + TT], tap == 0, tap == 30)
        A("activation", [bank, cvec_sb], [acc[3]], out=acc[3][:], in_=bank[:, 0:TT], func=ACTF.Identity,
          bias=cvec_sb[:, 3, 0:1])
        pe_part = P.defer
        P.defer = []
        for c in range(3):
            V("tensor_scalar", [ubuf, kern_sb, cvec_sb], [acc[c]], out=acc[c][:], in0=ubuf[:, c, 0:TT],
              scalar1=kern_sb[:, c, 0:1], scalar2=cvec_sb[:, c, 0:1], op0=ALU.mult, op1=ALU.add)
        for tap in range(1, 31):
            for c in range(3):
                V("scalar_tensor_tensor", [ubuf, kern_sb, acc[c]], [acc[c]], out=acc[c][:],
                  in0=ubuf[:, c, tap:tap + TT], scalar=kern_sb[:, c, tap:tap + 1], in1=acc[c][:],
                  op0=ALU.mult, op1=ALU.add)
        dve_part = P.defer
        P.defer = merge(pe_part, dve_part)

    def stageD(s):
        sqv = sqb
        for c in range(4):
            A("copy", [acc[c]], [cbf], out=cbf[:, c, :], in_=acc[c][:])
            A("activation", [acc[c]], [sqb], out=sqv[:, c, :], in_=acc[c][:], func=ACTF.Square)
        pmn = mmb[1]
        for c in range(4):
            MM(pmn, pmn[:, 0:TT], onesdiv, onesdiv[:], cbf, cbf[:, c, :], c == 0, c == 3)
        pq2 = mmb[1]
        for c in range(4):
            MM(pq2, pq2[:, TT:2 * TT], onesdiv, onesdiv[:], sqb, sqv[:, c, :], c == 0, c == 3)
        A("copy", [pmn], [mean_sb], out=mean_sb[:], in_=pmn[:, 0:TT])
        V("tensor_tensor", [mean_sb], [m2], out=m2[:], in0=mean_sb[:], in1=mean_sb[:], op=ALU.mult)
        V("scalar_tensor_tensor", [pq2, m2], [m2], out=var[:], in0=pq2[:, TT:2 * TT], scalar=EPS, in1=m2[:],
          op0=ALU.add, op1=ALU.subtract)
        A("sqrt", [m2], [m2], out=m2[:], in_=m2[:])
        V("reciprocal", [m2], [m2], out=m2[:], in_=m2[:])
        for c in range(4):
            V("tensor_tensor", [acc[c], mean_sb], [acc[c]], out=acc[c][:], in0=acc[c][:], in1=mean_sb[:],
              op=ALU.subtract)
        for c in range(4):
            V("tensor_tensor", [acc[c], rln], [acc[c]], out=acc[c][:], in0=acc[c][:], in1=rln[:], op=ALU.mult)
        for c in range(4):
            A("activation", [acc[c], cvec_sb], [uT], out=uT[:, c, :], in_=acc[c][:], func=ACTF.Silu,
              bias=cvec_sb[:, c, 2:3], scale=cvec_sb[:, c, 1:2])

    def stageE(s):
        hT = hTs[s % 2]
        for j in range(NBS):
            blk = s * NBS + j
            tok = slice(j * 128, (j + 1) * 128)
            for kc in range(8):
                MM(attA, attA[:, 0:512], hT, hT[:, kc, tok], w_in_sb, w_in_sb[:, kc, 1024:1536], kc == 0, kc == 7)
            for kc in range(8):
                MM(attB, attB[:, 0:256], hT, hT[:, kc, tok], w_in_sb, w_in_sb[:, kc, 1536:1792], kc == 0, kc == 7)
            A("activation", [attA], [tmpf], out=tmpf[:, 0:512], in_=attA[:, 0:512], func=ACTF.Square)
            A("activation", [attB], [tmpf], out=tmpf[:, 512:640], in_=attB[:, 0:128], func=ACTF.Square)
            V("tensor_reduce", [tmpf], [ssq10], out=ssq10[:], in_=tmpf[:, 0:640].rearrange("p (h d) -> p h d", d=64),
              axis=AX.X, op=ALU.add)
            V("tensor_scalar", [ssq10], [ms10], out=ms10[:], in0=ssq10[:], scalar1=1.0 / 64, scalar2=EPS,
              op0=ALU.mult, op1=ALU.add)
            G("tensor_tensor", [ms10, nhalf], [rs10], out=rs10[:], in0=ms10[:],
              in1=nhalf[:, 0:1].to_broadcast([128, 10]), op=ALU.pow)
            V("tensor_tensor", [attA, rs10, ssq10], [tmpf], out=tmpf[:, 0:512].rearrange("p (h d) -> p h d", d=64), in0=attA[:, 0:512].rearrange("p (h d) -> p h d", d=64),
              in1=rs10[:, 0:8, None].to_broadcast([128, 8, 64]), op=ALU.mult)
            V("tensor_tensor", [tmpf, gq8], [qn], out=qn[:].rearrange("p (h d) -> p h d", d=64), in0=tmpf[:, 0:512].rearrange("p (h d) -> p h d", d=64),
              in1=gq8[:], op=ALU.mult)
            V("tensor_tensor", [attB, rs10], [tmpf], out=tmpf[:, 512:640].rearrange("p (h d) -> p h d", d=64), in0=attB[:, 0:128].rearrange("p (h d) -> p h d", d=64),
              in1=rs10[:, 8:10, None].to_broadcast([128, 2, 64]), op=ALU.mult)
            for dd in range(2):
                V("tensor_tensor", [tmpf, gk2], [kpad], out=kpad[:, :, dd, dd * 64:(dd + 1) * 64],
                  in0=tmpf[:, 512:640].rearrange("p (h d) -> p h d", d=64), in1=gk2[:], op=ALU.mult)
            va = vaug[blk % 2]
            A("copy", [attB], [va], out=va[:, :, 0:64], in_=attB[:, 128:256].rearrange("p (h d) -> p h d", d=64))
            pt = gtp()
            for c in range(4):
                TR(pt, pt[:, c * 128:(c + 1) * 128], qn, qn[:, c * 128:(c + 1) * 128], ident_b)
            kflat = kpad[:].rearrange("p a b d -> p (a b d)")
            for kv in range(4):
                TR(pt, pt[:, (4 + kv) * 128:(5 + kv) * 128], kpad, kflat[:, kv * 128:(kv + 1) * 128], ident_b)
            kTc = kT[blk % 2]
            kTp = kT[(blk - 1) % 2]
            A("copy", [pt], [qT], out=qT[:], in_=pt[:, 0:512].rearrange("p (c t) -> p c t", c=4))
            A("copy", [pt], [kTc], out=kTc[:], in_=pt[:, 512:1024].rearrange("p (c t) -> p c t", c=4))
            js = [1] if blk == 0 else [0, 1]
            jsl = slice(js[0], 2)
            attv = [(attA, attA[:].rearrange("p (h j q) -> p h j q", h=2, j=2)),
                    (attB, attB[:].rearrange("p (h j q) -> p h j q", h=2, j=2))]
            lg4 = lgT[:].rearrange("p (h j q) -> p h j q", h=4, j=2)
            for g in range(2):
                for hh in range(4):
                    h = 4 * g + hh
                    c, r = h // 2, h % 2
                    for jj in js:
                        kTt = kTp if jj == 0 else kTc
                        at_, av_ = attv[hh // 2]
                        MM(at_, av_[:, hh % 2, jj, :], kTt, kTt[:, 2 * g + r, :], qT, qT[:, c, :], True, True)
                for hp in range(2):
                    at_, av_ = attv[hp]
                    V("tensor_tensor", [at_, bm_sb], [lgT], out=lg4[:, 2 * hp:2 * hp + 2, jsl, :], in0=av_[:, :, jsl, :],
                      in1=bm_sb[:, 4 * g + 2 * hp:4 * g + 2 * hp + 2, jsl, :], op=ALU.add)
                A("activation", [lgT, negc], [PT], out=PT[:, :, jsl, :], in_=lg4[:, :, jsl, :], func=ACTF.Exp,
                  bias=negc[:, 0:1])
                po = mmb[0]
                po3 = po[:, 0:260].rearrange("p (h e) -> p h e", e=65)
                vp = vaug[(blk - 1) % 2]
                for hh in range(4):
                    for jj in js:
                        vt = vp if jj == 0 else va
                        MM(po, po3[:, hh, :], PT, PT[:, hh, jj, :], vt, vt[:, g, :], jj == js[0], jj == js[-1])
                V("tensor_tensor", [po, esink], [den], out=den[:], in0=po3[:, :, 64], in1=esink[:, 4 * g:4 * g + 4],
                  op=ALU.add)
                V("reciprocal", [den], [rden], out=rden[:], in_=den[:])
                V("tensor_tensor", [po, rden], [onb],
                  out=onb[:].rearrange("p (h d) -> p h d", d=64)[:, 4 * g:4 * g + 4, :], in0=po3[:, :, 0:64],
                  in1=rden[:, :, None].to_broadcast([128, 4, 64]), op=ALU.mult)
            pt = gtp()
            for c in range(4):
                TR(pt, pt[:, c * 128:(c + 1) * 128], onb, onb[:, c * 128:(c + 1) * 128], ident_b)
            A("copy", [pt], [oT], out=oT[:, :, tok], in_=pt[:, 0:512].rearrange("p (c t) -> p c t", c=4))

    def stageFg(s):
        hT = hTs[s % 2]
        for mc in range(8):
            for kc in range(8):
                MM(fgA, fgA[:, 0:TT], w_in_sb, w_in_sb[:, kc, 1792 + mc * 128:1792 + (mc + 1) * 128], hT, hT[:, kc, :],
                   kc == 0, kc == 7)
            A("activation", [fgA], [sgc], out=sgc[:, mc, :], in_=fgA[:, 0:TT], func=ACTF.Tanh, scale=0.5)
            for kc in range(8):
                MM(fgB, fgB[:, TT:2 * TT], w_in_sb, w_in_sb[:, kc, 2816 + mc * 128:2816 + (mc + 1) * 128], hT, hT[:, kc, :],
                   kc == 0, kc == 7)
            A("activation", [fgB], [sga], out=sga[:, mc, :], in_=fgB[:, TT:2 * TT], func=ACTF.Tanh, scale=0.5)

    def stageF(s):
        for mc in range(8):
            ms_ = slice(mc * 128, (mc + 1) * 128)
            i2 = mc % 2
            bx, by = (mmb[2], mmb[3]) if i2 == 0 else (attA, attB)
            for kc in range(4):
                MM(bx, bx[:, 0:TT], wco_sb, wco_sb[:, kc, ms_], uT, uT[:, kc, :], kc == 0, kc == 3)
            V("scalar_tensor_tensor", [sgc, bx], [s1[i2]], out=s1[i2][:], in0=sgc[:, mc, :], scalar=1.0, in1=bx[:, 0:TT],
              op0=ALU.add, op1=ALU.mult)
            for kc in range(4):
                MM(by, by[:, 0:TT], wao_sb, wao_sb[:, kc, ms_], oT, oT[:, kc, :], kc == 0, kc == 3)
            V("scalar_tensor_tensor", [sga, by], [s2[i2]], out=s2[i2][:], in0=sga[:, mc, :], scalar=1.0, in1=by[:, 0:TT],
              op0=ALU.add, op1=ALU.mult)
            V("tensor_tensor", [s1[i2], s2[i2]], [mT], out=mT[:, mc, :], in0=s1[i2][:], in1=s2[i2][:], op=ALU.add)

    def stageG(s):
        for j in range(NBS):
            blk = s * NBS + j
            tok = slice(j * 128, (j + 1) * 128)
            rows = slice(blk * 128, (blk + 1) * 128)
            xrt = xr[blk % 2]
            load("sync", xrt, xrt[:], x[rows, :])
            for nh in range(2):
                cs = slice(nh * 512, (nh + 1) * 512)
                pp = mmb[3]
                for kc in range(8):
                    MM(pp, pp[:], mT, mT[:, kc, tok], wout_sb, wout_sb[:, kc, cs], kc == 0, kc == 7)
                V("tensor_tensor", [pp, xrt], [xrt], out=xrt[:, cs], in0=pp[:], in1=xrt[:, cs], op=ALU.add)
            store("gpsimd", xrt, out[rows, :], xrt[:])
            A("activation", [xrt], [h2b[0], ssq2], out=h2b[0][:], in_=xrt[:], func=ACTF.Square, accum_out=ssq2[:, 0:1])
            V("tensor_scalar", [ssq2], [msq2], out=msq2[:], in0=ssq2[:], scalar1=1.0 / D, scalar2=EPS,
              op0=ALU.mult, op1=ALU.add)
            G("tensor_tensor", [msq2, nhalf], [rstd2], out=rstd2[:], in0=msq2[:], in1=nhalf[:], op=ALU.pow)
            h2f = xrt
            V("scalar_tensor_tensor", [xrt, rstd2, mod], [xrt], out=xrt[:], in0=xrt[:], scalar=rstd2[:, 0:1],
              in1=A2, op0=ALU.mult, op1=ALU.mult)
            V("tensor_tensor", [xrt, mod], [xrt], out=xrt[:], in0=xrt[:], in1=B2, op=ALU.add)
            hbt = h2b[0]
            A("copy", [h2f], [hbt], out=hbt[:], in_=h2f[:])
            store("gpsimd", hbt, H2[rows, :], hbt[:])
            h2T3 = h2Tt[:].rearrange("p (k t) -> p k t", k=8)
            for hf in range(2):
                pp = mmb[3]
                for i in range(4):
                    kc = hf * 4 + i
                    TR(pp, pp[:, i * 128:(i + 1) * 128], h2f, h2f[:, kc * 128:(kc + 1) * 128], ident_f)
                if hf == 0:
                    A("copy", [pp], [h2Tt], out=h2T3[:, 0:4, :], in_=pp[:].rearrange("p (k t) -> p k t", k=4))
                else:
                    V("tensor_copy", [pp], [h2Tt], out=h2T3[:, 4:8, :], in_=pp[:].rearrange("p (k t) -> p k t", k=4))
            pp = mmb[3]
            for kc in range(8):
                MM(pp, pp[:, 0:36], h2Tt, h2T3[:, kc, :], wr_sb, wr_sb[:, kc, :], kc == 0, kc == 7)
            lr = Lrow[blk % 2]
            V("tensor_tensor", [pp, brb], [lr], out=lr[:], in0=pp[:, 0:36], in1=brb[:], op=ALU.add)
            store("gpsimd", lr, Ldram[:, blk, :], lr[:])
        if not phase1_only:
            for e_ in range(s * NE // NS, (s + 1) * NE // NS):
                expert_casts(e_)

    def run_stage(fn, s):
        P.defer = []
        fn(s)
        lst = P.defer
        P.defer = None
        return lst

    def merge(la, lb):
        res = []
        ia = ib = 0
        na, nb = len(la), len(lb)
        while ia < na or ib < nb:
            if ib >= nb or (ia < na and ia * nb <= ib * na):
                res.append(la[ia]); ia += 1
            else:
                res.append(lb[ib]); ib += 1
        return res

    def play(lst):
        for a in lst:
            P.op(*a)

    if NS_run:
        play(run_stage(stageA, 0) + run_stage(stageB, 0))
    for s in range(NS_run):
        eg = run_stage(stageE, s)
        if s > 0:
            eg = merge(eg, run_stage(stageG, s - 1))
        eg = merge(eg, run_stage(stageFg, s))
        play(merge(run_stage(stageC, s) + run_stage(stageD, s), eg))
        df = run_stage(stageF, s)
        if s + 1 < NS_run:
            df = merge(df, run_stage(stageA, s + 1) + run_stage(stageB, s + 1))
        play(df)
    if NS_run:
        play(run_stage(stageG, NS_run - 1))

    P.barrier()
    if debug:
        P.op("sync", lambda e: e.dma_start(out=Ldbg[:, :, :], in_=Ldram[:, :, :]), [], [], grp=P.grp("dbgL"))


    if not phase1_only:
        off[0] = persist_end
        KMAX = -(-T // BLK)
        w1 = sb("w1", [128, NB], F32)
        w2 = sb("w2", [128, NB], F32)
        d1i = sb("d1i", [128, NB], I32)
        d2i = sb("d2i", [128, NB], I32)
        blke_i = sb("blke_i", [128, NBLK], I32)
        Lt = sb("Lt", [128, NB, 36], F32)
        load("sync", Lt, Lt[:], Ldram[:, :, :])
        widx = sb("widx", [128, NBLK], I32)
        route_keep = off[0]
        ones_f = sb("ones_f", [128, 128], F32)
        ustr = sb("ustr", [128, 128], F32)
        gmax = sb("gmax", [128, NB], F32)
        ohg = sb("ohg", [128, NB, 4], F32)
        eg = sb("eg", [128, NB, 4], F32)
        sume = sb("sume", [128, NB], F32)
        ptop = sb("ptop", [128, NB], F32)
        tmp8 = sb("tmp8", [128, NB, 8], F32)
        elsel = sb("elsel", [128, NB, 8], F32)
        els2 = sb("els2", [128, NB, 8], F32)
        oh1 = sb("oh1", [128, NB, 8], F32)
        oh2 = sb("oh2", [128, NB, 8], F32)
        m1 = sb("m1", [128, NB], F32)
        m2v = sb("m2v", [128, NB], F32)
        ddv = sb("ddv", [128, NB], F32)
        e2v = sb("e2v", [128, NB], F32)
        OH1 = sb("OH1", [128, NB, 32], F32)
        OH2 = sb("OH2", [128, NB, 32], F32)
        TH = sb("TH", [128, NB, 32], F32)
        scn = [sb(f"scn{i}", [128, NB, 32], F32) for i in range(2)]
        cnt_p = sb("cnt_p", [128, 32], F32)
        tot_sb = sb("tot_sb", [128, 32], F32)
        pp_sb = sb("pp_sb", [128, 32], F32)
        thr_i = sb("thr_i", [128, KMAX], I32)
        thr_f = sb("thr_f", [128, KMAX], F32)
        cmpt = sb("cmpt", [128, 32, KMAX], F32)
        pc = sb("pc", [128, 32], F32)
        pe_ = [sb(f"pend{i}", [128, 32], F32) for i in range(2)]
        base = sb("base", [128, 32], F32)
        dst_f = sb("dst_f", [128, NB], F32)
        bthr_i = sb("bthr_i", [128, NBLK], I32)
        bthr_f = sb("bthr_f", [128, NBLK], F32)
        cmpb = sb("cmpb", [128, NBLK, 32], F32)
        blke_f = sb("blke_f", [128, NBLK], F32)
        hs = [sb(f"hs{i}", [128, D], BF16) for i in range(3)]
        pidx_i = sb("pidx_i", [128, 1], I32)
        pidx_f = sb("pidx_f", [128, 1], F32)
        widx_f = sb("widx_f", [128, NBLK], F32)

        G("memset", [], [ones_f], ones_f[:], 1.0)
        G("memset", [], [ustr], ustr[:], 1.0)
        G("affine_select", [ustr], [ustr], out=ustr[:], in_=ustr[:], pattern=[[1, 128]],
          compare_op=ALU.is_gt, fill=0.0, base=0, channel_multiplier=-1)
        G("iota", [], [thr_i], thr_i[:], pattern=[[BLK, KMAX]], base=0, channel_multiplier=0)
        G("iota", [], [bthr_i], bthr_i[:], pattern=[[BLK, NBLK]], base=0, channel_multiplier=0)
        V("tensor_copy", [thr_i], [thr_f], out=thr_f[:], in_=thr_i[:])
        V("tensor_copy", [bthr_i], [bthr_f], out=bthr_f[:], in_=bthr_i[:])

        gl = Lt[:, :, 0:4]
        el4 = Lt[:, :, 4:36].rearrange("p t (g e) -> p t g e", g=4)
        bc4 = lambda ap: ap[:, :, None].to_broadcast([128, NB, 4])
        bc8 = lambda ap: ap[:, :, None].to_broadcast([128, NB, 8])
        V("tensor_reduce", [Lt], [gmax], out=gmax[:], in_=gl, axis=AX.X, op=ALU.max)
        V("tensor_tensor", [Lt, gmax], [ohg], out=ohg[:], in0=gl, in1=bc4(gmax), op=ALU.is_equal)
        V("tensor_tensor", [Lt, gmax], [eg], out=eg[:], in0=gl, in1=bc4(gmax), op=ALU.subtract)
        A("activation", [eg], [eg], out=eg[:], in_=eg[:], func=ACTF.Exp)
        V("tensor_reduce", [eg], [sume], out=sume[:], in_=eg[:], axis=AX.X, op=ALU.add)
        V("reciprocal", [sume], [ptop], out=ptop[:], in_=sume[:])
        V("tensor_tensor", [Lt, ohg], [elsel], out=elsel[:], in0=el4[:, :, 0, :],
          in1=ohg[:, :, 0:1].to_broadcast([128, NB, 8]), op=ALU.mult)
        for g in range(1, 4):
            V("tensor_tensor", [Lt, ohg], [tmp8], out=tmp8[:], in0=el4[:, :, g, :],
              in1=ohg[:, :, g:g + 1].to_broadcast([128, NB, 8]), op=ALU.mult)
            V("tensor_tensor", [elsel, tmp8], [elsel], out=elsel[:], in0=elsel[:], in1=tmp8[:], op=ALU.add)
        V("tensor_reduce", [elsel], [m1], out=m1[:], in_=elsel[:], axis=AX.X, op=ALU.max)
        V("tensor_tensor", [elsel, m1], [oh1], out=oh1[:], in0=elsel[:], in1=bc8(m1), op=ALU.is_equal)
        V("scalar_tensor_tensor", [oh1, elsel], [els2], out=els2[:], in0=oh1[:], scalar=-1e30, in1=elsel[:],
          op0=ALU.mult, op1=ALU.add)
        V("tensor_reduce", [els2], [m2v], out=m2v[:], in_=els2[:], axis=AX.X, op=ALU.max)
        V("tensor_tensor", [els2, m2v], [oh2], out=oh2[:], in0=els2[:], in1=bc8(m2v), op=ALU.is_equal)
        V("tensor_tensor", [m2v, m1], [ddv], out=ddv[:], in0=m2v[:], in1=m1[:], op=ALU.subtract)
        A("activation", [ddv], [e2v], out=e2v[:], in_=ddv[:], func=ACTF.Exp)
        V("tensor_scalar", [e2v], [ddv], out=ddv[:], in0=e2v[:], scalar1=1.0, scalar2=None, op0=ALU.add)
        V("reciprocal", [ddv], [sume], out=sume[:], in_=ddv[:])
        V("tensor_tensor", [sume, ptop], [w1], out=w1[:], in0=sume[:], in1=ptop[:], op=ALU.mult)
        V("tensor_tensor", [w1, e2v], [w2], out=w2[:], in0=w1[:], in1=e2v[:], op=ALU.mult)
        for OHk, ohk in ((OH1, oh1), (OH2, oh2)):
            V("tensor_tensor", [ohg, ohk], [OHk], out=OHk[:].rearrange("p t (g e) -> p t g e", g=4),
              in0=ohg[:, :, :, None].to_broadcast([128, NB, 4, 8]),
              in1=ohk[:, :, None, :].to_broadcast([128, NB, 4, 8]), op=ALU.mult)
        V("tensor_tensor", [OH1, OH2], [TH], out=TH[:], in0=OH1[:], in1=OH2[:], op=ALU.add)
        V("tensor_reduce", [TH], [cnt_p], out=cnt_p[:], in_=TH[:].rearrange("p t e -> p e t"), axis=AX.X, op=ALU.add)
        pA = gmm()
        MM(pA, pA[:, 0:32], ustr, ustr[:], cnt_p, cnt_p[:], True, True)
        pB = gmm()
        MM(pB, pB[:, 0:32], ones_f, ones_f[:], cnt_p, cnt_p[:], True, True)
        A("copy", [pA], [pp_sb], out=pp_sb[:], in_=pA[:, 0:32])
        A("copy", [pB], [tot_sb], out=tot_sb[:], in_=pB[:, 0:32])
        V("tensor_tensor", [tot_sb, thr_f], [cmpt], out=cmpt[:], in0=tot_sb[:, :, None].to_broadcast([128, 32, KMAX]),
          in1=thr_f[:, None, :].to_broadcast([128, 32, KMAX]), op=ALU.is_gt)
        V("tensor_reduce", [cmpt], [pc], out=pc[:], in_=cmpt[:], axis=AX.X, op=ALU.add)
        V("tensor_scalar", [pc], [pc], out=pc[:], in0=pc[:], scalar1=float(BLK), scalar2=None, op0=ALU.mult)
        src = pc
        st = 1
        i = 0
        while st < 32:
            dstt = pe_[i % 2]
            V("tensor_tensor", [src], [dstt], out=dstt[:, st:], in0=src[:, st:], in1=src[:, 0:32 - st], op=ALU.add)
            V("tensor_copy", [src], [dstt], out=dstt[:, 0:st], in_=src[:, 0:st])
            src = dstt
            st *= 2
            i += 1
        pend = src
        V("tensor_tensor", [pend, pc], [base], out=base[:], in0=pend[:], in1=pc[:], op=ALU.subtract)
        V("tensor_tensor", [base, pp_sb], [base], out=base[:], in0=base[:], in1=pp_sb[:], op=ALU.add)
        src = TH
        st = 1
        i = 0
        while st < NB:
            dstt = scn[i % 2]
            V("tensor_tensor", [src], [dstt], out=dstt[:, st:, :], in0=src[:, st:, :], in1=src[:, 0:NB - st, :], op=ALU.add)
            G("tensor_copy", [src], [dstt], out=dstt[:, 0:st, :], in_=src[:, 0:st, :])
            src = dstt
            st *= 2
            i += 1
        pos = scn[i % 2] if src is not scn[i % 2] else scn[(i + 1) % 2]
        if src is TH:
            pos = scn[0]
        V("tensor_tensor", [src, TH], [pos], out=pos[:], in0=src[:], in1=TH[:], op=ALU.subtract)
        V("tensor_tensor", [pos, base], [pos], out=pos[:], in0=pos[:], in1=base[:, None, :].to_broadcast([128, NB, 32]),
          op=ALU.add)
        for OHk, dki in ((OH1, d1i), (OH2, d2i)):
            V("tensor_tensor", [OHk, pos], [OHk], out=OHk[:], in0=OHk[:], in1=pos[:], op=ALU.mult)
            V("tensor_reduce", [OHk], [dst_f], out=dst_f[:], in_=OHk[:], axis=AX.X, op=ALU.add)
            V("tensor_copy", [dst_f], [dki], out=dki[:], in_=dst_f[:])
        V("tensor_tensor", [pend, bthr_f], [cmpb], out=cmpb[:], in0=pend[:, None, :].to_broadcast([128, NBLK, 32]),
          in1=bthr_f[:, :, None].to_broadcast([128, NBLK, 32]), op=ALU.is_le)
        V("tensor_reduce", [cmpb], [blke_f], out=blke_f[:], in_=cmpb[:], axis=AX.X, op=ALU.add)
        V("tensor_scalar", [blke_f], [blke_f], out=blke_f[:], in0=blke_f[:], scalar1=float(NE - 1), scalar2=None,
          op0=ALU.min)
        V("tensor_copy", [blke_f], [blke_i], out=blke_i[:], in_=blke_f[:])
        G("iota", [], [pidx_i], pidx_i[:], pattern=[[0, 1]], base=0, channel_multiplier=1)
        V("tensor_copy", [pidx_i], [pidx_f], out=pidx_f[:], in_=pidx_i[:])
        V("tensor_scalar", [blke_f, pidx_f], [widx_f], out=widx_f[:], in0=blke_f[:], scalar1=128.0,
          scalar2=pidx_f[:, 0:1], op0=ALU.mult, op1=ALU.add)
        V("tensor_copy", [widx_f], [widx], out=widx[:], in_=widx_f[:])
        if debug:
            dbg = sb("dbg", [128, 4, NB], F32)
            V("tensor_copy", [w1], [dbg], out=dbg[:, 0, :], in_=w1[:])
            V("tensor_copy", [w2], [dbg], out=dbg[:, 1, :], in_=w2[:])
            V("tensor_copy", [d1i], [dbg], out=dbg[:, 2, :], in_=d1i[:])
            V("tensor_copy", [d2i], [dbg], out=dbg[:, 3, :], in_=d2i[:])
            store("sync", dbg, Ddbg[:, :, :], dbg[:])

        for t in range(0 if stop == "R" else NB):
            hst = hs[t % 3]
            load("sync", hst, hst[:], H2[t * 128:(t + 1) * 128, :])
            if hst.sg is None:
                hst.sg = P.grp("s_" + hst.name)
            for dki in (d1i, d2i):
                P.op("gpsimd", lambda e, hst=hst, dki=dki, t=t: e.indirect_dma_start(
                    out=XS[:, :], out_offset=bass.IndirectOffsetOnAxis(ap=dki[:, t:t + 1], axis=0),
                    in_=hst[:], in_offset=None), [hst, dki], [], grp=hst.sg)
        P.barrier()

        off[0] = route_keep
        g2t = sb("g2t", [128, D], F32)
        load("sync", g2t, g2t[:], G2d[:, :])
        Wgu = [sb(f"Wgu{i}", [128, 8, 2 * DE], BF16) for i in range(2)]
        Wd = [sb(f"Wd{i}", [128, 2, D], BF16) for i in range(2)]
        xs_in = [sb(f"xs_in{i}", [128, D], BF16) for i in range(3)]
        XTs = [sb(f"XT{i}", [128, 8, 128], BF16) for i in range(2)]
        sgls = [sb(f"sgl{i}", [128, DE], F32) for i in range(2)]
        hids = [sb(f"hid{i}", [128, DE], BF16) for i in range(2)]
        hidTs = [sb(f"hidT{i}", [128, 2, 128], BF16) for i in range(2)]
        ysb = [sb(f"ysb{i}", [128, D], BF16) for i in range(3)]
        wgu_v = WGU.rearrange("r k f -> r (k f)")
        wds_v = WDS.rearrange("r k f -> r (k f)")

        def dyn_load(tl, src_v, b):
            if tl.lg is None:
                tl.lg = P.grp("l_" + tl.name)
            return P.op("gpsimd", lambda e: e.indirect_dma_start(
                out=tl[:].rearrange("p k f -> p (k f)"), out_offset=None, in_=src_v[:, :],
                in_offset=bass.IndirectOffsetOnAxis(ap=widx[:, b:b + 1], axis=0)), [widx], [tl], grp=tl.lg)

        def gathers(b):
            dyn_load(Wgu[b % 2], wgu_v, b)
            dyn_load(Wd[b % 2], wds_v, b)

        NBLK_run = 0 if stop in ("R", "S") else NBLK
        if NBLK_run:
            gathers(0)
        att_bf = [attA[:].bitcast(BF16), attB[:].bitcast(BF16)]
        att_tl = [attA, attB]

        def front(sl):
            b = sl // SUB
            i = b % 2
            k = sl % 2
            rows = slice(sl * 128, (sl + 1) * 128)
            xst = xs_in[sl % 3]
            XT, sgl, hid = XTs[k], sgls[k], hids[k]
            pt = tp[k]
            for kc in range(8):
                TR(pt, pt[:, kc * 128:(kc + 1) * 128], xst, xst[:].rearrange("p (f k) -> p k f", k=8)[:, kc, :], ident_b)
            A("copy", [pt], [XT], out=XT[:], in_=pt[:].rearrange("p (k t) -> p k t", k=8))
            pm = mmb[k]
            for kc in range(8):
                MM(pm, pm[:], XT, XT[:, kc, :], Wgu[i], Wgu[i][:, kc, :], kc == 0, kc == 7)
            A("activation", [pm], [sgl], out=sgl[:], in_=pm[:, 0:DE], func=ACTF.Silu)
            V("tensor_tensor", [pm, sgl], [hid], out=hid[:], in0=pm[:, DE:2 * DE], in1=sgl[:], op=ALU.mult)

        def back(sl):
            b = sl // SUB
            i = b % 2
            k = sl % 2
            rows = slice(sl * 128, (sl + 1) * 128)
            hid, hidT = hids[k], hidTs[k]
            pt2t, pt2 = att_tl[k], att_bf[k]
            for kc in range(2):
                TR(pt2t, pt2[:, kc * 128:(kc + 1) * 128], hid, hid[:].rearrange("p (f k) -> p k f", k=2)[:, kc, :], ident_b)
            A("copy", [pt2t], [hidT], out=hidT[:], in_=pt2[:, 0:256].rearrange("p (k t) -> p k t", k=2))
            yst = ysb[sl % 3]
            for nh in range(2):
                po = mmb[2 + nh]
                for kc in range(2):
                    MM(po, po[:], hidT, hidT[:, kc, :], Wd[i], Wd[i][:, kc, nh * 512:(nh + 1) * 512], kc == 0, kc == 1)
                V("tensor_tensor", [po, g2t], [yst], out=yst[:, nh * 512:(nh + 1) * 512], in0=po[:],
                  in1=g2t[:, nh * 512:(nh + 1) * 512], op=ALU.mult)
            store("sync", yst, YS[rows, :], yst[:])

        NSL = NBLK_run * SUB

        def xload(sl):
            xst = xs_in[sl % 3]
            load("sync", xst, xst[:], XS[sl * 128:(sl + 1) * 128, :])

        if NSL:
            xload(0)
            if NSL > 1:
                xload(1)
            play(run_stage(front, 0))
        for sl in range(NSL):
            if sl + 2 < NSL:
                xload(sl + 2)
            if sl % SUB == 0 and sl // SUB + 1 < NBLK_run:
                gathers(sl // SUB + 1)
            if sl + 1 < NSL:
                play(run_stage(front, sl + 1))
            play(run_stage(back, sl))
        P.barrier()

        y1r = [sb(f"y1r{i}", [128, D], BF16) for i in range(2)]
        y2r = [sb(f"y2r{i}", [128, D], BF16) for i in range(2)]
        xo = [sb(f"xo{i}", [128, D], F32) for i in range(2)]
        for t in range(0 if stop in ("R", "S", "X") else NB):
            rows = slice(t * 128, (t + 1) * 128)
            y1, y2, xot = y1r[t % 2], y2r[t % 2], xo[t % 2]
            for yt, dki in ((y1, d1i), (y2, d2i)):
                if yt.lg is None:
                    yt.lg = P.grp("l_" + yt.name)
                P.op("gpsimd", lambda e, yt=yt, dki=dki, t=t: e.indirect_dma_start(
                    out=yt[:], out_offset=None, in_=YS[:, :],
                    in_offset=bass.IndirectOffsetOnAxis(ap=dki[:, t:t + 1], axis=0)), [dki], [yt], grp=yt.lg)
            load("sync", xot, xot[:], out[rows, :])
            V("scalar_tensor_tensor", [y1, w1, xot], [xot], out=xot[:], in0=y1[:], scalar=w1[:, t:t + 1], in1=xot[:],
              op0=ALU.mult, op1=ALU.add)
            V("scalar_tensor_tensor", [y2, w2, xot], [xot], out=xot[:], in0=y2[:], scalar=w2[:, t:t + 1], in1=xot[:],
              op0=ALU.mult, op1=ALU.add)
            store("scalar", xot, out[rows, :], xot[:])

    P.barrier()
    P.op("sync", lambda e: e.nop())
    P.finalize()
    sems = {e: nc.alloc_semaphore("sem_" + e) for e in ENGS}
    for g in P.grps:
        g.sem = nc.alloc_semaphore("g_" + g.name)
    with nc.Block() as block:
        @block.tensor
        def _(e):
            P.emit_engine("tensor", e, sems)

        @block.vector
        def _(e):
            P.emit_engine("vector", e, sems)

        @block.scalar
        def _(e):
            P.emit_engine("scalar", e, sems)

        @block.gpsimd
        def _(e):
            P.emit_engine("gpsimd", e, sems)

        @block.sync
        def _(e):
            P.emit_engine("sync", e, sems)
    return nc


def _t5_bucket_table():
    W = 128
    qi = np.arange(W)[:, None]
    kj = np.arange(2 * W)[None, :]
    dist = qi + W - kj
    in_window = (dist >= 0) & (dist < W)
    dc = np.clip(dist, 0, 128)
    max_exact = 16
    d = np.maximum(dc, 1).astype(np.float32)
    large = max_exact + (np.log(d / max_exact) / math.log(128 / max_exact) * (32 - max_exact)).astype(np.int32)
    large = np.minimum(large, 31)
    bucket = np.where(dc < max_exact, dc, large)
    return bucket, in_window


def prep_shared(inp):
    f = lambda a: np.ascontiguousarray(np.asarray(a, dtype=np.float32))
    bucket, in_window = _t5_bucket_table()
    tab = f(inp["rel_bias_table"])
    bias = tab[bucket]
    bias = np.where(in_window[:, :, None], bias, np.float32(-1e30))
    bmh = bias.reshape(128, 2, 128, 8).transpose(2, 3, 1, 0)
    sh = {
        "w_ada": f(inp["w_ada"][0]),
        "b_ada": f(inp["b_ada"][0]).reshape(1, -1),
        "gmix": f(inp["norm_mix_g"][0]).reshape(1, -1),
        "gffn": f(inp["norm_ffn_g"][0]).reshape(1, -1),
        "w_in": f(inp["w_in"][0]),
        "kern": f(np.asarray(inp["dw_kernel"][0]).reshape(31, 4, 128).transpose(2, 1, 0)),
        "cvec": f(np.stack([np.asarray(inp["dw_bias"][0]).reshape(4, 128).T,
                            np.asarray(inp["conv_ln_g"][0]).reshape(4, 128).T,
                            np.asarray(inp["conv_ln_b"][0]).reshape(4, 128).T], axis=2)),
        "wco": f(inp["w_conv_out"][0]),
        "wao": f(inp["w_attn_out"][0]),
        "wout": f(inp["w_out"][0]),
        "gq": f(inp["q_norm_g"][0]).reshape(1, -1),
        "gk": f(inp["k_norm_g"][0]).reshape(1, -1),
        "sinks": f(inp["sinks"][0]).reshape(1, -1),
        "bm": f(bmh),
        "wr": f(np.concatenate([np.asarray(inp["w_router_group"][0]), np.asarray(inp["w_router_expert"][0])], axis=1)),
        "br": f(np.concatenate([np.asarray(inp["b_router_group"][0]), np.asarray(inp["b_router_expert"][0])])).reshape(1, -1),
        "weg": f(inp["w_exp_gate"][0]),
        "weu": f(inp["w_exp_up"][0]),
        "wed": f(inp["w_exp_down"][0]),
    }
    return sh


def kernel(**inputs):
    x = np.asarray(inputs["x"], dtype=np.float32)
    c = np.asarray(inputs["c"], dtype=np.float32)
    Bn, T, _ = x.shape
    sh = prep_shared(inputs)
    nc = build(T)
    in_maps = []
    for b in range(Bn):
        m = dict(sh)
        m["x"] = np.ascontiguousarray(x[b])
        m["c_col"] = np.ascontiguousarray(c[b].reshape(8, 128).T)
        in_maps.append(m)
    res = run_bass_kernel_spmd(nc, in_maps, core_ids=list(range(Bn)))
    return np.stack([np.asarray(r["out"]) for r in res.results], axis=0).astype(np.float32)
```

```python
import math
import numpy as np
import concourse.bass as bass
import concourse.mybir as mybir
from concourse.bass_utils import run_bass_kernel_spmd

F32 = mybir.dt.float32
BF16 = mybir.dt.bfloat16
I32 = mybir.dt.int32
ALU = mybir.AluOpType
ACTF = mybir.ActivationFunctionType
AX = mybir.AxisListType

ENGS = ["tensor", "vector", "scalar", "gpsimd", "sync"]

D = 1024
DIN = 3840
TT = 256
NBS = TT // 128
EPS = 1e-6
NE = 32
DE = 256
SUB = 2
BLK = 128 * SUB


class Tl:
    def __init__(self, name, t):
        self.name = name
        self.t = t
        self.w = []
        self.r = []
        self.lg = None
        self.sg = None

    def __getitem__(self, k):
        return self.t[k]


class Grp:
    def __init__(self, name):
        self.name = name
        self.n = 0
        self.sem = None


class Op:
    __slots__ = ("eng", "fn", "deps", "grp", "signal", "count", "waits")

    def __init__(self, eng, fn, deps, grp=None):
        self.eng = eng
        self.fn = fn
        self.deps = deps
        self.grp = grp
        self.signal = False
        self.count = None
        self.waits = None


class Prog:
    def __init__(self, nc):
        self.nc = nc
        self.ops = {e: [] for e in ENGS}
        self.grps = []
        self.extra = {e: [] for e in ENGS}
        self.defer = None

    def grp(self, name):
        g = Grp(name)
        self.grps.append(g)
        return g

    def op(self, eng, fn, reads=(), writes=(), grp=None):
        if self.defer is not None:
            self.defer.append((eng, fn, list(reads), list(writes), grp))
            return None
        deps = []
        for t in reads:
            deps.extend(t.w)
        for t in writes:
            deps.extend(t.w)
            deps.extend(t.r)
        deps.extend(self.extra[eng])
        self.extra[eng] = []
        o = Op(eng, fn, deps, grp)
        self.ops[eng].append(o)
        idx = len(self.ops[eng]) - 1
        if grp is not None:
            grp.n += 1
            tok = ("d", grp, grp.n)
        else:
            tok = ("c", eng, idx)
        wset = set(id(t) for t in writes)
        for t in reads:
            if id(t) in wset:
                continue
            t.r = [x for x in t.r if not (x[0] == tok[0] and x[1] is tok[1])] + [tok]
        for t in writes:
            t.w = [tok]
            t.r = []
        return tok

    def barrier(self, exclude=()):
        toks = []
        for e in ENGS:
            if self.ops[e]:
                toks.append(("c", e, len(self.ops[e]) - 1))
        for g in self.grps:
            if g.n and not any(g is x for x in exclude):
                toks.append(("d", g, g.n))
        for e in ENGS:
            self.extra[e].extend(toks)

    def finalize(self):
        for e in ENGS:
            known_c = {}
            known_d = {}
            for i, o in enumerate(self.ops[e]):
                waits = []
                for d in o.deps:
                    if d[0] == "c":
                        _, e2, j = d
                        if e2 == e and (e == "tensor" or j < i - 2):
                            continue
                        if known_c.get(e2, -1) >= j:
                            continue
                        known_c[e2] = j
                        waits.append(d)
                    else:
                        _, g, n = d
                        if known_d.get(id(g), 0) >= n:
                            continue
                        known_d[id(g)] = n
                        waits.append(d)
                o.waits = waits
        for e in ENGS:
            for o in self.ops[e]:
                for d in o.waits:
                    if d[0] == "c":
                        self.ops[d[1]][d[2]].signal = True
        for e in ENGS:
            c = 0
            for o in self.ops[e]:
                if o.signal and o.grp is None:
                    c += 1
                    o.count = c

    def emit_engine(self, e, eng, sems):
        for o in self.ops[e]:
            for d in o.waits:
                if d[0] == "c":
                    tgt = self.ops[d[1]][d[2]]
                    if tgt.count is None:
                        continue
                    eng.wait_ge(sems[d[1]], tgt.count)
                else:
                    eng.wait_ge(d[1].sem, 16 * d[2])
            ins = o.fn(eng)
            if o.grp is not None:
                ins.then_inc(o.grp.sem, 16)
            elif o.signal:
                ins.then_inc(sems[e], 1)


def _dsize(dt):
    return 2 if dt == BF16 else 4


def build(T, phase1_only=False, debug=False, stop=None):
    NB = T // 128
    NS = T // TT
    NBLK = -(-2 * T // BLK) + NE
    NSLOT = NBLK * BLK
    nc = bass.Bass("TRN2", target_bir_lowering=False)
    P = Prog(nc)

    def din(name, shape, dt=F32):
        return nc.dram_tensor(name, shape, dt, kind="ExternalInput").ap()

    x = din("x", [T, D])
    c_col = din("c_col", [128, 8])
    w_ada = din("w_ada", [D, 6 * D])
    b_ada = din("b_ada", [1, 6 * D])
    gmix = din("gmix", [1, D])
    gffn = din("gffn", [1, D])
    w_in = din("w_in", [D, DIN])
    kern = din("kern", [128, 4, 31])
    cvec = din("cvec", [128, 4, 3])
    wco = din("wco", [512, D])
    wao = din("wao", [512, D])
    wout = din("wout", [D, D])
    gq = din("gq", [1, 64])
    gk = din("gk", [1, 64])
    sinks = din("sinks", [1, 8])
    bm = din("bm", [128, 8, 2, 128])
    wr = din("wr", [D, 36])
    br = din("br", [1, 36])
    weg = din("weg", [NE, D, DE])
    weu = din("weu", [NE, D, DE])
    wed = din("wed", [NE, DE, D])
    out = nc.dram_tensor("out", [T, D], F32, kind="ExternalOutput").ap()
    H2 = nc.dram_tensor("h2_scr", [T, D], BF16, kind="Internal").ap()
    Ldram = nc.dram_tensor("l_scr", [128, NB, 36], F32, kind="Internal").ap()
    G2d = nc.dram_tensor("g2_scr", [128, D], F32, kind="Internal").ap()
    WGU = nc.dram_tensor("wgu_scr", [NE * 128, 8, 2 * DE], BF16, kind="Internal").ap()
    WDS = nc.dram_tensor("wd_scr", [NE * 128, 2, D], BF16, kind="Internal").ap()
    XS = nc.dram_tensor("xs_scr", [NSLOT, D], BF16, kind="Internal").ap()
    YS = nc.dram_tensor("ys_scr", [NSLOT, D], F32, kind="Internal").ap()
    if debug:
        Ldbg = nc.dram_tensor("Ldbg", [128, NB, 36], F32, kind="ExternalOutput").ap()
        Ddbg = nc.dram_tensor("Ddbg", [128, 4, NB], F32, kind="ExternalOutput").ap()

    off = [16640]
    LIMIT = 229376 - 512

    def sb(name, shape, dt):
        nbytes = int(np.prod(shape[1:])) * _dsize(dt)
        nbytes = (nbytes + 63) // 64 * 64
        t = nc.alloc_sbuf_tensor_at(name, list(shape), dt, offset=off[0])
        off[0] += nbytes
        assert off[0] <= LIMIT, f"SBUF overflow at {name}: {off[0]}"
        return Tl(name, t)

    def ps(name, shape, dt):
        return Tl(name, nc.alloc_psum_tensor(name, list(shape), dt))

    tp = [ps(f"tp{i}", [128, 1024], BF16) for i in range(2)]
    mmb = [ps(f"mm{i}", [128, 512], F32) for i in range(4)]
    attA = ps("attA", [128, 512], F32)
    attB = ps("attB", [128, 512], F32)
    tpi = [0]
    mmi = [0]

    def gtp():
        tpi[0] += 1
        return tp[tpi[0] % 2]

    def gmm():
        mmi[0] += 1
        return mmb[mmi[0] % 4]

    def gmm_ab():
        mmi[0] += 1
        return mmb[mmi[0] % 2]

    def gmm_g():
        mmi[0] += 1
        return mmb[2 + mmi[0] % 2]

    def E(eng, fn, reads, writes, *a, **k):
        return P.op(eng, lambda e: getattr(e, fn)(*a, **k), reads, writes)

    def V(fn, reads, writes, *a, **k):
        return E("vector", fn, reads, writes, *a, **k)

    def A(fn, reads, writes, *a, **k):
        return E("scalar", fn, reads, writes, *a, **k)

    def G(fn, reads, writes, *a, **k):
        return E("gpsimd", fn, reads, writes, *a, **k)

    def MM(o_tl, o_ap, l_tl, l_ap, r_tl, r_ap, start, stop):
        return P.op("tensor", lambda e: e.matmul(o_ap, lhsT=l_ap, rhs=r_ap, start=start, stop=stop),
                    [l_tl, r_tl], [o_tl])

    def TR(o_tl, o_ap, i_tl, i_ap, id_tl):
        return P.op("tensor", lambda e: e.transpose(out=o_ap, in_=i_ap, identity=id_tl[:]),
                    [i_tl, id_tl], [o_tl])

    def load(q, tl, o_ap, i_ap):
        if tl.lg is None:
            tl.lg = P.grp("l_" + tl.name)
        return P.op(q, lambda e: e.dma_start(out=o_ap, in_=i_ap), [], [tl], grp=tl.lg)

    def store(q, tl, o_ap, i_ap):
        if tl.sg is None:
            tl.sg = P.grp("s_" + tl.name)
        return P.op(q, lambda e: e.dma_start(out=o_ap, in_=i_ap), [tl], [], grp=tl.sg)

    mod = sb("mod", [128, 4 * D], F32)
    B1, A1 = mod[:, 0:D], mod[:, D:2 * D]
    B2, A2 = mod[:, 2 * D:3 * D], mod[:, 3 * D:4 * D]
    ident_f = sb("ident_f", [128, 128], F32)
    ident_b = sb("ident_b", [128, 128], BF16)
    nhalf = sb("nhalf", [128, 1], F32)
    persist_end = off[0]

    G("memset", [], [ident_f], ident_f[:], 1.0)
    G("affine_select", [ident_f], [ident_f], out=ident_f[:], in_=ident_f[:], pattern=[[-1, 128]],
      compare_op=ALU.is_equal, fill=0.0, base=0, channel_multiplier=1)
    V("tensor_copy", [ident_f], [ident_b], out=ident_b[:], in_=ident_f[:])
    G("memset", [], [nhalf], nhalf[:], -0.5)

    w_in_sb = sb("w_in_sb", [128, 8, DIN], BF16)
    wco_sb = sb("wco_sb", [128, 4, D], BF16)
    wao_sb = sb("wao_sb", [128, 4, D], BF16)
    wout_sb = sb("wout_sb", [128, 8, D], BF16)
    kern_sb = sb("kern_sb", [128, 4, 31], F32)
    cvec_sb = sb("cvec_sb", [128, 4, 3], F32)
    bm_sb = sb("bm_sb", [128, 8, 2, 128], F32)
    wr_sb = sb("wr_sb", [128, 8, 36], F32)
    brb = sb("brb", [128, 36], F32)
    gqb = sb("gqb", [128, 64], F32)
    gkb = sb("gkb", [128, 64], F32)
    gq8 = sb("gq8", [128, 8, 64], F32)
    gk2 = sb("gk2", [128, 2, 64], F32)
    snk = sb("snk", [128, 8], F32)
    esink = sb("esink", [128, 8], F32)
    negc = sb("negc", [128, 1], F32)
    mq = sb("mq", [128, 1], F32)
    mk = sb("mk", [128, 1], F32)
    onesdiv = sb("onesdiv", [128, 128], BF16)

    cgrp = P.grp("wcast")

    def expert_casts(e_):
        rws = slice(e_ * 128, (e_ + 1) * 128)
        P.op("gpsimd", lambda e: e.dma_start(
            out=WGU[rws, :, 0:DE], in_=weg[e_, :, :].rearrange("(p kc) f -> p kc f", p=128)), [], [], grp=cgrp)
        P.op("gpsimd", lambda e: e.dma_start(
            out=WGU[rws, :, DE:2 * DE], in_=weu[e_, :, :].rearrange("(p kc) f -> p kc f", p=128)), [], [], grp=cgrp)
        P.op("gpsimd", lambda e: e.dma_start(
            out=WDS[rws, :, :], in_=wed[e_, :, :].rearrange("(p kc) n -> p kc n", p=128)), [], [], grp=cgrp)
    load("sync", kern_sb, kern_sb[:], kern[:, :, :])
    load("sync", cvec_sb, cvec_sb[:], cvec[:, :, :])
    load("sync", bm_sb, bm_sb[:], bm[:, :, :, :])
    load("sync", wr_sb, wr_sb[:], wr.rearrange("(kc p) n -> p kc n", p=128))
    load("sync", brb, brb[:], br[0, :].partition_broadcast(128))
    load("sync", gqb, gqb[:], gq[0, :].partition_broadcast(128))
    load("sync", gkb, gkb[:], gk[0, :].partition_broadcast(128))
    load("sync", snk, snk[:], sinks[0, :].partition_broadcast(128))
    resident_end = off[0]
    stgw = [sb(f"stgw{i}", [128, 4096], F32) for i in range(2)]
    w_in_v = w_in.rearrange("(kc p) n -> p kc n", p=128)
    wco_v = wco.rearrange("(kc p) n -> p kc n", p=128)
    wao_v = wao.rearrange("(kc p) n -> p kc n", p=128)
    wout_v = wout.rearrange("(kc p) n -> p kc n", p=128)
    jobs = []
    for kc in range(8):
        jobs.append((w_in_v[:, kc, :], w_in_sb, w_in_sb[:, kc, :], 3840))
    jobs.append((wco_v, wco_sb, wco_sb[:], 4096))
    jobs.append((wao_v, wao_sb, wao_sb[:], 4096))
    for hh_ in range(2):
        jobs.append((wout_v[:, hh_ * 4:(hh_ + 1) * 4, :], wout_sb, wout_sb[:, hh_ * 4:(hh_ + 1) * 4, :], 4096))
    for ji, (src, dtl, dap, nel) in enumerate(jobs):
        st_ = stgw[ji % 2]
        sview = st_[:, 0:nel]
        if len(src.shape) == 3:
            sview = sview.rearrange("p (k n) -> p k n", k=src.shape[1])
        load("scalar", st_, sview, src)
        if ji % 2 == 0:
            V("tensor_copy", [st_], [dtl], out=dap, in_=sview)
        else:
            A("copy", [st_], [dtl], out=dap, in_=sview)

    csb = sb("csb", [128, 8], F32)
    scs = sb("scs", [128, 8], F32)
    scb = sb("scb", [128, 8, 128], F32)
    gmb = sb("gmb", [128, D], F32)
    gfb = sb("gfb", [128, D], F32)
    wa = [sb(f"wa{i}", [128, 8, 256], F32) for i in range(2)]
    modT = sb("modT", [128, 6 * D], F32)
    G1 = modT[:, 2 * D:3 * D]
    load("sync", csb, csb[:], c_col[:, :])
    load("sync", modT, modT[:], b_ada[0, :].partition_broadcast(128))
    load("sync", gmb, gmb[:], gmix[0, :].partition_broadcast(128))
    load("sync", gfb, gfb[:], gffn[0, :].partition_broadcast(128))
    A("activation", [csb], [scs], out=scs[:], in_=csb[:], func=ACTF.Silu)
    for kc in range(8):
        V("tensor_copy", [scs], [scb], out=scb[:, kc, :], in_=scs[:, kc:kc + 1].to_broadcast([128, 128]))
    w_ada_v = w_ada.rearrange("(kc p) n -> p kc n", p=128)
    for n in range(24):
        wt = wa[n % 2]
        load("sync", wt, wt[:], w_ada_v[:, :, n * 256:(n + 1) * 256])
        pm = gmm()
        for kc in range(8):
            MM(pm, pm[:, 0:256], scb, scb[:, kc, :], wt, wt[:, kc, :], kc == 0, kc == 7)
        V("tensor_tensor", [pm, modT], [modT], out=modT[:, n * 256:(n + 1) * 256], in0=pm[:, 0:256],
          in1=modT[:, n * 256:(n + 1) * 256], op=ALU.add)
    V("scalar_tensor_tensor", [modT, gmb], [modT], out=modT[:, D:2 * D], in0=modT[:, D:2 * D], scalar=1.0, in1=gmb[:],
      op0=ALU.add, op1=ALU.mult)
    V("scalar_tensor_tensor", [modT, gfb], [modT], out=modT[:, 4 * D:5 * D], in0=modT[:, 4 * D:5 * D], scalar=1.0,
      in1=gfb[:], op0=ALU.add, op1=ALU.mult)
    V("tensor_copy", [modT], [mod], out=mod[:, 0:2 * D], in_=modT[:, 0:2 * D])
    V("tensor_copy", [modT], [mod], out=mod[:, 2 * D:4 * D], in_=modT[:, 3 * D:5 * D])
    for kc in range(8):
        V("scalar_tensor_tensor", [wout_sb, modT], [wout_sb], out=wout_sb[:, kc, :], in0=wout_sb[:, kc, :], scalar=0.5,
          in1=G1, op0=ALU.mult, op1=ALU.mult)
    store("sync", modT, G2d[:, :], modT[:, 5 * D:6 * D])
    P.barrier()
    off[0] = resident_end
    NS_run = NS
    if stop == "p0":
        NS_run = 0

    G("memset", [], [onesdiv], onesdiv[:], 1.0 / 512.0)
    V("tensor_scalar", [gqb], [gq8], out=gq8[:], in0=gqb[:, None, :].to_broadcast([128, 8, 64]),
      scalar1=0.125, scalar2=None, op0=ALU.mult)
    V("tensor_copy", [gkb], [gk2], out=gk2[:], in_=gkb[:, None, :].to_broadcast([128, 2, 64]))
    V("reduce_max", [gqb], [mq], out=mq[:], in_=gqb[:], axis=AX.X, apply_absolute_value=True)
    V("reduce_max", [gkb], [mk], out=mk[:], in_=gkb[:], axis=AX.X, apply_absolute_value=True)
    V("scalar_tensor_tensor", [mq, mk], [negc], out=negc[:], in0=mq[:], scalar=-8.0, in1=mk[:],
      op0=ALU.mult, op1=ALU.mult)
    A("activation", [snk, negc], [esink], out=esink[:], in_=snk[:], func=ACTF.Exp, bias=negc[:, 0:1])

    xin = [sb(f"xin{i}", [128, D], F32) for i in range(2)]
    xr = [sb(f"xr{i}", [128, D], F32) for i in range(2)]
    tmpf = sb("tmpf", [128, D], F32)
    h2f = tmpf
    Lrow = [sb(f"Lrow{i}", [128, 36], F32) for i in range(2)]
    h2Tt = sb("h2Tt", [128, 1024], F32)
    hb = [sb(f"hb{i}", [128, D], BF16) for i in range(1)]
    h2b = [sb(f"h2b{i}", [128, D], BF16) for i in range(1)]
    hTs = [sb(f"hT{i}", [128, 8, TT], BF16) for i in range(2)]
    ubuf = sb("ubuf", [128, 4, TT + 30], F32)
    ub3 = sb("ub3", [128, TT + 30], BF16)
    dg = [sb(f"dg{i}", [128, 128], BF16) for i in range(4)]
    dgi = [0]
    acc = [sb(f"acc{c}", [128, TT], F32) for c in range(4)]
    cbf = sb("cbf", [128, 4, TT], BF16)
    sqb = sb("sqb", [128, 4, TT], BF16)
    uT = sb("uT", [128, 4, TT], BF16)
    oT = sb("oT", [128, 4, TT], BF16)
    mT = sb("mT", [128, 8, TT], BF16)
    sgc = sb("sgc", [128, 8, TT], BF16)
    sga = sb("sga", [128, 8, TT], BF16)
    fgA = mmb[2]
    fgB = mmb[2]
    sgb = [sb(f"sgb{i}", [128, TT], F32) for i in range(1)] * 2
    s1 = [sb(f"s1_{i}", [128, TT], F32) for i in range(2)]
    s2 = [sb(f"s2_{i}", [128, TT], F32) for i in range(2)]
    t1 = s1
    t2 = s2
    mean_sb = s1[0]
    m2 = s2[0]
    var = m2
    rln = m2
    ssq = sb("ssq", [128, 1], F32)
    msq = sb("msq", [128, 1], F32)
    rstd = sb("rstd", [128, 1], F32)
    ssq2 = sb("ssq2", [128, 1], F32)
    msq2 = sb("msq2", [128, 1], F32)
    rstd2 = sb("rstd2", [128, 1], F32)
    ssq10 = sb("ssq10", [128, 10], F32)
    ms10 = sb("ms10", [128, 10], F32)
    rs10 = sb("rs10", [128, 10], F32)
    qn = sb("qn", [128, 512], BF16)
    kpad = sb("kpad", [128, 2, 2, 128], BF16)
    vaug = [sb(f"vaug{i}", [128, 2, 65], BF16) for i in range(2)]
    qT = sb("qT", [128, 4, 128], BF16)
    kT = [sb(f"kT{i}", [128, 4, 128], BF16) for i in range(2)]
    lgT = sb("lgT", [128, 1024], F32)
    PT = sb("PT", [128, 4, 2, 128], BF16)
    den = sb("den", [128, 4], F32)
    rden = sb("rden", [128, 4], F32)
    onb = sb("onb", [128, 512], BF16)
    phase1_end = off[0]

    for i in range(2):
        G("memset", [], [vaug[i]], vaug[i][:], 1.0)
    G("memset", [], [kpad], kpad[:], 0.0)

    def stageA(s):
        hT = hTs[s % 2]
        for j in range(NBS):
            blk = s * NBS + j
            xi = xin[blk % 2]
            load("sync", xi, xi[:], x[blk * 128:(blk + 1) * 128, :])
            A("activation", [xi], [hb[0], ssq], out=hb[0][:], in_=xi[:], func=ACTF.Square, accum_out=ssq[:, 0:1])
            V("tensor_scalar", [ssq], [msq], out=msq[:], in0=ssq[:], scalar1=1.0 / D, scalar2=EPS,
              op0=ALU.mult, op1=ALU.add)
            G("tensor_tensor", [msq, nhalf], [rstd], out=rstd[:], in0=msq[:], in1=nhalf[:], op=ALU.pow)
            V("scalar_tensor_tensor", [xi, rstd, mod], [tmpf], out=tmpf[:], in0=xi[:], scalar=rstd[:, 0:1],
              in1=A1, op0=ALU.mult, op1=ALU.mult)
            hbt = hb[0]
            V("tensor_tensor", [tmpf, mod], [hbt], out=hbt[:], in0=tmpf[:], in1=B1, op=ALU.add)
            pt = gtp()
            for kc in range(8):
                TR(pt, pt[:, kc * 128:(kc + 1) * 128], hbt, hbt[:, kc * 128:(kc + 1) * 128], ident_b)
            A("copy", [pt], [hT], out=hT[:, :, j * 128:(j + 1) * 128],
              in_=pt[:].rearrange("p (k t) -> p k t", k=8))

    def stageB(s):
        hT = hTs[s % 2]
        if s == 0:
            G("memset", [], [ubuf], ubuf[:, :, 0:30], 0.0)
            G("memset", [], [ub3], ub3[:, 0:30], 0.0)
        else:
            A("copy", [ubuf], [ubuf], out=ubuf[:, 0:3, 0:30], in_=ubuf[:, 0:3, TT:TT + 30])
            A("copy", [ub3], [ub3], out=ub3[:, 0:30], in_=ub3[:, TT:TT + 30])
        for c in range(4):
            pa = gmm_ab()
            for kc in range(8):
                MM(pa, pa[:, 0:TT], w_in_sb, w_in_sb[:, kc, c * 128:(c + 1) * 128], hT, hT[:, kc, :], kc == 0, kc == 7)
            pb = gmm_ab()
            for kc in range(8):
                MM(pb, pb[:, 0:TT], w_in_sb, w_in_sb[:, kc, 512 + c * 128:512 + (c + 1) * 128], hT, hT[:, kc, :],
                   kc == 0, kc == 7)
            sg = sgb[c % 2]
            A("activation", [pb], [sg], out=sg[:], in_=pb[:, 0:TT], func=ACTF.Sigmoid)
            if c == 3:
                V("tensor_tensor", [pa, sg], [ub3], out=ub3[:, 30:30 + TT], in0=pa[:, 0:TT], in1=sg[:], op=ALU.mult)
            else:
                V("tensor_tensor", [pa, sg], [ubuf], out=ubuf[:, c, 30:30 + TT], in0=pa[:, 0:TT], in1=sg[:], op=ALU.mult)

    def stageC(s):
        bank = mmb[1]
        for tap in range(31):
            dgt = dg[dgi[0] % 4]
            dgi[0] += 1
            A("activation", [ident_f, kern_sb], [dgt], out=dgt[:], in_=ident_f[:], func=ACTF.Identity,
              scale=kern_sb[:, 3, tap:tap + 1])
            MM(bank, bank[:, 0:TT], dgt, dgt[:], ub3, ub3[:, tap:tap + TT], tap == 0, tap == 30)
        A("activation", [bank, cvec_sb], [acc[3]], out=acc[3][:], in_=bank[:, 0:TT], func=ACTF.Identity,
          bias=cvec_sb[:, 3, 0:1])
        pe_part = P.defer
        P.defer = []
        for c in range(3):
            V("tensor_scalar", [ubuf, kern_sb, cvec_sb], [acc[c]], out=acc[c][:], in0=ubuf[:, c, 0:TT],
              scalar1=kern_sb[:, c, 0:1], scalar2=cvec_sb[:, c, 0:1], op0=ALU.mult, op1=ALU.add)
        for tap in range(1, 31):
            for c in range(3):
                V("scalar_tensor_tensor", [ubuf, kern_sb, acc[c]], [acc[c]], out=acc[c][:],
                  in0=ubuf[:, c, tap:tap + TT], scalar=kern_sb[:, c, tap:tap + 1], in1=acc[c][:],
                  op0=ALU.mult, op1=ALU.add)
        dve_part = P.defer
        P.defer = merge(pe_part, dve_part)

    def stageD(s):
        sqv = sqb
        for c in range(4):
            A("copy", [acc[c]], [cbf], out=cbf[:, c, :], in_=acc[c][:])
            A("activation", [acc[c]], [sqb], out=sqv[:, c, :], in_=acc[c][:], func=ACTF.Square)
        pmn = mmb[1]
        for c in range(4):
            MM(pmn, pmn[:, 0:TT], onesdiv, onesdiv[:], cbf, cbf[:, c, :], c == 0, c == 3)
        pq2 = mmb[1]
        for c in range(4):
            MM(pq2, pq2[:, TT:2 * TT], onesdiv, onesdiv[:], sqb, sqv[:, c, :], c == 0, c == 3)
        A("copy", [pmn], [mean_sb], out=mean_sb[:], in_=pmn[:, 0:TT])
        V("tensor_tensor", [mean_sb], [m2], out=m2[:], in0=mean_sb[:], in1=mean_sb[:], op=ALU.mult)
        V("scalar_tensor_tensor", [pq2, m2], [m2], out=var[:], in0=pq2[:, TT:2 * TT], scalar=EPS, in1=m2[:],
          op0=ALU.add, op1=ALU.subtract)
        A("sqrt", [m2], [m2], out=m2[:], in_=m2[:])
        V("reciprocal", [m2], [m2], out=m2[:], in_=m2[:])
        for c in range(4):
            V("tensor_tensor", [acc[c], mean_sb], [acc[c]], out=acc[c][:], in0=acc[c][:], in1=mean_sb[:],
              op=ALU.subtract)
        for c in range(4):
            V("tensor_tensor", [acc[c], rln], [acc[c]], out=acc[c][:], in0=acc[c][:], in1=rln[:], op=ALU.mult)
        for c in range(4):
            A("activation", [acc[c], cvec_sb], [uT], out=uT[:, c, :], in_=acc[c][:], func=ACTF.Silu,
              bias=cvec_sb[:, c, 2:3], scale=cvec_sb[:, c, 1:2])

    def stageE(s):
        hT = hTs[s % 2]
        for j in range(NBS):
            blk = s * NBS + j
            tok = slice(j * 128, (j + 1) * 128)
            for kc in range(8):
                MM(attA, attA[:, 0:512], hT, hT[:, kc, tok], w_in_sb, w_in_sb[:, kc, 1024:1536], kc == 0, kc == 7)
            for kc in range(8):
                MM(attB, attB[:, 0:256], hT, hT[:, kc, tok], w_in_sb, w_in_sb[:, kc, 1536:1792], kc == 0, kc == 7)
            A("activation", [attA], [tmpf], out=tmpf[:, 0:512], in_=attA[:, 0:512], func=ACTF.Square)
            A("activation", [attB], [tmpf], out=tmpf[:, 512:640], in_=attB[:, 0:128], func=ACTF.Square)
            V("tensor_reduce", [tmpf], [ssq10], out=ssq10[:], in_=tmpf[:, 0:640].rearrange("p (h d) -> p h d", d=64),
              axis=AX.X, op=ALU.add)
            V("tensor_scalar", [ssq10], [ms10], out=ms10[:], in0=ssq10[:], scalar1=1.0 / 64, scalar2=EPS,
              op0=ALU.mult, op1=ALU.add)
            G("tensor_tensor", [ms10, nhalf], [rs10], out=rs10[:], in0=ms10[:],
              in1=nhalf[:, 0:1].to_broadcast([128, 10]), op=ALU.pow)
            V("tensor_tensor", [attA, rs10, ssq10], [tmpf], out=tmpf[:, 0:512].rearrange("p (h d) -> p h d", d=64), in0=attA[:, 0:512].rearrange("p (h d) -> p h d", d=64),
              in1=rs10[:, 0:8, None].to_broadcast([128, 8, 64]), op=ALU.mult)
            V("tensor_tensor", [tmpf, gq8], [qn], out=qn[:].rearrange("p (h d) -> p h d", d=64), in0=tmpf[:, 0:512].rearrange("p (h d) -> p h d", d=64),
              in1=gq8[:], op=ALU.mult)
            V("tensor_tensor", [attB, rs10], [tmpf], out=tmpf[:, 512:640].rearrange("p (h d) -> p h d", d=64), in0=attB[:, 0:128].rearrange("p (h d) -> p h d", d=64),
              in1=rs10[:, 8:10, None].to_broadcast([128, 2, 64]), op=ALU.mult)
            for dd in range(2):
                V("tensor_tensor", [tmpf, gk2], [kpad], out=kpad[:, :, dd, dd * 64:(dd + 1) * 64],
                  in0=tmpf[:, 512:640].rearrange("p (h d) -> p h d", d=64), in1=gk2[:], op=ALU.mult)
            va = vaug[blk % 2]
            A("copy", [attB], [va], out=va[:, :, 0:64], in_=attB[:, 128:256].rearrange("p (h d) -> p h d", d=64))
            pt = gtp()
            for c in range(4):
                TR(pt, pt[:, c * 128:(c + 1) * 128], qn, qn[:, c * 128:(c + 1) * 128], ident_b)
            kflat = kpad[:].rearrange("p a b d -> p (a b d)")
            for kv in range(4):
                TR(pt, pt[:, (4 + kv) * 128:(5 + kv) * 128], kpad, kflat[:, kv * 128:(kv + 1) * 128], ident_b)
            kTc = kT[blk % 2]
            kTp = kT[(blk - 1) % 2]
            A("copy", [pt], [qT], out=qT[:], in_=pt[:, 0:512].rearrange("p (c t) -> p c t", c=4))
            A("copy", [pt], [kTc], out=kTc[:], in_=pt[:, 512:1024].rearrange("p (c t) -> p c t", c=4))
            js = [1] if blk == 0 else [0, 1]
            jsl = slice(js[0], 2)
            attv = [(attA, attA[:].rearrange("p (h j q) -> p h j q", h=2, j=2)),
                    (attB, attB[:].rearrange("p (h j q) -> p h j q", h=2, j=2))]
            lg4 = lgT[:].rearrange("p (h j q) -> p h j q", h=4, j=2)
            for g in range(2):
                for hh in range(4):
                    h = 4 * g + hh
                    c, r = h // 2, h % 2
                    for jj in js:
                        kTt = kTp if jj == 0 else kTc
                        at_, av_ = attv[hh // 2]
                        MM(at_, av_[:, hh % 2, jj, :], kTt, kTt[:, 2 * g + r, :], qT, qT[:, c, :], True, True)
                for hp in range(2):
                    at_, av_ = attv[hp]
                    V("tensor_tensor", [at_, bm_sb], [lgT], out=lg4[:, 2 * hp:2 * hp + 2, jsl, :], in0=av_[:, :, jsl, :],
                      in1=bm_sb[:, 4 * g + 2 * hp:4 * g + 2 * hp + 2, jsl, :], op=ALU.add)
                A("activation", [lgT, negc], [PT], out=PT[:, :, jsl, :], in_=lg4[:, :, jsl, :], func=ACTF.Exp,
                  bias=negc[:, 0:1])
                po = mmb[0]
                po3 = po[:, 0:260].rearrange("p (h e) -> p h e", e=65)
                vp = vaug[(blk - 1) % 2]
                for hh in range(4):
                    for jj in js:
                        vt = vp if jj == 0 else va
                        MM(po, po3[:, hh, :], PT, PT[:, hh, jj, :], vt, vt[:, g, :], jj == js[0], jj == js[-1])
                V("tensor_tensor", [po, esink], [den], out=den[:], in0=po3[:, :, 64], in1=esink[:, 4 * g:4 * g + 4],
                  op=ALU.add)
                V("reciprocal", [den], [rden], out=rden[:], in_=den[:])
                V("tensor_tensor", [po, rden], [onb],
                  out=onb[:].rearrange("p (h d) -> p h d", d=64)[:, 4 * g:4 * g + 4, :], in0=po3[:, :, 0:64],
                  in1=rden[:, :, None].to_broadcast([128, 4, 64]), op=ALU.mult)
            pt = gtp()
            for c in range(4):
                TR(pt, pt[:, c * 128:(c + 1) * 128], onb, onb[:, c * 128:(c + 1) * 128], ident_b)
            A("copy", [pt], [oT], out=oT[:, :, tok], in_=pt[:, 0:512].rearrange("p (c t) -> p c t", c=4))

    def stageFg(s):
        hT = hTs[s % 2]
        for mc in range(8):
            for kc in range(8):
                MM(fgA, fgA[:, 0:TT], w_in_sb, w_in_sb[:, kc, 1792 + mc * 128:1792 + (mc + 1) * 128], hT, hT[:, kc, :],
                   kc == 0, kc == 7)
            A("activation", [fgA], [sgc], out=sgc[:, mc, :], in_=fgA[:, 0:TT], func=ACTF.Tanh, scale=0.5)
            for kc in range(8):
                MM(fgB, fgB[:, TT:2 * TT], w_in_sb, w_in_sb[:, kc, 2816 + mc * 128:2816 + (mc + 1) * 128], hT, hT[:, kc, :],
                   kc == 0, kc == 7)
            A("activation", [fgB], [sga], out=sga[:, mc, :], in_=fgB[:, TT:2 * TT], func=ACTF.Tanh, scale=0.5)

    def stageF(s):
        for mc in range(8):
            ms_ = slice(mc * 128, (mc + 1) * 128)
            i2 = mc % 2
            bx, by = (mmb[2], mmb[3]) if i2 == 0 else (attA, attB)
            for kc in range(4):
                MM(bx, bx[:, 0:TT], wco_sb, wco_sb[:, kc, ms_], uT, uT[:, kc, :], kc == 0, kc == 3)
            V("scalar_tensor_tensor", [sgc, bx], [s1[i2]], out=s1[i2][:], in0=sgc[:, mc, :], scalar=1.0, in1=bx[:, 0:TT],
              op0=ALU.add, op1=ALU.mult)
            for kc in range(4):
                MM(by, by[:, 0:TT], wao_sb, wao_sb[:, kc, ms_], oT, oT[:, kc, :], kc == 0, kc == 3)
            V("scalar_tensor_tensor", [sga, by], [s2[i2]], out=s2[i2][:], in0=sga[:, mc, :], scalar=1.0, in1=by[:, 0:TT],
              op0=ALU.add, op1=ALU.mult)
            V("tensor_tensor", [s1[i2], s2[i2]], [mT], out=mT[:, mc, :], in0=s1[i2][:], in1=s2[i2][:], op=ALU.add)

    def stageG(s):
        for j in range(NBS):
            blk = s * NBS + j
            tok = slice(j * 128, (j + 1) * 128)
            rows = slice(blk * 128, (blk + 1) * 128)
            xrt = xr[blk % 2]
            load("sync", xrt, xrt[:], x[rows, :])
            for nh in range(2):
                cs = slice(nh * 512, (nh + 1) * 512)
                pp = mmb[3]
                for kc in range(8):
                    MM(pp, pp[:], mT, mT[:, kc, tok], wout_sb, wout_sb[:, kc, cs], kc == 0, kc == 7)
                V("tensor_tensor", [pp, xrt], [xrt], out=xrt[:, cs], in0=pp[:], in1=xrt[:, cs], op=ALU.add)
            store("gpsimd", xrt, out[rows, :], xrt[:])
            A("activation", [xrt], [h2b[0], ssq2], out=h2b[0][:], in_=xrt[:], func=ACTF.Square, accum_out=ssq2[:, 0:1])
            V("tensor_scalar", [ssq2], [msq2], out=msq2[:], in0=ssq2[:], scalar1=1.0 / D, scalar2=EPS,
              op0=ALU.mult, op1=ALU.add)
            G("tensor_tensor", [msq2, nhalf], [rstd2], out=rstd2[:], in0=msq2[:], in1=nhalf[:], op=ALU.pow)
            h2f = xrt
            V("scalar_tensor_tensor", [xrt, rstd2, mod], [xrt], out=xrt[:], in0=xrt[:], scalar=rstd2[:, 0:1],
              in1=A2, op0=ALU.mult, op1=ALU.mult)
            V("tensor_tensor", [xrt, mod], [xrt], out=xrt[:], in0=xrt[:], in1=B2, op=ALU.add)
            hbt = h2b[0]
            A("copy", [h2f], [hbt], out=hbt[:], in_=h2f[:])
            store("gpsimd", hbt, H2[rows, :], hbt[:])
            h2T3 = h2Tt[:].rearrange("p (k t) -> p k t", k=8)
            for hf in range(2):
                pp = mmb[3]
                for i in range(4):
                    kc = hf * 4 + i
                    TR(pp, pp[:, i * 128:(i + 1) * 128], h2f, h2f[:, kc * 128:(kc + 1) * 128], ident_f)
                if hf == 0:
                    A("copy", [pp], [h2Tt], out=h2T3[:, 0:4, :], in_=pp[:].rearrange("p (k t) -> p k t", k=4))
                else:
                    V("tensor_copy", [pp], [h2Tt], out=h2T3[:, 4:8, :], in_=pp[:].rearrange("p (k t) -> p k t", k=4))
            pp = mmb[3]
            for kc in range(8):
                MM(pp, pp[:, 0:36], h2Tt, h2T3[:, kc, :], wr_sb, wr_sb[:, kc, :], kc == 0, kc == 7)
            lr = Lrow[blk % 2]
            V("tensor_tensor", [pp, brb], [lr], out=lr[:], in0=pp[:, 0:36], in1=brb[:], op=ALU.add)
            store("gpsimd", lr, Ldram[:, blk, :], lr[:])
        if not phase1_only:
            for e_ in range(s * NE // NS, (s + 1) * NE // NS):
                expert_casts(e_)

    def run_stage(fn, s):
        P.defer = []
        fn(s)
        lst = P.defer
        P.defer = None
        return lst

    def merge(la, lb):
        res = []
        ia = ib = 0
        na, nb = len(la), len(lb)
        while ia < na or ib < nb:
            if ib >= nb or (ia < na and ia * nb <= ib * na):
                res.append(la[ia]); ia += 1
            else:
                res.append(lb[ib]); ib += 1
        return res

    def play(lst):
        for a in lst:
            P.op(*a)

    if NS_run:
        play(run_stage(stageA, 0) + run_stage(stageB, 0))
    for s in range(NS_run):
        eg = run_stage(stageE, s)
        if s > 0:
            eg = merge(eg, run_stage(stageG, s - 1))
        eg = merge(eg, run_stage(stageFg, s))
        play(merge(run_stage(stageC, s) + run_stage(stageD, s), eg))
        df = run_stage(stageF, s)
        if s + 1 < NS_run:
            df = merge(df, run_stage(stageA, s + 1) + run_stage(stageB, s + 1))
        play(df)
    if NS_run:
        play(run_stage(stageG, NS_run - 1))

    P.barrier()
    if debug:
        P.op("sync", lambda e: e.dma_start(out=Ldbg[:, :, :], in_=Ldram[:, :, :]), [], [], grp=P.grp("dbgL"))


    if not phase1_only:
        off[0] = persist_end
        KMAX = -(-T // BLK)
        w1 = sb("w1", [128, NB], F32)
        w2 = sb("w2", [128, NB], F32)
        d1i = sb("d1i", [128, NB], I32)
        d2i = sb("d2i", [128, NB], I32)
        blke_i = sb("blke_i", [128, NBLK], I32)
        Lt = sb("Lt", [128, NB, 36], F32)
        load("sync", Lt, Lt[:], Ldram[:, :, :])
        widx = sb("widx", [128, NBLK], I32)
        route_keep = off[0]
        ones_f = sb("ones_f", [128, 128], F32)
        ustr = sb("ustr", [128, 128], F32)
        gmax = sb("gmax", [128, NB], F32)
        ohg = sb("ohg", [128, NB, 4], F32)
        eg = sb("eg", [128, NB, 4], F32)
        sume = sb("sume", [128, NB], F32)
        ptop = sb("ptop", [128, NB], F32)
        tmp8 = sb("tmp8", [128, NB, 8], F32)
        elsel = sb("elsel", [128, NB, 8], F32)
        els2 = sb("els2", [128, NB, 8], F32)
        oh1 = sb("oh1", [128, NB, 8], F32)
        oh2 = sb("oh2", [128, NB, 8], F32)
        m1 = sb("m1", [128, NB], F32)
        m2v = sb("m2v", [128, NB], F32)
        ddv = sb("ddv", [128, NB], F32)
        e2v = sb("e2v", [128, NB], F32)
        OH1 = sb("OH1", [128, NB, 32], F32)
        OH2 = sb("OH2", [128, NB, 32], F32)
        TH = sb("TH", [128, NB, 32], F32)
        scn = [sb(f"scn{i}", [128, NB, 32], F32) for i in range(2)]
        cnt_p = sb("cnt_p", [128, 32], F32)
        tot_sb = sb("tot_sb", [128, 32], F32)
        pp_sb = sb("pp_sb", [128, 32], F32)
        thr_i = sb("thr_i", [128, KMAX], I32)
        thr_f = sb("thr_f", [128, KMAX], F32)
        cmpt = sb("cmpt", [128, 32, KMAX], F32)
        pc = sb("pc", [128, 32], F32)
        pe_ = [sb(f"pend{i}", [128, 32], F32) for i in range(2)]
        base = sb("base", [128, 32], F32)
        dst_f = sb("dst_f", [128, NB], F32)
        bthr_i = sb("bthr_i", [128, NBLK], I32)
        bthr_f = sb("bthr_f", [128, NBLK], F32)
        cmpb = sb("cmpb", [128, NBLK, 32], F32)
        blke_f = sb("blke_f", [128, NBLK], F32)
        hs = [sb(f"hs{i}", [128, D], BF16) for i in range(3)]
        pidx_i = sb("pidx_i", [128, 1], I32)
        pidx_f = sb("pidx_f", [128, 1], F32)
        widx_f = sb("widx_f", [128, NBLK], F32)

        G("memset", [], [ones_f], ones_f[:], 1.0)
        G("memset", [], [ustr], ustr[:], 1.0)
        G("affine_select", [ustr], [ustr], out=ustr[:], in_=ustr[:], pattern=[[1, 128]],
          compare_op=ALU.is_gt, fill=0.0, base=0, channel_multiplier=-1)
        G("iota", [], [thr_i], thr_i[:], pattern=[[BLK, KMAX]], base=0, channel_multiplier=0)
        G("iota", [], [bthr_i], bthr_i[:], pattern=[[BLK, NBLK]], base=0, channel_multiplier=0)
        V("tensor_copy", [thr_i], [thr_f], out=thr_f[:], in_=thr_i[:])
        V("tensor_copy", [bthr_i], [bthr_f], out=bthr_f[:], in_=bthr_i[:])

        gl = Lt[:, :, 0:4]
        el4 = Lt[:, :, 4:36].rearrange("p t (g e) -> p t g e", g=4)
        bc4 = lambda ap: ap[:, :, None].to_broadcast([128, NB, 4])
        bc8 = lambda ap: ap[:, :, None].to_broadcast([128, NB, 8])
        V("tensor_reduce", [Lt], [gmax], out=gmax[:], in_=gl, axis=AX.X, op=ALU.max)
        V("tensor_tensor", [Lt, gmax], [ohg], out=ohg[:], in0=gl, in1=bc4(gmax), op=ALU.is_equal)
        V("tensor_tensor", [Lt, gmax], [eg], out=eg[:], in0=gl, in1=bc4(gmax), op=ALU.subtract)
        A("activation", [eg], [eg], out=eg[:], in_=eg[:], func=ACTF.Exp)
        V("tensor_reduce", [eg], [sume], out=sume[:], in_=eg[:], axis=AX.X, op=ALU.add)
        V("reciprocal", [sume], [ptop], out=ptop[:], in_=sume[:])
        V("tensor_tensor", [Lt, ohg], [elsel], out=elsel[:], in0=el4[:, :, 0, :],
          in1=ohg[:, :, 0:1].to_broadcast([128, NB, 8]), op=ALU.mult)
        for g in range(1, 4):
            V("tensor_tensor", [Lt, ohg], [tmp8], out=tmp8[:], in0=el4[:, :, g, :],
              in1=ohg[:, :, g:g + 1].to_broadcast([128, NB, 8]), op=ALU.mult)
            V("tensor_tensor", [elsel, tmp8], [elsel], out=elsel[:], in0=elsel[:], in1=tmp8[:], op=ALU.add)
        V("tensor_reduce", [elsel], [m1], out=m1[:], in_=elsel[:], axis=AX.X, op=ALU.max)
        V("tensor_tensor", [elsel, m1], [oh1], out=oh1[:], in0=elsel[:], in1=bc8(m1), op=ALU.is_equal)
        V("scalar_tensor_tensor", [oh1, elsel], [els2], out=els2[:], in0=oh1[:], scalar=-1e30, in1=elsel[:],
          op0=ALU.mult, op1=ALU.add)
        V("tensor_reduce", [els2], [m2v], out=m2v[:], in_=els2[:], axis=AX.X, op=ALU.max)
        V("tensor_tensor", [els2, m2v], [oh2], out=oh2[:], in0=els2[:], in1=bc8(m2v), op=ALU.is_equal)
        V("tensor_tensor", [m2v, m1], [ddv], out=ddv[:], in0=m2v[:], in1=m1[:], op=ALU.subtract)
        A("activation", [ddv], [e2v], out=e2v[:], in_=ddv[:], func=ACTF.Exp)
        V("tensor_scalar", [e2v], [ddv], out=ddv[:], in0=e2v[:], scalar1=1.0, scalar2=None, op0=ALU.add)
        V("reciprocal", [ddv], [sume], out=sume[:], in_=ddv[:])
        V("tensor_tensor", [sume, ptop], [w1], out=w1[:], in0=sume[:], in1=ptop[:], op=ALU.mult)
        V("tensor_tensor", [w1, e2v], [w2], out=w2[:], in0=w1[:], in1=e2v[:], op=ALU.mult)
        for OHk, ohk in ((OH1, oh1), (OH2, oh2)):
            V("tensor_tensor", [ohg, ohk], [OHk], out=OHk[:].rearrange("p t (g e) -> p t g e", g=4),
              in0=ohg[:, :, :, None].to_broadcast([128, NB, 4, 8]),
              in1=ohk[:, :, None, :].to_broadcast([128, NB, 4, 8]), op=ALU.mult)
        V("tensor_tensor", [OH1, OH2], [TH], out=TH[:], in0=OH1[:], in1=OH2[:], op=ALU.add)
        V("tensor_reduce", [TH], [cnt_p], out=cnt_p[:], in_=TH[:].rearrange("p t e -> p e t"), axis=AX.X, op=ALU.add)
        pA = gmm()
        MM(pA, pA[:, 0:32], ustr, ustr[:], cnt_p, cnt_p[:], True, True)
        pB = gmm()
        MM(pB, pB[:, 0:32], ones_f, ones_f[:], cnt_p, cnt_p[:], True, True)
        A("copy", [pA], [pp_sb], out=pp_sb[:], in_=pA[:, 0:32])
        A("copy", [pB], [tot_sb], out=tot_sb[:], in_=pB[:, 0:32])
        V("tensor_tensor", [tot_sb, thr_f], [cmpt], out=cmpt[:], in0=tot_sb[:, :, None].to_broadcast([128, 32, KMAX]),
          in1=thr_f[:, None, :].to_broadcast([128, 32, KMAX]), op=ALU.is_gt)
        V("tensor_reduce", [cmpt], [pc], out=pc[:], in_=cmpt[:], axis=AX.X, op=ALU.add)
        V("tensor_scalar", [pc], [pc], out=pc[:], in0=pc[:], scalar1=float(BLK), scalar2=None, op0=ALU.mult)
        src = pc
        st = 1
        i = 0
        while st < 32:
            dstt = pe_[i % 2]
            V("tensor_tensor", [src], [dstt], out=dstt[:, st:], in0=src[:, st:], in1=src[:, 0:32 - st], op=ALU.add)
            V("tensor_copy", [src], [dstt], out=dstt[:, 0:st], in_=src[:, 0:st])
            src = dstt
            st *= 2
            i += 1
        pend = src
        V("tensor_tensor", [pend, pc], [base], out=base[:], in0=pend[:], in1=pc[:], op=ALU.subtract)
        V("tensor_tensor", [base, pp_sb], [base], out=base[:], in0=base[:], in1=pp_sb[:], op=ALU.add)
        src = TH
        st = 1
        i = 0
        while st < NB:
            dstt = scn[i % 2]
            V("tensor_tensor", [src], [dstt], out=dstt[:, st:, :], in0=src[:, st:, :], in1=src[:, 0:NB - st, :], op=ALU.add)
            G("tensor_copy", [src], [dstt], out=dstt[:, 0:st, :], in_=src[:, 0:st, :])
            src = dstt
            st *= 2
            i += 1
        pos = scn[i % 2] if src is not scn[i % 2] else scn[(i + 1) % 2]
        if src is TH:
            pos = scn[0]
        V("tensor_tensor", [src, TH], [pos], out=pos[:], in0=src[:], in1=TH[:], op=ALU.subtract)
        V("tensor_tensor", [pos, base], [pos], out=pos[:], in0=pos[:], in1=base[:, None, :].to_broadcast([128, NB, 32]),
          op=ALU.add)
        for OHk, dki in ((OH1, d1i), (OH2, d2i)):
            V("tensor_tensor", [OHk, pos], [OHk], out=OHk[:], in0=OHk[:], in1=pos[:], op=ALU.mult)
            V("tensor_reduce", [OHk], [dst_f], out=dst_f[:], in_=OHk[:], axis=AX.X, op=ALU.add)
            V("tensor_copy", [dst_f], [dki], out=dki[:], in_=dst_f[:])
        V("tensor_tensor", [pend, bthr_f], [cmpb], out=cmpb[:], in0=pend[:, None, :].to_broadcast([128, NBLK, 32]),
          in1=bthr_f[:, :, None].to_broadcast([128, NBLK, 32]), op=ALU.is_le)
        V("tensor_reduce", [cmpb], [blke_f], out=blke_f[:], in_=cmpb[:], axis=AX.X, op=ALU.add)
        V("tensor_scalar", [blke_f], [blke_f], out=blke_f[:], in0=blke_f[:], scalar1=float(NE - 1), scalar2=None,
          op0=ALU.min)
        V("tensor_copy", [blke_f], [blke_i], out=blke_i[:], in_=blke_f[:])
        G("iota", [], [pidx_i], pidx_i[:], pattern=[[0, 1]], base=0, channel_multiplier=1)
        V("tensor_copy", [pidx_i], [pidx_f], out=pidx_f[:], in_=pidx_i[:])
        V("tensor_scalar", [blke_f, pidx_f], [widx_f], out=widx_f[:], in0=blke_f[:], scalar1=128.0,
          scalar2=pidx_f[:, 0:1], op0=ALU.mult, op1=ALU.add)
        V("tensor_copy", [widx_f], [widx], out=widx[:], in_=widx_f[:])
        if debug:
            dbg = sb("dbg", [128, 4, NB], F32)
            V("tensor_copy", [w1], [dbg], out=dbg[:, 0, :], in_=w1[:])
            V("tensor_copy", [w2], [dbg], out=dbg[:, 1, :], in_=w2[:])
            V("tensor_copy", [d1i], [dbg], out=dbg[:, 2, :], in_=d1i[:])
            V("tensor_copy", [d2i], [dbg], out=dbg[:, 3, :], in_=d2i[:])
            store("sync", dbg, Ddbg[:, :, :], dbg[:])

        for t in range(0 if stop == "R" else NB):
            hst = hs[t % 3]
            load("sync", hst, hst[:], H2[t * 128:(t + 1) * 128, :])
            if hst.sg is None:
                hst.sg = P.grp("s_" + hst.name)
            for dki in (d1i, d2i):
                P.op("gpsimd", lambda e, hst=hst, dki=dki, t=t: e.indirect_dma_start(
                    out=XS[:, :], out_offset=bass.IndirectOffsetOnAxis(ap=dki[:, t:t + 1], axis=0),
                    in_=hst[:], in_offset=None), [hst, dki], [], grp=hst.sg)
        P.barrier()

        off[0] = route_keep
        g2t = sb("g2t", [128, D], F32)
        load("sync", g2t, g2t[:], G2d[:, :])
        Wgu = [sb(f"Wgu{i}", [128, 8, 2 * DE], BF16) for i in range(2)]
        Wd = [sb(f"Wd{i}", [128, 2, D], BF16) for i in range(2)]
        xs_in = [sb(f"xs_in{i}", [128, SUB, D], BF16) for i in range(3)]
        XTs = [sb(f"XT{i}", [128, 8, 128], BF16) for i in range(2)]
        sgls = [sb(f"sgl{i}", [128, DE], F32) for i in range(2)]
        hids = [sb(f"hid{i}", [128, DE], BF16) for i in range(2)]
        hidTs = [sb(f"hidT{i}", [128, 2, 128], BF16) for i in range(2)]
        ysb = [sb(f"ysb{i}", [128, SUB, D], F32) for i in range(2)]
        wgu_v = WGU.rearrange("r k f -> r (k f)")
        wds_v = WDS.rearrange("r k f -> r (k f)")

        def dyn_load(tl, src_v, b):
            if tl.lg is None:
                tl.lg = P.grp("l_" + tl.name)
            return P.op("gpsimd", lambda e: e.indirect_dma_start(
                out=tl[:].rearrange("p k f -> p (k f)"), out_offset=None, in_=src_v[:, :],
                in_offset=bass.IndirectOffsetOnAxis(ap=widx[:, b:b + 1], axis=0)), [widx], [tl], grp=tl.lg)

        def gathers(b):
            dyn_load(Wgu[b % 2], wgu_v, b)
            dyn_load(Wd[b % 2], wds_v, b)

        NBLK_run = 0 if stop in ("R", "S") else NBLK
        if NBLK_run:
            gathers(0)
        att_bf = [attA[:].bitcast(BF16), attB[:].bitcast(BF16)]
        att_tl = [attA, attB]

        def front_a(sl):
            k = sl % 2
            xst = xs_in[(sl // SUB) % 3]
            xv = xst[:, sl % SUB, :].rearrange("p (f k) -> p k f", k=8)
            XT = XTs[k]
            pt = tp[k]
            for kc in range(8):
                TR(pt, pt[:, kc * 128:(kc + 1) * 128], xst, xv[:, kc, :], ident_b)
            A("copy", [pt], [XT], out=XT[:], in_=pt[:].rearrange("p (k t) -> p k t", k=8))

        def front_b(sl):
            i = (sl // SUB) % 2
            k = sl % 2
            XT, sgl, hid = XTs[k], sgls[k], hids[k]
            pm = mmb[k]
            for kc in range(8):
                MM(pm, pm[:], XT, XT[:, kc, :], Wgu[i], Wgu[i][:, kc, :], kc == 0, kc == 7)
            A("activation", [pm], [sgl], out=sgl[:], in_=pm[:, 0:DE], func=ACTF.Silu)
            V("tensor_tensor", [pm, sgl], [hid], out=hid[:], in0=pm[:, DE:2 * DE], in1=sgl[:], op=ALU.mult)

        def back_a(sl):
            k = sl % 2
            hid, hidT = hids[k], hidTs[k]
            pt2t, pt2 = att_tl[k], att_bf[k]
            for kc in range(2):
                TR(pt2t, pt2[:, kc * 128:(kc + 1) * 128], hid, hid[:].rearrange("p (f k) -> p k f", k=2)[:, kc, :], ident_b)
            A("copy", [pt2t], [hidT], out=hidT[:], in_=pt2[:, 0:256].rearrange("p (k t) -> p k t", k=2))

        def back_b(sl):
            i = (sl // SUB) % 2
            k = sl % 2
            rows = slice(sl * 128, (sl + 1) * 128)
            hidT = hidTs[k]
            bb = sl // SUB
            yst = ysb[bb % 2]
            for nh in range(2):
                po = mmb[2 + nh]
                for kc in range(2):
                    MM(po, po[:], hidT, hidT[:, kc, :], Wd[i], Wd[i][:, kc, nh * 512:(nh + 1) * 512], kc == 0, kc == 1)
                V("tensor_tensor", [po, g2t], [yst], out=yst[:, sl % SUB, nh * 512:(nh + 1) * 512], in0=po[:],
                  in1=g2t[:, nh * 512:(nh + 1) * 512], op=ALU.mult)
            if sl % SUB == SUB - 1:
                store("sync", yst, YS[bb * BLK:(bb + 1) * BLK, :].rearrange("(s p) d -> p s d", p=128), yst[:])

        NSL = NBLK_run * SUB

        def xload(bb):
            xst = xs_in[bb % 3]
            load("sync", xst, xst[:], XS[bb * BLK:(bb + 1) * BLK, :].rearrange("(s p) d -> p s d", p=128))

        if NSL:
            for q_ in range(min(2, NBLK_run)):
                xload(q_)
            front_a(0)
            if NSL > 1:
                front_a(1)
            front_b(0)
        for sl in range(NSL):
            if sl % SUB == 0 and sl // SUB + 2 < NBLK_run:
                xload(sl // SUB + 2)
            if sl % SUB == 0 and sl // SUB + 1 < NBLK_run:
                gathers(sl // SUB + 1)
            if sl + 2 < NSL:
                front_a(sl + 2)
            back_a(sl)
            if sl + 1 < NSL:
                front_b(sl + 1)
            back_b(sl)
        P.barrier()

        y1r = [sb(f"y1r{i}", [128, D], F32) for i in range(2)]
        y2r = [sb(f"y2r{i}", [128, D], F32) for i in range(2)]
        xo = [sb(f"xo{i}", [128, D], F32) for i in range(2)]
        for t in range(0 if stop in ("R", "S", "X") else NB):
            rows = slice(t * 128, (t + 1) * 128)
            y1, y2, xot = y1r[t % 2], y2r[t % 2], xo[t % 2]
            for yt, dki in ((y1, d1i), (y2, d2i)):
                if yt.lg is None:
                    yt.lg = P.grp("l_" + yt.name)
                P.op("gpsimd", lambda e, yt=yt, dki=dki, t=t: e.indirect_dma_start(
                    out=yt[:], out_offset=None, in_=YS[:, :],
                    in_offset=bass.IndirectOffsetOnAxis(ap=dki[:, t:t + 1], axis=0)), [dki], [yt], grp=yt.lg)
            load("sync", xot, xot[:], out[rows, :])
            V("scalar_tensor_tensor", [y1, w1, xot], [xot], out=xot[:], in0=y1[:], scalar=w1[:, t:t + 1], in1=xot[:],
              op0=ALU.mult, op1=ALU.add)
            V("scalar_tensor_tensor", [y2, w2, xot], [xot], out=xot[:], in0=y2[:], scalar=w2[:, t:t + 1], in1=xot[:],
              op0=ALU.mult, op1=ALU.add)
            store("scalar", xot, out[rows, :], xot[:])

    P.barrier()
    P.op("sync", lambda e: e.nop())
    P.finalize()
    sems = {e: nc.alloc_semaphore("sem_" + e) for e in ENGS}
    for g in P.grps:
        g.sem = nc.alloc_semaphore("g_" + g.name)
    with nc.Block() as block:
        @block.tensor
        def _(e):
            P.emit_engine("tensor", e, sems)

        @block.vector
        def _(e):
            P.emit_engine("vector", e, sems)

        @block.scalar
        def _(e):
            P.emit_engine("scalar", e, sems)

        @block.gpsimd
        def _(e):
            P.emit_engine("gpsimd", e, sems)

        @block.sync
        def _(e):
            P.emit_engine("sync", e, sems)
    return nc


def _t5_bucket_table():
    W = 128
    qi = np.arange(W)[:, None]
    kj = np.arange(2 * W)[None, :]
    dist = qi + W - kj
    in_window = (dist >= 0) & (dist < W)
    dc = np.clip(dist, 0, 128)
    max_exact = 16
    d = np.maximum(dc, 1).astype(np.float32)
    large = max_exact + (np.log(d / max_exact) / math.log(128 / max_exact) * (32 - max_exact)).astype(np.int32)
    large = np.minimum(large, 31)
    bucket = np.where(dc < max_exact, dc, large)
    return bucket, in_window


def prep_shared(inp):
    f = lambda a: np.ascontiguousarray(np.asarray(a, dtype=np.float32))
    bucket, in_window = _t5_bucket_table()
    tab = f(inp["rel_bias_table"])
    bias = tab[bucket]
    bias = np.where(in_window[:, :, None], bias, np.float32(-1e30))
    bmh = bias.reshape(128, 2, 128, 8).transpose(2, 3, 1, 0)
    sh = {
        "w_ada": f(inp["w_ada"][0]),
        "b_ada": f(inp["b_ada"][0]).reshape(1, -1),
        "gmix": f(inp["norm_mix_g"][0]).reshape(1, -1),
        "gffn": f(inp["norm_ffn_g"][0]).reshape(1, -1),
        "w_in": f(inp["w_in"][0]),
        "kern": f(np.asarray(inp["dw_kernel"][0]).reshape(31, 4, 128).transpose(2, 1, 0)),
        "cvec": f(np.stack([np.asarray(inp["dw_bias"][0]).reshape(4, 128).T,
                            np.asarray(inp["conv_ln_g"][0]).reshape(4, 128).T,
                            np.asarray(inp["conv_ln_b"][0]).reshape(4, 128).T], axis=2)),
        "wco": f(inp["w_conv_out"][0]),
        "wao": f(inp["w_attn_out"][0]),
        "wout": f(inp["w_out"][0]),
        "gq": f(inp["q_norm_g"][0]).reshape(1, -1),
        "gk": f(inp["k_norm_g"][0]).reshape(1, -1),
        "sinks": f(inp["sinks"][0]).reshape(1, -1),
        "bm": f(bmh),
        "wr": f(np.concatenate([np.asarray(inp["w_router_group"][0]), np.asarray(inp["w_router_expert"][0])], axis=1)),
        "br": f(np.concatenate([np.asarray(inp["b_router_group"][0]), np.asarray(inp["b_router_expert"][0])])).reshape(1, -1),
        "weg": f(inp["w_exp_gate"][0]),
        "weu": f(inp["w_exp_up"][0]),
        "wed": f(inp["w_exp_down"][0]),
    }
    return sh


def kernel(**inputs):
    x = np.asarray(inputs["x"], dtype=np.float32)
    c = np.asarray(inputs["c"], dtype=np.float32)
    Bn, T, _ = x.shape
    sh = prep_shared(inputs)
    nc = build(T)
    in_maps = []
    for b in range(Bn):
        m = dict(sh)
        m["x"] = np.ascontiguousarray(x[b])
        m["c_col"] = np.ascontiguousarray(c[b].reshape(8, 128).T)
        in_maps.append(m)
    res = run_bass_kernel_spmd(nc, in_maps, core_ids=list(range(Bn)))
    return np.stack([np.asarray(r["out"]) for r in res.results], axis=0).astype(np.float32)
```
